# Optimizing a Trainium2 kernel written in Bass

```python
import jax
import jax.numpy as jnp
from jax import lax
import numpy as np

D_MODEL = 2048
BATCH = 8
SEQ = 4096
DEPTH = 1
DEC_BATCH = 8
DEC_SEQ = 2048
PAST_LEN = 128

RW_WIDTH = D_MODEL // 2
RW_HEAD_DIM = 64
RW_HEADS = RW_WIDTH // RW_HEAD_DIM
W_LORA = 64
A_LORA = 64
G_LORA = 128
RW_COLS = 3 * RW_WIDTH + 2 * W_LORA + 2 * A_LORA + G_LORA
RW_GN_EPS = 64e-5
HG_WIDTH = D_MODEL - RW_WIDTH
HG_HEADS = 8
HG_HEAD_DIM = HG_WIDTH // HG_HEADS
HG_COLS = 5 * HG_WIDTH
HG_CHUNK = 64
IN_COLS = RW_COLS + HG_COLS
N_EXPERTS = 64
TOP_K = 6
N_GROUPS = 8
TOPK_GROUPS = 4
D_EXPERT = 512
D_SHARED = 512
ROUTED_SCALE = 2.5
EXPERT_BLOCK = 256
NORM_EPS = 1e-6

kernel_name = 'hybrid_rwkv7_hgrn2_moe_encoder'


def _rms(x, g):
    xf = x.astype(jnp.float32)
    y = xf * lax.rsqrt(jnp.mean(xf * xf, axis=-1, keepdims=True) + NORM_EPS)
    return (y * g.astype(jnp.float32)).astype(x.dtype)


def _centred_shift(u):
    p = jnp.pad(u, ((0, 0), (1, 1), (0, 0)))
    return 0.5 * (p[:, :-2] + p[:, 2:])


def _flip(t):
    return jnp.flip(t, axis=1)


def _rwkv7_scan(r, w, k, v, kk, a):
    B, S, H, N = r.shape

    def step(state, inp):
        r_t, w_t, k_t, v_t, kk_t, a_t = inp
        sa = jnp.einsum('bhvk,bhk->bhv', state, -kk_t)
        state = (state * w_t[:, :, None, :] + sa[..., None] * (kk_t * a_t)[:, :, None, :]
                 + v_t[..., None] * k_t[:, :, None, :])
        return state, jnp.einsum('bhvk,bhk->bhv', state, r_t)

    xs = tuple(jnp.swapaxes(t, 0, 1) for t in (r, w, k, v, kk, a))
    _, o = lax.scan(step, jnp.zeros((B, H, N, N), jnp.float32), xs)
    return jnp.swapaxes(o, 0, 1)


def _rwkv7_group(zr, w0, w_up, a0, a_up, g_up, k_k, k_a, r_k, ln_w, ln_b):
    B, S, _ = zr.shape
    W = RW_WIDTH
    zr = zr.astype(jnp.float32)
    r = zr[..., 0:W]
    k = zr[..., W:2 * W]
    v = zr[..., 2 * W:3 * W]
    o0 = 3 * W
    w_lat = zr[..., o0:o0 + 2 * W_LORA].reshape(B, S, 2, W_LORA)
    o0 += 2 * W_LORA
    a_lat = zr[..., o0:o0 + 2 * A_LORA].reshape(B, S, 2, A_LORA)
    o0 += 2 * A_LORA
    g_lat = zr[..., o0:o0 + G_LORA]
    w_raw = w0 + jnp.einsum('bsdr,drw->bsdw', jnp.tanh(w_lat), w_up)
    decay = jnp.exp(-jnp.exp(-jax.nn.softplus(-w_raw) - 0.5))
    a = jax.nn.sigmoid(a0 + jnp.einsum('bsdr,drw->bsdw', a_lat, a_up))
    g = jax.nn.sigmoid(g_lat) @ g_up

    def heads(t):
        return t.reshape(*t.shape[:-1], RW_HEADS, RW_HEAD_DIM)

    kk = heads(k * k_k)
    kk = kk / jnp.maximum(jnp.sqrt(jnp.sum(kk * kk, axis=-1, keepdims=True)), 1e-12)
    k_dir = k[:, :, None, :] * (1.0 + (a - 1.0) * k_a)
    rh, vh = heads(r), heads(v)
    o_f = _rwkv7_scan(rh, heads(decay[:, :, 0]), heads(k_dir[:, :, 0]), vh, kk, heads(a[:, :, 0]))
    o_b = _flip(_rwkv7_scan(_flip(rh), _flip(heads(decay[:, :, 1])), _flip(heads(k_dir[:, :, 1])),
                            _flip(vh), _flip(kk), _flip(heads(a[:, :, 1]))))
    o = o_f + o_b
    mu = jnp.mean(o, axis=-1, keepdims=True)
    var = jnp.mean(jnp.square(o - mu), axis=-1, keepdims=True)
    o = ((o - mu) * lax.rsqrt(var + RW_GN_EPS)).reshape(B, S, W) * ln_w + ln_b
    k_bonus = heads(0.5 * (k_dir[:, :, 0] + k_dir[:, :, 1]))
    o = o + (jnp.sum(rh * k_bonus * r_k, axis=-1, keepdims=True) * vh).reshape(B, S, W)
    return o * g


def _hgrn2_chunk_scan(q, k, v, log_f):
    B, S, H, DK = q.shape
    DV = v.shape[-1]
    NC = S // HG_CHUNK

    def chunks(t):
        return t.reshape(B, NC, HG_CHUNK, H, t.shape[-1]).transpose(1, 0, 3, 2, 4)

    incl = jnp.tril(jnp.ones((HG_CHUNK, HG_CHUNK), bool))[:, :, None]

    def step(state, inp):
        qc, kc, vc, fc = inp
        b = jnp.cumsum(fc, axis=2)
        diff = b[:, :, :, None, :] - b[:, :, None, :, :]
        dec = jnp.exp(jnp.where(incl, diff, -jnp.inf))
        scores = jnp.einsum('bhtk,bhsk,bhtsk->bhts', qc, kc, dec)
        o = (jnp.einsum('bhts,bhsv->bhtv', scores, vc)
             + jnp.einsum('bhtk,bhkv->bhtv', qc * jnp.exp(b), state))
        b_last = b[:, :, -1:, :]
        state = (jnp.exp(b_last[:, :, 0, :])[..., None] * state
                 + jnp.einsum('bhsk,bhsv->bhkv', kc * jnp.exp(b_last - b), vc))
        return state, o

    xs = tuple(chunks(t) for t in (q, k, v, log_f))
    _, o = lax.scan(step, jnp.zeros((B, H, DK, DV), jnp.float32), xs)
    return o.transpose(1, 0, 3, 2, 4).reshape(B, S, H, DV)


def _hgrn2_group(zh, lb, norm_w):
    B, S, _ = zh.shape
    zh = zh.astype(jnp.float32)
    q, ff, fb, i, g = jnp.split(zh, 5, axis=-1)

    def heads(t):
        return t.reshape(B, S, HG_HEADS, HG_HEAD_DIM)

    q = heads(jax.nn.silu(q))
    i = heads(i)
    f_f = heads(lb[0] + (1.0 - lb[0]) * jax.nn.sigmoid(ff))
    f_b = heads(lb[1] + (1.0 - lb[1]) * jax.nn.sigmoid(fb))
    o_f = _hgrn2_chunk_scan(q, 1.0 - f_f, i, jnp.log(f_f))
    o_b = _flip(_hgrn2_chunk_scan(_flip(q), _flip(1.0 - f_b), _flip(i), _flip(jnp.log(f_b))))
    o = o_f + o_b
    o = o * lax.rsqrt(jnp.mean(o * o, axis=-1, keepdims=True) + NORM_EPS)
    return o.reshape(B, S, HG_WIDTH) * norm_w * jax.nn.silu(g)


def _token_mixer(h, w_in, rw_mu, rw_w0, rw_w_up, rw_a0, rw_a_up, rw_g_up, rw_k_k, rw_k_a,
                 rw_r_k, rw_ln_w, rw_ln_b, hg_lb, hg_norm_w, w_out):
    z = h @ w_in
    zr = z[..., :RW_COLS]
    zh = z[..., RW_COLS:]
    zr = zr + rw_mu * (_centred_shift(zr) - zr)
    o_rw = _rwkv7_group(zr, rw_w0, rw_w_up, rw_a0, rw_a_up, rw_g_up, rw_k_k, rw_k_a, rw_r_k,
                        rw_ln_w, rw_ln_b)
    o_hg = _hgrn2_group(zh, hg_lb, hg_norm_w)
    return jnp.concatenate([o_rw, o_hg], axis=-1).astype(h.dtype) @ w_out


def _swiglu(x, wg, wu, wd):
    return (jax.nn.silu(x @ wg) * (x @ wu)) @ wd


def _routed_experts(x, eidx, wsel, w_gate, w_up, w_down):
    T = x.shape[0]
    A = T * TOP_K
    flat_e = eidx.reshape(-1)
    order = jnp.argsort(flat_e)
    sorted_e = flat_e[order]
    counts = jnp.bincount(flat_e, length=N_EXPERTS)
    padded = (counts + EXPERT_BLOCK - 1) // EXPERT_BLOCK * EXPERT_BLOCK
    pad_end = jnp.cumsum(padded)
    pad_start = pad_end - padded
    grp_start = jnp.cumsum(counts) - counts
    dest = pad_start[sorted_e] + jnp.arange(A) - grp_start[sorted_e]
    n_blocks = (A + EXPERT_BLOCK - 1) // EXPERT_BLOCK + N_EXPERTS
    P = n_blocks * EXPERT_BLOCK
    row_tok = jnp.zeros((P,), jnp.int32).at[dest].set((order // TOP_K).astype(jnp.int32))
    row_w = jnp.zeros((P,), x.dtype).at[dest].set(wsel.reshape(-1)[order])
    block_e = jnp.minimum(jnp.searchsorted(pad_end, jnp.arange(n_blocks) * EXPERT_BLOCK, side='right'),
                          N_EXPERTS - 1)

    def step(acc, blk):
        b, e = blk
        rows = lax.dynamic_slice_in_dim(row_tok, b * EXPERT_BLOCK, EXPERT_BLOCK)
        wr = lax.dynamic_slice_in_dim(row_w, b * EXPERT_BLOCK, EXPERT_BLOCK)
        yb = _swiglu(x[rows], w_gate[e], w_up[e], w_down[e])
        return acc.at[rows].add(yb * wr[:, None]), None

    acc, _ = lax.scan(step, jnp.zeros_like(x), (jnp.arange(n_blocks), block_e))
    return acc


def _moe(h, w_router, e_bias, w_exp_gate, w_exp_up, w_exp_down, w_sh_gate, w_sh_up, w_sh_down):
    B, S, D = h.shape
    x = h.reshape(-1, D)
    T = x.shape[0]
    scores = jax.nn.sigmoid(x.astype(jnp.float32) @ w_router.astype(jnp.float32))
    biased = scores + e_bias.astype(jnp.float32)
    grp = biased.reshape(T, N_GROUPS, N_EXPERTS // N_GROUPS)
    grp_score = jnp.sum(lax.top_k(grp, 2)[0], axis=-1)
    _, gidx = lax.top_k(grp_score, TOPK_GROUPS)
    gmask = jnp.any(gidx[..., None] == jnp.arange(N_GROUPS), axis=-2)
    emask = jnp.repeat(gmask, N_EXPERTS // N_GROUPS, axis=-1)
    _, eidx = lax.top_k(jnp.where(emask, biased, -jnp.inf), TOP_K)
    wsel = jnp.take_along_axis(scores, eidx, axis=-1)
    wsel = wsel / jnp.sum(wsel, axis=-1, keepdims=True) * ROUTED_SCALE
    routed = _routed_experts(x, eidx, wsel.astype(x.dtype), w_exp_gate, w_exp_up, w_exp_down)
    shared = _swiglu(x, w_sh_gate, w_sh_up, w_sh_down)
    return (routed + shared).reshape(B, S, D)


def _trunk(x, c, params):
    (w_ada, b_ada, norm_pre_mix, norm_post_mix, norm_pre_ffn, norm_post_ffn, w_in, rw_mu,
     rw_w0, rw_w_up, rw_a0, rw_a_up, rw_g_up, rw_k_k, rw_k_a, rw_r_k, rw_ln_w, rw_ln_b,
     hg_lb_gamma, hg_norm_w, w_out, w_router, e_bias, w_exp_gate, w_exp_up, w_exp_down,
     w_sh_gate, w_sh_up, w_sh_down) = params
    lower_bounds = jnp.cumsum(jax.nn.softmax(hg_lb_gamma.astype(jnp.float32), axis=0), axis=0)
    for l in range(DEPTH):
        mod = (jax.nn.silu(c) @ w_ada[l] + b_ada[l])[:, None, :]
        sh1, sc1, gt1, sh2, sc2, gt2 = jnp.split(mod, 6, axis=-1)
        h = _rms(x, norm_pre_mix[l]) * (1 + sc1) + sh1
        m = _token_mixer(h, w_in[l], rw_mu[l], rw_w0[l], rw_w_up[l], rw_a0[l], rw_a_up[l],
                         rw_g_up[l], rw_k_k[l], rw_k_a[l], rw_r_k[l], rw_ln_w[l], rw_ln_b[l],
                         lower_bounds[l], hg_norm_w[l], w_out[l])
        x = x + gt1 * _rms(m, norm_post_mix[l])
        h = _rms(x, norm_pre_ffn[l]) * (1 + sc2) + sh2
        f = _moe(h, w_router[l], e_bias[l], w_exp_gate[l], w_exp_up[l], w_exp_down[l],
                 w_sh_gate[l], w_sh_up[l], w_sh_down[l])
        x = x + gt2 * _rms(f, norm_post_ffn[l])
    return x


def setup_inputs(seed: int = 0) -> dict:
    key = jax.random.key(seed)
    ks = iter(jax.random.split(key, 40))
    L, D = DEPTH, D_MODEL
    f32 = jnp.float32

    def nrm(shape, s):
        return jax.random.normal(next(ks), shape, f32) * s

    def uni(shape, lo, hi):
        return jax.random.uniform(next(ks), shape, f32, minval=lo, maxval=hi)

    return {
        'x_prompt': nrm((BATCH, SEQ, D), 1.0),
        'x_sample': nrm((DEC_BATCH, DEC_SEQ, D), 1.0),
        'c_prompt': nrm((BATCH, D), 1.0),
        'c_sample': nrm((DEC_BATCH, D), 1.0),
        'w_ada': nrm((L, D, 6 * D), 0.5 * D ** -0.5),
        'b_ada': nrm((L, 6 * D), 0.02),
        'norm_pre_mix': 1.0 + nrm((L, D), 0.05),
        'norm_post_mix': 1.0 + nrm((L, D), 0.05),
        'norm_pre_ffn': 1.0 + nrm((L, D), 0.05),
        'norm_post_ffn': 1.0 + nrm((L, D), 0.05),
        'w_in': nrm((L, D, IN_COLS), D ** -0.5),
        'rw_mu': uni((L, RW_COLS), 0.0, 1.0),
        'rw_w0': uni((L, 2, RW_WIDTH), -5.0, 0.0),
        'rw_w_up': nrm((L, 2, W_LORA, RW_WIDTH), 0.1),
        'rw_a0': nrm((L, 2, RW_WIDTH), 0.5),
        'rw_a_up': nrm((L, 2, A_LORA, RW_WIDTH), 0.5 * A_LORA ** -0.5),
        'rw_g_up': nrm((L, G_LORA, RW_WIDTH), G_LORA ** -0.5),
        'rw_k_k': 0.85 + nrm((L, RW_WIDTH), 0.05),
        'rw_k_a': 1.0 + nrm((L, RW_WIDTH), 0.05),
        'rw_r_k': nrm((L, RW_HEADS, RW_HEAD_DIM), 0.1),
        'rw_ln_w': 1.0 + nrm((L, RW_WIDTH), 0.05),
        'rw_ln_b': nrm((L, RW_WIDTH), 0.02),
        'hg_lb_gamma': nrm((L + 1, 2, HG_WIDTH), 0.5),
        'hg_norm_w': 1.0 + nrm((L, HG_WIDTH), 0.05),
        'w_out': nrm((L, D, D), D ** -0.5),
        'w_router': nrm((L, D, N_EXPERTS), D ** -0.5),
        'e_bias': nrm((L, N_EXPERTS), 0.01),
        'w_exp_gate': nrm((L, N_EXPERTS, D, D_EXPERT), D ** -0.5),
        'w_exp_up': nrm((L, N_EXPERTS, D, D_EXPERT), D ** -0.5),
        'w_exp_down': nrm((L, N_EXPERTS, D_EXPERT, D), D_EXPERT ** -0.5),
        'w_sh_gate': nrm((L, D, D_SHARED), D ** -0.5),
        'w_sh_up': nrm((L, D, D_SHARED), D ** -0.5),
        'w_sh_down': nrm((L, D_SHARED, D), D_SHARED ** -0.5),
    }


def reference(x_prompt, x_sample, c_prompt, c_sample, w_ada, b_ada, norm_pre_mix, norm_post_mix,
              norm_pre_ffn, norm_post_ffn, w_in, rw_mu, rw_w0, rw_w_up, rw_a0, rw_a_up, rw_g_up,
              rw_k_k, rw_k_a, rw_r_k, rw_ln_w, rw_ln_b, hg_lb_gamma, hg_norm_w, w_out, w_router,
              e_bias, w_exp_gate, w_exp_up, w_exp_down, w_sh_gate, w_sh_up, w_sh_down):
    params = (w_ada, b_ada, norm_pre_mix, norm_post_mix, norm_pre_ffn, norm_post_ffn, w_in, rw_mu,
              rw_w0, rw_w_up, rw_a0, rw_a_up, rw_g_up, rw_k_k, rw_k_a, rw_r_k, rw_ln_w, rw_ln_b,
              hg_lb_gamma, hg_norm_w, w_out, w_router, e_bias, w_exp_gate, w_exp_up, w_exp_down,
              w_sh_gate, w_sh_up, w_sh_down)
    y_prompt = _trunk(x_prompt, c_prompt, params)
    y_sample = _trunk(x_sample, c_sample, params)
    return (y_prompt, y_sample)
```

```python
import numpy as np
import concourse.bass as bass
import concourse.mybir as mybir

F32 = mybir.dt.float32
BF16 = mybir.dt.bfloat16
I32 = mybir.dt.int32
AF = mybir.ActivationFunctionType
ALU = mybir.AluOpType
AX = mybir.AxisListType

NDMASEM = 8


class Op:
    __slots__ = ("q", "fn", "deps", "signals", "sem", "ticket", "idx", "prewait", "isbar")

    def __init__(self, q, fn):
        self.q = q
        self.fn = fn
        self.deps = []
        self.signals = False
        self.sem = None
        self.ticket = 0
        self.prewait = None
        self.isbar = False


class Prog:
    ENG = {"pe": "pe", "act": "act", "dve": "dve", "pool": "pool",
           "dsp": "sp", "dpool": "pool", "dact": "act", "dpool2": "pool"}
    DMAQ = ("dsp", "dpool", "dact", "dpool2")

    def __init__(self, nc):
        self.nc = nc
        self.ops = []
        self.last_w = {}
        self.readers = {}
        self.sb_cur = 16512
        self.sb_mark = []
        self.uid = 0
        self.limit = None

    def sb(self, shape, dtype, name=None):
        self.uid += 1
        nm = f"{name or 't'}_{self.uid}"
        esz = {F32: 4, BF16: 2, I32: 4, mybir.dt.uint32: 4, mybir.dt.uint16: 2}[dtype]
        per = int(np.prod(shape[1:])) * esz
        per = (per + 31) // 32 * 32
        off = self.sb_cur
        assert off + per <= 229344, f"SBUF overflow allocating {nm}: {off}+{per}"
        self.sb_cur += per
        return self.nc.alloc_sbuf_tensor_at(nm, list(shape), dtype, offset=off)

    def push(self):
        self.sb_mark.append(self.sb_cur)

    def pop(self):
        self.sb_cur = self.sb_mark.pop()

    def add(self, q, fn, reads=(), writes=()):
        if self.limit is not None and len(self.ops) >= self.limit:
            return None
        op = Op(q, fn)
        op.idx = len(self.ops)
        if self.limit is not None:
            import sys as _s
            f = _s._getframe(1)
            ln = []
            while f is not None and len(ln) < 3:
                ln.append(f.f_lineno)
                f = f.f_back
            self.lines = getattr(self, "lines", {})
            self.lines[op.idx] = (q, ln)
        deps = set()
        for k in reads:
            if k in self.last_w:
                deps.add(self.last_w[k])
            if isinstance(k, str) and k.startswith("ps"):
                for r in self.readers.get(k, ()):
                    if self.ops[r].q != q:
                        deps.add(r)
        for k in writes:
            if k in self.last_w:
                deps.add(self.last_w[k])
            for r in self.readers.get(k, ()):
                deps.add(r)
        deps.discard(op.idx)
        if q == "pe":
            deps = {d for d in deps if self.ops[d].q != "pe"}
        op.deps = sorted(deps)
        for k in reads:
            self.readers.setdefault(k, []).append(op.idx)
        for k in writes:
            self.last_w[k] = op.idx
            self.readers[k] = []
        self.ops.append(op)
        return op

    def barrier(self):
        op = Op("bar", None)
        op.idx = len(self.ops)
        op.isbar = True
        self.ops.append(op)
        self.last_w = {k: v for k, v in self.last_w.items() if isinstance(k, str) and k.startswith("persist:")}
        self.readers = {k: v for k, v in self.readers.items() if isinstance(k, str) and k.startswith("persist:")}

    def emit(self):
        nc = self.nc
        ops = self.ops
        queues = ["pe", "act", "dve", "pool", "dsp", "dpool", "dact", "dpool2"]
        hist = {q: [] for q in queues}
        bar_deps = {}
        for op in ops:
            if op.isbar:
                deps = []
                for q in ("pe", "act", "dve", "pool"):
                    deps += hist[q][-1:]
                for q in ("dsp", "dpool", "dact"):
                    deps += hist[q][-NDMASEM:]
                bar_deps[op.idx] = deps
                for d in deps:
                    ops[d].signals = True
            else:
                hist[op.q].append(op.idx)
        for op in ops:
            for d in op.deps:
                ops[d].signals = True
        sem_c = {q: nc.alloc_semaphore(f"s_{q}") for q in ("pe", "act", "dve", "pool")}
        sem_d = {q: [nc.alloc_semaphore(f"s_{q}{i}") for i in range(NDMASEM)]
                 for q in self.DMAQ}
        cnt_c = {q: 0 for q in sem_c}
        cnt_d = {q: [0] * NDMASEM for q in sem_d}
        rr = {q: 0 for q in sem_d}
        for op in ops:
            if op.isbar:
                continue
            if op.q in sem_c:
                if op.signals:
                    cnt_c[op.q] += 1
                    op.sem = sem_c[op.q]
                    op.ticket = cnt_c[op.q]
            else:
                i = rr[op.q]
                rr[op.q] = (i + 1) % NDMASEM
                op.prewait = (sem_d[op.q][i], cnt_d[op.q][i])
                cnt_d[op.q][i] += 16
                op.sem = sem_d[op.q][i]
                op.ticket = cnt_d[op.q][i]
                op.signals = True
        streams = {"pe": [], "act": [], "dve": [], "pool": [], "sp": []}
        for op in ops:
            if op.isbar:
                for e in streams:
                    streams[e].append(op)
            else:
                streams[self.ENG[op.q]].append(op)
        self.n_wait = 0

        def run_stream(eng, lst):
            waited = {}

            def w(sem, val):
                if val <= 0:
                    return
                if waited.get(sem.num, 0) >= val:
                    return
                eng.wait_ge(sem, val)
                self.n_wait += 1
                waited[sem.num] = val

            for op in lst:
                if op.isbar:
                    for d in bar_deps[op.idx]:
                        w(ops[d].sem, ops[d].ticket)
                    continue
                for d in op.deps:
                    w(ops[d].sem, ops[d].ticket)
                if op.prewait is not None:
                    w(*op.prewait)
                ins = op.fn(eng)
                if op.signals:
                    ins.then_inc(op.sem, 16 if op.q in self.DMAQ else 1)
            return waited

        with nc.Block() as block:
            @block.tensor
            def _(e):
                run_stream(e, streams["pe"])

            @block.scalar
            def _(e):
                run_stream(e, streams["act"])

            @block.vector
            def _(e):
                run_stream(e, streams["dve"])

            @block.gpsimd
            def _(e):
                run_stream(e, streams["pool"])

            @block.sync
            def _(e):
                run_stream(e, streams["sp"])
D = 2048
RW = 1024
HG = 1024
RWC = 3456
INC = 8576
NE = 64


def build_program(cfg):
    LP, LS = cfg["LP"], cfg["LS"]
    DE = cfg.get("DE", 512)
    DBG = cfg.get("debug", None)
    T = LP + LS
    NT = T // 128
    nc = bass.Bass("TRN2", target_bir_lowering=False)
    P = Prog(nc)
    P.limit = cfg.get("limit", None)
    global LAST_PROG
    LAST_PROG = P

    def din(name, shape, dt=F32):
        return nc.dram_tensor(name, list(shape), dt, kind="ExternalInput").ap()

    def dout(name, shape, dt=F32):
        return nc.dram_tensor(name, list(shape), dt, kind="ExternalOutput").ap()

    def dscr(name, shape, dt=F32):
        return nc.dram_tensor(name, list(shape), dt, kind="Internal").ap()

    x = din("x", [T, D])
    c = din("c", [2, D])
    w_ada = din("w_ada", [D, 6 * D])
    b_ada = din("b_ada", [1, 6 * D])
    nrm = din("nrm", [4, D])
    w_in = din("w_in", [D, INC])
    y = dout("y", [T, D])

    vec_d = dscr("vec_d", [6, 2, D])
    hT_d = dscr("hT_d", [16, 128, T], BF16)

    ident_f = P.sb([128, 128], F32, "identf")
    ident_b = P.sb([128, 128], BF16, "identb")
    ps = [nc.alloc_psum_tensor(f"ps{i}", [128, 512], F32) for i in range(6)]
    psb = [nc.alloc_psum_tensor(f"psb{i}", [128, 1024], BF16) for i in range(2)]

    def dma(q, out, in_, reads=(), writes=(), **kw):
        return P.add(q, lambda e: e.dma_start(out=out, in_=in_, **kw), reads, writes)

    def act(out, in_, func, reads, writes, bias=None, scale=1.0, accum_out=None):
        kw = {}
        if bias is not None:
            kw["bias"] = bias
        if accum_out is not None:
            kw["accum_out"] = accum_out
        return P.add("act", lambda e: e.activation(out=out, in_=in_, func=func, scale=scale, **kw), reads, writes)

    def tt(out, in0, in1, op, reads, writes, q="dve"):
        return P.add(q, lambda e: e.tensor_tensor(out=out, in0=in0, in1=in1, op=op), reads, writes)

    def ts(out, in0, s1, s2, op0, op1, reads, writes, q="dve", accum_out=None):
        kw = {}
        if accum_out is not None:
            kw["accum_out"] = accum_out
        if op1 is None:
            return P.add(q, lambda e: e.tensor_scalar(out=out, in0=in0, scalar1=s1, scalar2=None, op0=op0, **kw), reads, writes)
        return P.add(q, lambda e: e.tensor_scalar(out=out, in0=in0, scalar1=s1, scalar2=s2, op0=op0, op1=op1, **kw), reads, writes)

    def stt(out, in0, scalar, in1, op0, op1, reads, writes):
        return P.add("dve", lambda e: e.scalar_tensor_tensor(out=out, in0=in0, scalar=scalar, in1=in1, op0=op0, op1=op1), reads, writes)

    def cp(out, in_, reads, writes, q="dve"):
        if q == "act":
            return P.add("act", lambda e: e.copy(out=out, in_=in_), reads, writes)
        return P.add(q, lambda e: e.tensor_copy(out=out, in_=in_), reads, writes)

    def mm(out, lhsT, rhs, start, stop, reads, writes):
        return P.add("pe", lambda e: e.matmul(out, lhsT, rhs, start=start, stop=stop), reads, writes)

    def tr(out, in_, ident, reads, writes):
        return P.add("pe", lambda e: e.transpose(out, in_, ident), reads, writes)

    def memset(ap, val, writes, q="pool"):
        return P.add(q, lambda e: e.memset(ap, val), (), writes)

    memset(ident_f[:], 0.0, ["identf"])
    P.add("pool", lambda e: e.affine_select(out=ident_f[:], in_=ident_f[:], pattern=[[-1, 128]],
                                            compare_op=ALU.not_equal, fill=1.0, base=0, channel_multiplier=1),
          ["identf"], ["identf"])
    cp(ident_b[:], ident_f[:], ["identf"], ["identb"])

    P.push()
    cT = P.sb([128, 16, 2], F32, "cT")
    scT = P.sb([128, 16, 2], F32, "scT")
    mod = P.sb([2, 6 * D], F32, "mod")
    bb = [P.sb([2, 512], F32, f"bb{i}") for i in range(2)]
    nr = P.sb([2, 4, D], F32, "nr")
    wa = [P.sb([128, 16, 512], F32, f"wa{i}") for i in range(2)]
    for s_ in range(2):
        dma("dsp", cT[:, :, s_], c[s_, :].rearrange("(k p) -> p k", p=128), (), ["cT"], allow_slow_non_contiguous=True)
    dma("dsp", nr[:], nrm.rearrange("(o f) d -> o f d", o=1).partition_broadcast(2), (), ["nr"])
    act(scT[:], cT[:], AF.Silu, ["cT"], ["scT"])
    for cg in range(24):
        b = cg % 2
        dma("dsp", wa[b][:], w_ada[:, cg * 512:(cg + 1) * 512].rearrange("(k p) n -> p k n", p=128), (), [f"wa{b}"])
        dma("dsp", bb[b][:], b_ada[0:1, cg * 512:(cg + 1) * 512].partition_broadcast(2), (), [f"bb{b}"])
        for k in range(16):
            mm(ps[0][0:2, :], scT[:, k, :], wa[b][:, k, :], k == 0, k == 15, ["scT", f"wa{b}"], ["ps0"])
        tt(mod[:, cg * 512:(cg + 1) * 512], ps[0][0:2, :], bb[b][:], ALU.add,
           ["ps0", f"bb{b}"], ["mod"])
    vecs = P.sb([2, 6, D], F32, "vecs")
    stt(vecs[:, 0, :], mod[:, D:2 * D], 1.0, nr[:, 0, :], ALU.add, ALU.mult, ["mod", "nr"], ["vecs"])
    cp(vecs[:, 1, :], mod[:, 0:D], ["mod"], ["vecs"])
    tt(vecs[:, 2, :], mod[:, 2 * D:3 * D], nr[:, 1, :], ALU.mult, ["mod", "nr"], ["vecs"])
    stt(vecs[:, 3, :], mod[:, 4 * D:5 * D], 1.0, nr[:, 2, :], ALU.add, ALU.mult, ["mod", "nr"], ["vecs"])
    cp(vecs[:, 4, :], mod[:, 3 * D:4 * D], ["mod"], ["vecs"])
    tt(vecs[:, 5, :], mod[:, 5 * D:6 * D], nr[:, 3, :], ALU.mult, ["mod", "nr"], ["vecs"])
    dma("dsp", vec_d.rearrange("f s d -> s f d"), vecs[:], ["vecs"], ["vec_d"])
    P.barrier()
    P.pop()

    def seq_of_tile(t):
        return 0 if t * 128 < LP else 1

    P.push()
    g1 = [P.sb([128, D], F32, f"g1_{s}") for s in range(2)]
    s1 = [P.sb([128, D], F32, f"s1_{s}") for s in range(2)]
    for s in range(2):
        dma("dsp", g1[s][:], vec_d[0, s:s + 1, :].partition_broadcast(128), (), [f"g1_{s}"])
        dma("dsp", s1[s][:], vec_d[1, s:s + 1, :].partition_broadcast(128), (), [f"s1_{s}"])
    xt = [P.sb([128, D], F32, f"xt{i}") for i in range(2)]
    junk = P.sb([128, D], F32, "junk")
    hb = [P.sb([128, D], BF16, f"hb{i}") for i in range(2)]
    hTs = [P.sb([128, 16, 128], BF16, f"hTs{i}") for i in range(2)]
    ss = [P.sb([128, 1], F32, f"ss{i}") for i in range(2)]
    rs = [P.sb([128, 1], F32, f"rs{i}") for i in range(2)]
    for t in range(NT):
        b = t % 2
        s = seq_of_tile(t)
        dma("dsp", xt[b][:], x[t * 128:(t + 1) * 128, :], (), [f"xt{b}"])
        act(junk[:], xt[b][:], AF.Square, [f"xt{b}"], ["junk", f"ss{b}"], accum_out=ss[b][:])
        ts(rs[b][:], ss[b][:], 1.0 / D, 1e-6, ALU.mult, ALU.add, [f"ss{b}"], [f"rs{b}"])
        act(rs[b][:], rs[b][:], AF.Sqrt, [f"rs{b}"], [f"rs{b}"])
        P.add("dve", lambda e, b=b: e.reciprocal(out=rs[b][:], in_=rs[b][:]), [f"rs{b}"], [f"rs{b}"])
        stt(xt[b][:], xt[b][:], rs[b][:], g1[s][:], ALU.mult, ALU.mult, [f"xt{b}", f"rs{b}", f"g1_{s}"], [f"xt{b}"])
        tt(hb[b][:], xt[b][:], s1[s][:], ALU.add, [f"xt{b}", f"s1_{s}"], [f"hb{b}"])
        for k in range(16):
            pb = k % 2
            tr(psb[pb][:, 0:128], hb[b][:, k * 128:(k + 1) * 128], ident_b[:], [f"hb{b}", "identb"], [f"psb{pb}"])
            cp(hTs[b][:, k, :], psb[pb][:, 0:128], [f"psb{pb}"], [f"hTs{b}"], q=("act" if k % 2 else "dve"))
        dma("dsp", hT_d[:, :, t * 128:(t + 1) * 128].rearrange("k p t -> p k t"), hTs[b][:], [f"hTs{b}"], ["hT_d"])
    P.barrier()
    P.pop()

    if DBG == "p1":
        dbg = dout("dbg", [16, 128, T], BF16)
        dma("dsp", dbg, hT_d, (), ["dbg"])
        dbg2 = dout("dbg2", [6, 2, D])
        dma("dsp", dbg2, vec_d, (), ["dbg2"])
        P.barrier()
        P.emit()
        return nc

    TB = 512 if (LP % 512 == 0 and LS % 512 == 0) else 128
    NF = 6528
    win_b = dscr("win_b", [D, INC], BF16)
    zT_d = dscr("zT_d", [NF, T])
    ztok_d = dscr("ztok_d", [T, 2048])
    for k in range(16):
        dma("dpool", win_b[k * 128:(k + 1) * 128, :], w_in[k * 128:(k + 1) * 128, :], (), ["win_b"])
    P.barrier()
    DS = DE
    w_exp_gate = din("w_exp_gate", [NE, D, DE])
    w_exp_up = din("w_exp_up", [NE, D, DE])
    w_exp_down = din("w_exp_down", [NE, DE, D])
    w_sh_gate = din("w_sh_gate", [D, DS])
    w_sh_up = din("w_sh_up", [D, DS])
    w_sh_down = din("w_sh_down", [DS, D])
    wg_b = dscr("wg_b", [NE + 1, 128, 16 * DE], BF16)
    wu_b = dscr("wu_b", [NE + 1, 128, 16 * DE], BF16)
    wd_b = dscr("wd_b", [NE + 1, 128, (DE // 128) * D], BF16)
    for e_ in ([] if DBG in ("p2", "p3", "p4") else [NE] + list(range(NE))):
        sg_ = w_sh_gate if e_ == NE else w_exp_gate[e_]
        su_ = w_sh_up if e_ == NE else w_exp_up[e_]
        sd_ = w_sh_down if e_ == NE else w_exp_down[e_]
        dma("dpool2", wg_b[e_].rearrange("p (k n) -> p k n", k=16), sg_.rearrange("(k p) n -> p k n", p=128), (), ["persist:wg%d" % e_])
        dma("dpool2", wu_b[e_].rearrange("p (k n) -> p k n", k=16), su_.rearrange("(k p) n -> p k n", p=128), (), ["persist:wu%d" % e_])
        dma("dpool2", wd_b[e_].rearrange("p (k n) -> p k n", k=DE // 128), sd_.rearrange("(k p) n -> p k n", p=128), (), ["persist:wd%d" % e_])
    P.push()
    hTb = [P.sb([128, 16, TB], BF16, f"hTb{i}") for i in range(2)]
    wt = [P.sb([128, 16, 512], BF16, f"wt{i}") for i in range(2)]
    zs = [P.sb([128, 512], F32, f"zs{i}") for i in range(3)]
    groups = [(i * 512, 512, "F") for i in range(12)] + [(6144, 384, "F")] + [(6528 + i * 512, 512, "T") for i in range(4)]
    it = 0
    zi = 0
    for tb in range(T // TB):
        hb_ = tb % 2
        dma("dsp", hTb[hb_][:], hT_d[:, :, tb * TB:(tb + 1) * TB].rearrange("k p t -> p k t"), (), [f"hTb{hb_}"])
        for (c0, ncol, mode) in groups:
            wb = it % 2
            it += 1
            dma("dsp", wt[wb][:, :, 0:ncol], win_b[:, c0:c0 + ncol].rearrange("(k p) n -> p k n", p=128), (), [f"wt{wb}"])
            if mode == "F":
                for ci in range(ncol // 128):
                    pi = zi % 4
                    z_ = zi % 3
                    zi += 1
                    for k in range(16):
                        mm(ps[pi][:, 0:TB], wt[wb][:, k, ci * 128:(ci + 1) * 128], hTb[hb_][:, k, :], k == 0, k == 15,
                           [f"wt{wb}", f"hTb{hb_}"], [f"ps{pi}"])
                    cp(zs[z_][:, 0:TB], ps[pi][:, 0:TB], [f"ps{pi}"], [f"zs{z_}"], q=("act" if zi % 2 else "dve"))
                    dma("dsp", zT_d[c0 + ci * 128:c0 + (ci + 1) * 128, tb * TB:(tb + 1) * TB], zs[z_][:, 0:TB], [f"zs{z_}"], ["zT_d"])
            else:
                for ti in range(TB // 128):
                    pi = zi % 4
                    z_ = zi % 3
                    zi += 1
                    for k in range(16):
                        mm(ps[pi][:, :], hTb[hb_][:, k, ti * 128:(ti + 1) * 128], wt[wb][:, k, :], k == 0, k == 15,
                           [f"wt{wb}", f"hTb{hb_}"], [f"ps{pi}"])
                    cp(zs[z_][:, :], ps[pi][:, :], [f"ps{pi}"], [f"zs{z_}"], q=("act" if zi % 2 else "dve"))
                    dma("dsp", ztok_d[tb * TB + ti * 128:tb * TB + (ti + 1) * 128, c0 - 6528:c0 - 6528 + 512], zs[z_][:, :], [f"zs{z_}"], ["ztok_d"])
    P.barrier()
    P.pop()

    if DBG == "p2":
        dbg = dout("dbg", [NF, T])
        dma("dsp", dbg, zT_d, (), ["dbg"])
        dbg2 = dout("dbg2", [T, 2048])
        dma("dsp", dbg2, ztok_d, (), ["dbg2"])
        P.barrier()
        P.emit()
        return nc

    if DBG is not None:
        print("ops before P3:", len(P.ops))
    rw_mu = din("rw_mu", [RWC])
    rw_w0 = din("rw_w0", [2, RW])
    rw_a0 = din("rw_a0", [2, RW])
    rw_w_up = din("rw_w_up", [2, 64, RW])
    rw_a_up = din("rw_a_up", [2, 64, RW])
    rw_g_up = din("rw_g_up", [128, RW])
    rw_vecs = din("rw_vecs", [5, RW])
    m_d = dscr("m_d", [T, 2048])
    of_d = dscr("of_d", [T, RW])
    P.push()
    CH = 128
    NCB = TB // CH
    muT = P.sb([64, 54], F32, "muT")
    ommT = P.sb([64, 54], F32, "ommT")
    hmuT = P.sb([64, 54], F32, "hmuT")
    dma("dsp", muT[:], rw_mu.rearrange("(j p) -> p j", p=64), (), ["muT"], allow_slow_non_contiguous=True)
    ts(ommT[:], muT[:], -1.0, 1.0, ALU.mult, ALU.add, ["muT"], ["ommT"])
    ts(hmuT[:], muT[:], 0.5, None, ALU.mult, None, ["muT"], ["hmuT"])
    mug = P.sb([128, 3], F32, "mug")
    dma("dsp", mug[:, 0:1], rw_mu[3328:3456].rearrange("(p o) -> p o", o=1), (), ["mug"])
    ts(mug[:, 1:2], mug[:, 0:1], -1.0, 1.0, ALU.mult, ALU.add, ["mug"], ["mug"])
    ts(mug[:, 2:3], mug[:, 0:1], 0.5, None, ALU.mult, None, ["mug"], ["mug"])
    w0T = P.sb([64, 2, 16], F32, "w0T")
    a0T = P.sb([64, 2, 16], F32, "a0T")
    for d_ in range(2):
        dma("dsp", w0T[:, d_, :], rw_w0[d_, :].rearrange("(h p) -> p h", p=64), (), ["w0T"], allow_slow_non_contiguous=True)
        dma("dsp", a0T[:, d_, :], rw_a0[d_, :].rearrange("(h p) -> p h", p=64), (), ["a0T"], allow_slow_non_contiguous=True)
    vT = P.sb([64, 5, 16], F32, "vT")
    for i_ in range(3):
        dma("dsp", vT[:, i_, :], rw_vecs[i_, :].rearrange("(h p) -> p h", p=64), (), ["vT"], allow_slow_non_contiguous=True)
    omka = P.sb([64, 16], F32, "omka")
    ts(omka[:], vT[:, 1, :], -1.0, 1.0, ALU.mult, ALU.add, ["vT"], ["omka"])
    lnw_b = P.sb([128, RW], F32, "lnw_b")
    lnb_b = P.sb([128, RW], F32, "lnb_b")
    rk_b = P.sb([128, RW], F32, "rk_b")
    dma("dsp", rk_b[:], rw_vecs[2:3, :].partition_broadcast(128), (), ["rk_b"])
    dma("dsp", lnw_b[:], rw_vecs[3:4, :].partition_broadcast(128), (), ["lnw_b"])
    dma("dsp", lnb_b[:], rw_vecs[4:5, :].partition_broadcast(128), (), ["lnb_b"])
    wup = P.sb([64, 2, RW], BF16, "wup")
    aup = P.sb([64, 2, RW], BF16, "aup")
    gup = P.sb([128, RW], BF16, "gup")
    for d_ in range(2):
        dma("dpool", wup[:, d_, :], rw_w_up[d_], (), ["wup"])
        dma("dpool", aup[:, d_, :], rw_a_up[d_], (), ["aup"])
    dma("dpool", gup[:], rw_g_up, (), ["gup"])
    ones_f = P.sb([128, 128], F32, "ones_f")
    memset(ones_f[:], 1.0, ["ones_f"])
    MASK2 = [P.sb([128, 256], F32, f"mask2_{d_}") for d_ in range(2)]
    MASKX = [P.sb([128, 128], F32, f"maskx_{d_}") for d_ in range(2)]
    for d_ in range(2):
        memset(MASK2[d_][:], 1.0, [f"mask2_{d_}"])
        memset(MASKX[d_][:], 1.0, [f"maskx_{d_}"])
        sgn = 1 if d_ == 0 else -1
        P.add("pool", lambda e, d_=d_, sgn=sgn: e.affine_select(out=MASK2[d_][:, 0:128], in_=MASK2[d_][:, 0:128], pattern=[[sgn, 128]],
              compare_op=ALU.is_gt, fill=0.0, base=0, channel_multiplier=-sgn), [f"mask2_{d_}"], [f"mask2_{d_}"])
        P.add("pool", lambda e, d_=d_, sgn=sgn: e.affine_select(out=MASK2[d_][:, 128:256], in_=MASK2[d_][:, 128:256], pattern=[[sgn, 128]],
              compare_op=ALU.is_ge, fill=0.0, base=0, channel_multiplier=-sgn), [f"mask2_{d_}"], [f"mask2_{d_}"])
        P.add("pool", lambda e, d_=d_, sgn=sgn: e.affine_select(out=MASKX[d_][:], in_=MASKX[d_][:], pattern=[[-sgn, 128]],
              compare_op=ALU.is_gt, fill=0.0, base=0, channel_multiplier=sgn), [f"maskx_{d_}"], [f"maskx_{d_}"])
    ST = P.sb([64, 16, 64], F32, "ST")
    STb = P.sb([64, 16, 64], F32 if cfg.get("rw_fp32", True) else BF16, "STb")
    zh = [P.sb([64, TB + 2], F32, f"zh{i}") for i in range(2)]
    sft = P.sb([64, TB], F32, "sft")
    lat = P.sb([64, 5, TB], BF16, "lat")
    sgl = P.sb([128, TB], BF16, "sgl")
    zg = P.sb([128, TB + 2], F32, "zg")
    sftg = P.sb([128, TB], F32, "sftg")
    rr_ = P.sb([64, TB], F32, "rr")
    kk_ = P.sb([64, TB], F32, "kk")
    vv_ = P.sb([64, TB], F32, "vv")
    kn = P.sb([64, TB], F32, "kn")
    t1 = P.sb([64, TB], F32, "t1")
    t2 = P.sb([64, TB], F32, "t2")
    lw = P.sb([64, TB], F32, "lw")
    aa = P.sb([64, TB], F32, "aa")
    kd = P.sb([64, TB], F32, "kd")
    bb_ = P.sb([64, TB], F32, "bbk")
    cl = P.sb([64, TB], F32, "cl")
    ex = P.sb([64, TB], F32, "ex")
    CD = F32 if cfg.get("rw_fp32", True) else BF16
    ident_c = ident_f if CD == F32 else ident_b
    identc_k = "identf" if CD == F32 else "identb"
    ART = P.sb([64, NCB, 2, CH], CD, "ART")
    BT = P.sb([64, TB], CD, "BT")
    KT = P.sb([64, TB], CD, "KT")
    BhT = P.sb([64, TB], CD, "BhT")
    KhT = P.sb([64, TB], CD, "KhT")
    AfT = P.sb([64, TB], CD, "AfT")
    RfT = P.sb([64, TB], CD, "RfT")
    VT = P.sb([64, TB], CD, "VT")
    rkv32 = P.sb([64, 2, TB], F32, "rkv32")
    pc = P.sb([64, NCB], F32, "pc")
    clc = P.sb([64, NCB], F32, "clc")
    tok = P.sb([128, 5, 64], CD, "tok")
    NN = [P.sb([128, 256], CD, f"NN{i}") for i in range(2)]
    NX = [P.sb([128, 256], CD, f"NX{i}") for i in range(2)]
    Wt = [P.sb([128, 128], CD, f"Wt{i}") for i in range(2)]
    RhT = P.sb([64, 128], CD, "RhT")
    GB = P.sb([64, 64], CD, "GB")
    o_tok = P.sb([128, NCB, RW], F32, "o_tok")
    g_tok = P.sb([128, NCB, RW], F32, "g_tok")
    kb_tok = P.sb([128, NCB, RW], F32, "kb_tok")
    v_tok32 = P.sb([128, NCB, RW], F32, "v_tok32")
    of_t = P.sb([128, RW], F32, "of_t")
    st8 = P.sb([128, 16], F32, "st8")
    st9 = P.sb([128, 16], F32, "st9")
    onr = P.sb([128, RW], F32, "onr")

    seq_bounds = [(0, LP), (LP, T)]

    def load_shift(dst_sft, ztile, row0, nrow, t0, s0, s1, colj, key_z, key_o, omm_ap, hmu_ap):
        lo = t0 - 1
        hi = t0 + TB + 1
        a = max(lo, s0)
        b = min(hi, s1)
        if lo < s0:
            memset(ztile[0:nrow, 0:1], 0.0, [key_z])
        if hi > s1:
            memset(ztile[0:nrow, TB + 1:TB + 2], 0.0, [key_z])
        dma("dsp", ztile[0:nrow, a - lo:b - lo], zT_d[row0:row0 + nrow, a:b], (), [key_z])
        tt(dst_sft, ztile[0:nrow, 0:TB], ztile[0:nrow, 2:TB + 2], ALU.add, [key_z], [key_o])
        ts(dst_sft, dst_sft, hmu_ap, None, ALU.mult, None, [key_o], [key_o])
        stt(dst_sft, ztile[0:nrow, 1:TB + 1], omm_ap, dst_sft, ALU.mult, ALU.add, [key_z, key_o], [key_o])

    zi = 0
    for d_ in range(2):
        for (s0, s1) in seq_bounds:
            nblk = (s1 - s0) // TB
            memset(ST[:], 0.0, ["ST"])
            memset(STb[:], 0.0, ["STb"])
            blks = range(nblk) if d_ == 0 else range(nblk - 1, -1, -1)
            for bi in blks:
                t0 = s0 + bi * TB
                for j_ in range(4):
                    zb = zi % 2
                    zi += 1
                    load_shift(sft[:], zh[zb], 3072 + j_ * 64, 64, t0, s0, s1, 48 + j_, f"zh{zb}", "sft",
                               ommT[:, 48 + j_:49 + j_], hmuT[:, 48 + j_:49 + j_])
                    if j_ < 2:
                        act(lat[:, j_, :], sft[:], AF.Tanh, ["sft"], ["lat"])
                    else:
                        cp(lat[:, j_, :], sft[:], ["sft"], ["lat"])
                load_shift(sftg[:], zg, 3328, 128, t0, s0, s1, 0, "zg", "sftg", mug[:, 1:2], mug[:, 2:3])
                act(sgl[:], sftg[:], AF.Sigmoid, ["sftg"], ["sgl"])
                for ci in range(NCB):
                    for half in range(2):
                        pi = 4 + half
                        mm(ps[pi][:, :], sgl[:, ci * CH:(ci + 1) * CH], gup[:, half * 512:(half + 1) * 512], True, True,
                           ["sgl", "gup"], [f"ps{pi}"])
                        cp(g_tok[:, ci, half * 512:(half + 1) * 512], ps[pi][:, :], [f"ps{pi}"], ["g_tok"], q="act")
                if d_ == 1:
                    for ci in range(NCB):
                        memset(kb_tok[:, ci, :], 0.0, ["kb_tok"])
                for h in range(16):
                    for (dst, base, colj) in ((rr_, 0, h), (kk_, 1024, 16 + h), (vv_, 2048, 32 + h)):
                        zb = zi % 2
                        zi += 1
                        load_shift(dst[:], zh[zb], base + h * 64, 64, t0, s0, s1, colj, f"zh{zb}", dst.name,
                                   ommT[:, colj:colj + 1], hmuT[:, colj:colj + 1])
                    ts(kn[:], kk_[:], vT[:, 0, h:h + 1], None, ALU.mult, None, [kk_.name, "vT"], ["kn"])
                    tt(t1[:], kn[:], kn[:], ALU.mult, ["kn"], ["t1"])
                    mm(ps[0][0:64, 0:TB], ones_f[0:64, 0:64], t1[:], True, True, ["ones_f", "t1"], ["ps0"])
                    act(t2[:], ps[0][0:64, 0:TB], AF.Sqrt, ["ps0"], ["t2"])
                    ts(t2[:], t2[:], 1e-12, None, ALU.max, None, ["t2"], ["t2"])
                    P.add("dve", lambda e: e.reciprocal(out=t2[:], in_=t2[:]), ["t2"], ["t2"])
                    tt(kn[:], kn[:], t2[:], ALU.mult, ["kn", "t2"], ["kn"])
                    mm(ps[1][0:64, 0:TB], wup[:, d_, h * 64:(h + 1) * 64], lat[:, d_, :], True, True, ["wup", "lat"], ["ps1"])
                    act(lw[:], ps[1][0:64, 0:TB], AF.Sigmoid, ["ps1", "w0T"], ["lw"], bias=w0T[:, d_, h:h + 1])
                    ts(lw[:], lw[:], -0.6065306597126334, None, ALU.mult, None, ["lw"], ["lw"])
                    mm(ps[2][0:64, 0:TB], aup[:, d_, h * 64:(h + 1) * 64], lat[:, 2 + d_, :], True, True, ["aup", "lat"], ["ps2"])
                    act(aa[:], ps[2][0:64, 0:TB], AF.Sigmoid, ["ps2", "a0T"], ["aa"], bias=a0T[:, d_, h:h + 1])
                    ts(t1[:], aa[:], vT[:, 1, h:h + 1], omka[:, h:h + 1], ALU.mult, ALU.add, ["aa", "vT", "omka"], ["t1"])
                    tt(kd[:], kk_[:], t1[:], ALU.mult, [kk_.name, "t1"], ["kd"])
                    tt(bb_[:], kn[:], aa[:], ALU.mult, ["kn", "aa"], ["bbk"])
                    stt(rkv32[:, 1, :], rr_[:], 0.5, kd[:], ALU.mult, ALU.mult, [rr_.name, "kd"], ["rkv32"])
                    for ci in range(NCB):
                        sl = slice(ci * CH, (ci + 1) * CH)
                        P.add("dve", lambda e, sl=sl: e.tensor_tensor_scan(out=cl[:, sl], data0=ones_f[0:64, 0:CH], data1=lw[:, sl],
                              initial=0.0, op0=ALU.mult, op1=ALU.add), ["ones_f", "lw"], ["cl"])
                        if d_ == 1:
                            cp(clc[:, ci:ci + 1], cl[:, ci * CH + CH - 1:ci * CH + CH], ["cl"], ["clc"])
                            tt(cl[:, sl], lw[:, sl], cl[:, sl], ALU.subtract, ["lw", "cl"], ["cl"])
                            ts(cl[:, sl], cl[:, sl], clc[:, ci:ci + 1], None, ALU.add, None, ["cl", "clc"], ["cl"])
                        else:
                            cp(clc[:, ci:ci + 1], cl[:, ci * CH + CH - 1:ci * CH + CH], ["cl"], ["clc"])
                    act(pc[:], clc[:], AF.Exp, ["clc"], ["pc"])
                    act(ex[:], cl[:], AF.Exp, ["cl"], ["ex"])
                    tt(t1[:], rr_[:], ex[:], ALU.mult, [rr_.name, "ex"], ["t1"])
                    for ci in range(NCB):
                        cp(ART[:, ci, 1, :], t1[:, ci * CH:(ci + 1) * CH], ["t1"], ["ART"], q="act")
                    cp(RfT[:], t1[:], ["t1"], ["RfT"], q="act")
                    tt(t2[:], cl[:], lw[:], ALU.subtract, ["cl", "lw"], ["t2"])
                    act(ex[:], t2[:], AF.Exp, ["t2"], ["ex"])
                    stt(t1[:], kn[:], -1.0, ex[:], ALU.mult, ALU.mult, ["kn", "ex"], ["t1"])
                    for ci in range(NCB):
                        cp(ART[:, ci, 0, :], t1[:, ci * CH:(ci + 1) * CH], ["t1"], ["ART"], q="act")
                    cp(AfT[:], t1[:], ["t1"], ["AfT"], q="act")
                    act(ex[:], cl[:], AF.Exp, ["cl"], ["ex"], scale=-1.0)
                    tt(BT[:], bb_[:], ex[:], ALU.mult, ["bbk", "ex"], ["BT"])
                    tt(KT[:], kd[:], ex[:], ALU.mult, ["kd", "ex"], ["KT"])
                    for ci in range(NCB):
                        sl = slice(ci * CH, (ci + 1) * CH)
                        act(ex[:, sl], cl[:, sl], AF.Exp, ["cl", "clc"], ["ex"], scale=-1.0, bias=clc[:, ci:ci + 1])
                    tt(BhT[:], bb_[:], ex[:], ALU.mult, ["bbk", "ex"], ["BhT"])
                    tt(KhT[:], kd[:], ex[:], ALU.mult, ["kd", "ex"], ["KhT"])
                    cp(VT[:], vv_[:], [vv_.name], ["VT"], q="act")
                    cis = range(NCB) if d_ == 0 else range(NCB - 1, -1, -1)
                    for ci in cis:
                        sl = slice(ci * CH, (ci + 1) * CH)
                        for i_, (src, sk) in enumerate(((AfT, "AfT"), (BhT, "BhT"), (KhT, "KhT"), (RfT, "RfT"), (VT, "VT"))):
                            tr((ps[3] if CD == F32 else psb[0])[:, 192 + i_ * 64:192 + (i_ + 1) * 64], src[:, sl], ident_c[0:64, 0:64], [sk, identc_k], ["ps3"])
                        cp(tok[:].rearrange("p a b -> p (a b)"), (ps[3] if CD == F32 else psb[0])[:, 192:512], ["ps3"], ["tok"])
                        tr(ps[3][:, 0:64], rkv32[:, 1, sl], ident_f[0:64, 0:64], ["rkv32", "identf"], ["ps3"])
                        tr(ps[3][:, 64:128], rr_[:, sl], ident_f[0:64, 0:64], [rr_.name, "identf"], ["ps3"])
                        tr(ps[3][:, 128:192], vv_[:, sl], ident_f[0:64, 0:64], [vv_.name, "identf"], ["ps3"])
                        tt(kb_tok[:, ci, h * 64:(h + 1) * 64], ps[3][:, 0:64], rk_b[:, h * 64:(h + 1) * 64], ALU.mult, ["ps3", "rk_b"], ["kb_tok"]) if d_ == 0 else \
                            stt(kb_tok[:, ci, h * 64:(h + 1) * 64], ps[3][:, 0:64], 1.0, rk_b[:, h * 64:(h + 1) * 64], ALU.mult, ALU.mult, ["ps3", "rk_b"], ["kb_tok"])
                        cp(v_tok32[:, ci, h * 64:(h + 1) * 64], ps[3][:, 128:192], ["ps3"], ["v_tok32"], q="act")
                        mm(ps[0][:, 0:256], BT[:, sl], ART[:, ci, :, :].rearrange("p a b -> p (a b)"), True, True, ["BT", "ART"], ["ps0"])
                        tt(NN[0][:], ps[0][:, 0:256], MASK2[d_][:], ALU.mult, ["ps0", f"mask2_{d_}"], ["NN0"])
                        mm(ps[1][:, 0:256], KT[:, sl], ART[:, ci, :, :].rearrange("p a b -> p (a b)"), True, True, ["KT", "ART"], ["ps1"])
                        tt(NN[1][:], ps[1][:, 0:256], MASK2[d_][:], ALU.mult, ["ps1", f"mask2_{d_}"], ["NN1"])
                        mm(ps[2][:, 0:128], ART[:, ci, 0, :], BT[:, sl], True, True, ["ART", "BT"], ["ps2"])
                        cur = 0
                        cp(NX[cur][:, 0:128], NN[0][:, 0:128], ["NN0"], [f"NX{cur}"], q="act")
                        tt(NX[cur][:, 128:256], ps[2][:, 0:128], MASKX[d_][:], ALU.mult, ["ps2", f"maskx_{d_}"], [f"NX{cur}"])
                        mm(ps[2][:, 128:192], NN[1][:, 0:128], tok[:, 4, :], True, True, ["NN1", "tok"], ["ps2"])
                        cp(Wt[0][:, 0:64], tok[:, 0, :], ["tok"], ["Wt0"], q="act")
                        cp(Wt[0][:, 64:128], ps[2][:, 128:192], ["ps2"], ["Wt0"])
                        wc = 0
                        for j_ in range(7):
                            mm(ps[4][:, 0:128], ident_c[:], Wt[wc][:], True, False, [identc_k, f"Wt{wc}"], ["ps4"])
                            mm(ps[4][:, 0:128], NX[cur][:, 0:128], Wt[wc][:], False, True, [f"NX{cur}", f"Wt{wc}"], ["ps4"])
                            cp(Wt[1 - wc][:], ps[4][:, 0:128], ["ps4"], [f"Wt{1 - wc}"], q="act")
                            wc = 1 - wc
                            if j_ < 6:
                                mm(ps[5][:, 0:128], NX[cur][:, 128:256], NX[cur][:, 0:128], True, True, [f"NX{cur}"], ["ps5"])
                                mm(ps[5][:, 128:256], NX[cur][:, 0:128], NX[cur][:, 128:256], True, True, [f"NX{cur}"], ["ps5"])
                                cp(NX[1 - cur][:], ps[5][:, 0:256], ["ps5"], [f"NX{1 - cur}"])
                                cur = 1 - cur
                        W = Wt[wc]
                        wk = f"Wt{wc}"
                        mm(ps[0][0:64, 0:128], tok[:, 3, :], ident_c[:], True, False, ["tok", identc_k], ["ps0"])
                        mm(ps[0][0:64, 0:128], W[:, 0:64], NN[0][:, 128:256], False, True, [wk, "NN0"], ["ps0"])
                        cp(RhT[:], ps[0][0:64, 0:128], ["ps0"], ["RhT"], q="act")
                        mm(ps[1][:, 0:64], RhT[:], STb[:, h, :], True, False, ["RhT", "STb"], ["ps1"])
                        mm(ps[1][:, 0:64], NN[0][:, 128:256], W[:, 64:128], False, False, ["NN0", wk], ["ps1"])
                        mm(ps[1][:, 0:64], NN[1][:, 128:256], tok[:, 4, :], False, True, ["NN1", "tok"], ["ps1"])
                        cp(o_tok[:, ci, h * 64:(h + 1) * 64], ps[1][:, 0:64], ["ps1"], ["o_tok"])
                        mm(ps[2][0:64, 0:64], W[:, 0:64], tok[:, 1, :], True, True, [wk, "tok"], ["ps2"])
                        cp(GB[:], ps[2][0:64, 0:64], ["ps2"], ["GB"], q="act")
                        mm(ps[4][0:64, 0:64], GB[:], STb[:, h, :], True, False, ["GB", "STb"], ["ps4"])
                        mm(ps[4][0:64, 0:64], tok[:, 1, :], W[:, 64:128], False, False, ["tok", wk], ["ps4"])
                        mm(ps[4][0:64, 0:64], tok[:, 2, :], tok[:, 4, :], False, True, ["tok"], ["ps4"])
                        stt(ST[:, h, :], ST[:, h, :], pc[:, ci:ci + 1], ps[4][0:64, 0:64], ALU.mult, ALU.add, ["ST", "pc", "ps4"], ["ST"])
                        cp(STb[:, h, :], ST[:, h, :], ["ST"], ["STb"], q="act")
                for ci in range(NCB):
                    r0 = t0 + ci * CH
                    if d_ == 0:
                        dma("dsp", of_d[r0:r0 + CH, :], o_tok[:, ci, :], ["o_tok"], ["of_d"])
                        dma("dsp", m_d[r0:r0 + CH, 0:RW], kb_tok[:, ci, :], ["kb_tok"], ["m_d"])
                    else:
                        dma("dsp", of_t[:], of_d[r0:r0 + CH, :], ["of_d"], ["of_t"])
                        tt(o_tok[:, ci, :], o_tok[:, ci, :], of_t[:], ALU.add, ["o_tok", "of_t"], ["o_tok"])
                        dma("dsp", of_t[:], m_d[r0:r0 + CH, 0:RW], ["m_d"], ["of_t"])
                        tt(kb_tok[:, ci, :], kb_tok[:, ci, :], of_t[:], ALU.add, ["kb_tok", "of_t"], ["kb_tok"])
                        o3 = o_tok[:, ci, :].rearrange("p (h v) -> p h v", v=64)
                        P.add("dve", lambda e, o3=o3: e.tensor_reduce(out=st8[:], in_=o3, axis=AX.X, op=ALU.add), ["o_tok"], ["st8"])
                        ts(st8[:], st8[:], 1.0 / 64, None, ALU.mult, None, ["st8"], ["st8"])
                        tt(o3, o3, st8[:].unsqueeze(2).to_broadcast([128, 16, 64]), ALU.subtract, ["o_tok", "st8"], ["o_tok"])
                        tt(onr[:], o_tok[:, ci, :], o_tok[:, ci, :], ALU.mult, ["o_tok"], ["onr"])
                        P.add("dve", lambda e: e.tensor_reduce(out=st9[:], in_=onr[:].rearrange("p (h v) -> p h v", v=64), axis=AX.X, op=ALU.add), ["onr"], ["st9"])
                        ts(st9[:], st9[:], 1.0 / 64, 64e-5, ALU.mult, ALU.add, ["st9"], ["st9"])
                        act(st9[:], st9[:], AF.Sqrt, ["st9"], ["st9"])
                        P.add("dve", lambda e: e.reciprocal(out=st9[:], in_=st9[:]), ["st9"], ["st9"])
                        tt(o3, o3, st9[:].unsqueeze(2).to_broadcast([128, 16, 64]), ALU.mult, ["o_tok", "st9"], ["o_tok"])
                        tt(o_tok[:, ci, :], o_tok[:, ci, :], lnw_b[:], ALU.mult, ["o_tok", "lnw_b"], ["o_tok"])
                        tt(o_tok[:, ci, :], o_tok[:, ci, :], lnb_b[:], ALU.add, ["o_tok", "lnb_b"], ["o_tok"])
                        P.add("dve", lambda e, ci=ci: e.tensor_reduce(out=st8[:], in_=kb_tok[:, ci, :].rearrange("p (h v) -> p h v", v=64), axis=AX.X, op=ALU.add), ["kb_tok"], ["st8"])
                        tt(onr[:].rearrange("p (h v) -> p h v", v=64), v_tok32[:, ci, :].rearrange("p (h v) -> p h v", v=64),
                           st8[:].unsqueeze(2).to_broadcast([128, 16, 64]), ALU.mult, ["v_tok32", "st8"], ["onr"])
                        tt(o_tok[:, ci, :], o_tok[:, ci, :], onr[:], ALU.add, ["o_tok", "onr"], ["o_tok"])
                        tt(o_tok[:, ci, :], o_tok[:, ci, :], g_tok[:, ci, :], ALU.mult, ["o_tok", "g_tok"], ["o_tok"])
                        dma("dsp", m_d[r0:r0 + CH, 0:RW], o_tok[:, ci, :], ["o_tok"], ["m_d"])
    P.barrier()
    P.pop()

    if DBG == "p3":
        print("ops after P3:", len(P.ops))
        P.limit = None
        dbg = dout("dbg", [T, 2048])
        dma("dsp", dbg, m_d, (), ["dbg"])
        P.barrier()
        P.emit()
        return nc

    hg_gamma = din("hg_gamma", [2, 2, HG])
    hg_norm_w = din("hg_norm_w", [1, HG])
    P.push()
    C2 = 64
    NC2 = TB // C2
    gm = P.sb([128, 2, 2, 8], F32, "gm")
    for l_ in range(2):
        for d_ in range(2):
            dma("dsp", gm[:, l_, d_, :], hg_gamma[l_, d_, :].rearrange("(h p) -> p h", p=128), (), ["gm"], allow_slow_non_contiguous=True)
    lbT = P.sb([128, 2, 8], F32, "lbT")
    omlb = P.sb([128, 2, 8], F32, "omlb")
    tt(lbT[:], gm[:, 0, :, :], gm[:, 1, :, :], ALU.subtract, ["gm"], ["lbT"])
    act(lbT[:], lbT[:], AF.Sigmoid, ["lbT"], ["lbT"])
    ts(omlb[:], lbT[:], -1.0, 1.0, ALU.mult, ALU.add, ["lbT"], ["omlb"])
    hnw = P.sb([64, HG], F32, "hnw")
    dma("dsp", hnw[:], hg_norm_w[0:1, :].partition_broadcast(64), (), ["hnw"])
    ones2 = P.sb([128, 64], F32, "ones2")
    memset(ones2[:], 1.0, ["ones2"])
    MI = [P.sb([64, 64], F32, f"mi{d_}") for d_ in range(2)]
    for d_ in range(2):
        sgn = 1 if d_ == 0 else -1
        memset(MI[d_][:], 1.0, [f"mi{d_}"])
        P.add("pool", lambda e, d_=d_, sgn=sgn: e.affine_select(out=MI[d_][:], in_=MI[d_][:], pattern=[[sgn, 64]],
              compare_op=ALU.is_ge, fill=0.0, base=0, channel_multiplier=-sgn), [f"mi{d_}"], [f"mi{d_}"])
    S2 = P.sb([128, 8, 128], F32, "S2")
    qz = P.sb([128, TB], F32, "qz")
    fz = P.sb([128, TB], F32, "fz")
    lf = P.sb([128, TB], F32, "lf")
    kq = P.sb([128, TB], F32, "kq")
    b2 = P.sb([128, TB], F32, "b2")
    e2 = P.sb([128, TB], F32, "e2")
    qT = P.sb([128, TB], F32, "qT")
    kT2 = P.sb([128, TB], F32, "kT2")
    khT = P.sb([128, TB], F32, "khT")
    bc2 = P.sb([128, NC2], F32, "bc2")
    pc2 = P.sb([128, NC2], F32, "pc2")
    vt2 = P.sb([64, NC2, 128], F32, "vt2")
    gt2_ = P.sb([64, NC2, HG], F32, "gt2")
    o2 = P.sb([64, NC2, HG], F32, "o2")
    of2 = P.sb([64, HG], F32, "of2")
    sc2_ = P.sb([64, 64], F32, "sc2")
    kh_tok = P.sb([64, 128], F32, "kh_tok")
    sq2 = P.sb([64, HG], F32, "sq2")
    st2 = P.sb([64, 8], F32, "st2")
    for d_ in range(2):
        for (s0, s1) in seq_bounds:
            nblk = (s1 - s0) // TB
            memset(S2[:], 0.0, ["S2"])
            blks = range(nblk) if d_ == 0 else range(nblk - 1, -1, -1)
            for bi in blks:
                t0 = s0 + bi * TB
                if d_ == 1:
                    for ci in range(NC2):
                        dma("dsp", gt2_[:, ci, :], ztok_d[t0 + ci * C2:t0 + (ci + 1) * C2, 1024:2048], (), ["gt2"])
                for h in range(8):
                    dma("dsp", qz[:], zT_d[3456 + h * 128:3456 + (h + 1) * 128, t0:t0 + TB], (), ["qz"])
                    dma("dsp", fz[:], zT_d[4480 + d_ * 1024 + h * 128:4480 + d_ * 1024 + (h + 1) * 128, t0:t0 + TB], (), ["fz"])
                    for ci in range(NC2):
                        dma("dsp", vt2[:, ci, :], ztok_d[t0 + ci * C2:t0 + (ci + 1) * C2, h * 128:(h + 1) * 128], (), ["vt2"])
                    act(qz[:], qz[:], AF.Silu, ["qz"], ["qz"])
                    act(fz[:], fz[:], AF.Sigmoid, ["fz"], ["fz"])
                    ts(fz[:], fz[:], omlb[:, d_, h:h + 1], lbT[:, d_, h:h + 1], ALU.mult, ALU.add, ["fz", "omlb", "lbT"], ["fz"])
                    act(lf[:], fz[:], AF.Ln, ["fz"], ["lf"])
                    ts(kq[:], fz[:], -1.0, 1.0, ALU.mult, ALU.add, ["fz"], ["kq"])
                    for ci in range(NC2):
                        sl = slice(ci * C2, (ci + 1) * C2)
                        P.add("dve", lambda e, sl=sl: e.tensor_tensor_scan(out=b2[:, sl], data0=ones2[:, 0:C2], data1=lf[:, sl],
                              initial=0.0, op0=ALU.mult, op1=ALU.add), ["ones2", "lf"], ["b2"])
                        cp(bc2[:, ci:ci + 1], b2[:, ci * C2 + C2 - 1:ci * C2 + C2], ["b2"], ["bc2"])
                        if d_ == 1:
                            tt(b2[:, sl], lf[:, sl], b2[:, sl], ALU.subtract, ["lf", "b2"], ["b2"])
                            ts(b2[:, sl], b2[:, sl], bc2[:, ci:ci + 1], None, ALU.add, None, ["b2", "bc2"], ["b2"])
                    act(pc2[:], bc2[:], AF.Exp, ["bc2"], ["pc2"])
                    act(e2[:], b2[:], AF.Exp, ["b2"], ["e2"])
                    tt(qT[:], qz[:], e2[:], ALU.mult, ["qz", "e2"], ["qT"])
                    act(e2[:], b2[:], AF.Exp, ["b2"], ["e2"], scale=-1.0)
                    tt(kT2[:], kq[:], e2[:], ALU.mult, ["kq", "e2"], ["kT2"])
                    for ci in range(NC2):
                        sl = slice(ci * C2, (ci + 1) * C2)
                        act(e2[:, sl], b2[:, sl], AF.Exp, ["b2", "bc2"], ["e2"], scale=-1.0, bias=bc2[:, ci:ci + 1])
                    tt(khT[:], kq[:], e2[:], ALU.mult, ["kq", "e2"], ["khT"])
                    cis = range(NC2) if d_ == 0 else range(NC2 - 1, -1, -1)
                    for ci in cis:
                        sl = slice(ci * C2, (ci + 1) * C2)
                        mm(ps[0][0:64, 0:64], kT2[:, sl], qT[:, sl], True, True, ["kT2", "qT"], ["ps0"])
                        tt(sc2_[:], ps[0][0:64, 0:64], MI[d_][:], ALU.mult, ["ps0", f"mi{d_}"], ["sc2"])
                        tr(ps[1][0:64, 0:128], khT[:, sl], ident_f[:], ["khT", "identf"], ["ps1"])
                        cp(kh_tok[:], ps[1][0:64, 0:128], ["ps1"], ["kh_tok"], q="act")
                        mm(ps[2][0:64, 0:128], sc2_[:], vt2[:, ci, :], True, False, ["sc2", "vt2"], ["ps2"])
                        mm(ps[2][0:64, 0:128], qT[:, sl], S2[:, h, :], False, True, ["qT", "S2"], ["ps2"])
                        cp(o2[:, ci, h * 128:(h + 1) * 128], ps[2][0:64, 0:128], ["ps2"], ["o2"], q="act")
                        mm(ps[4][:, 0:128], kh_tok[:], vt2[:, ci, :], True, True, ["kh_tok", "vt2"], ["ps4"])
                        stt(S2[:, h, :], S2[:, h, :], pc2[:, ci:ci + 1], ps[4][:, 0:128], ALU.mult, ALU.add, ["S2", "pc2", "ps4"], ["S2"])
                for ci in range(NC2):
                    r0 = t0 + ci * C2
                    if d_ == 0:
                        dma("dsp", of_d[r0:r0 + C2, :], o2[:, ci, :], ["o2"], ["of_d"])
                    else:
                        dma("dsp", of2[:], of_d[r0:r0 + C2, :], ["of_d"], ["of2"])
                        tt(o2[:, ci, :], o2[:, ci, :], of2[:], ALU.add, ["o2", "of2"], ["o2"])
                        tt(sq2[:], o2[:, ci, :], o2[:, ci, :], ALU.mult, ["o2"], ["sq2"])
                        P.add("dve", lambda e: e.tensor_reduce(out=st2[:], in_=sq2[:].rearrange("p (h v) -> p h v", v=128), axis=AX.X, op=ALU.add), ["sq2"], ["st2"])
                        ts(st2[:], st2[:], 1.0 / 128, 1e-6, ALU.mult, ALU.add, ["st2"], ["st2"])
                        act(st2[:], st2[:], AF.Sqrt, ["st2"], ["st2"])
                        P.add("dve", lambda e: e.reciprocal(out=st2[:], in_=st2[:]), ["st2"], ["st2"])
                        o3 = o2[:, ci, :].rearrange("p (h v) -> p h v", v=128)
                        tt(o3, o3, st2[:].unsqueeze(2).to_broadcast([64, 8, 128]), ALU.mult, ["o2", "st2"], ["o2"])
                        tt(o2[:, ci, :], o2[:, ci, :], hnw[:], ALU.mult, ["o2", "hnw"], ["o2"])
                        act(gt2_[:, ci, :], gt2_[:, ci, :], AF.Silu, ["gt2"], ["gt2"])
                        tt(o2[:, ci, :], o2[:, ci, :], gt2_[:, ci, :], ALU.mult, ["o2", "gt2"], ["o2"])
                        dma("dsp", m_d[r0:r0 + C2, 1024:2048], o2[:, ci, :], ["o2"], ["m_d"])
    P.barrier()
    P.pop()

    if DBG == "p4":
        dbg = dout("dbg", [T, 2048])
        dma("dsp", dbg, m_d, (), ["dbg"])
        P.barrier()
        P.emit()
        return nc

    w_out = din("w_out", [D, D])
    x1_d = dscr("x1_d", [T, D])
    P.push()
    wo = P.sb([128, 16, D], BF16, "wo")
    for k in range(16):
        dma("dpool", wo[:, k, :], w_out[k * 128:(k + 1) * 128, :], (), ["wo"])
    gn1 = [P.sb([128, D], F32, f"gn1_{s_}") for s_ in range(2)]
    for s_ in range(2):
        dma("dsp", gn1[s_][:], vec_d[2, s_:s_ + 1, :].partition_broadcast(128), (), [f"gn1_{s_}"])
    mt = [P.sb([128, D], F32, f"mt{i}") for i in range(2)]
    mb = [P.sb([128, D], BF16, f"mb{i}") for i in range(2)]
    mTs = [P.sb([128, 16, 128], BF16, f"mTs{i}") for i in range(2)]
    mo = [P.sb([128, D], F32, f"mo{i}") for i in range(2)]
    xr = [P.sb([128, D], F32, f"xr{i}") for i in range(2)]
    junk5 = P.sb([128, D], F32, "junk5")
    ss5 = [P.sb([128, 1], F32, f"ss5{i}") for i in range(2)]
    for t in range(NT):
        b = t % 2
        s_ = seq_of_tile(t)
        dma("dsp", mt[b][:], m_d[t * 128:(t + 1) * 128, :], (), [f"mt{b}"])
        dma("dsp", xr[b][:], x[t * 128:(t + 1) * 128, :], (), [f"xr{b}"])
        cp(mb[b][:], mt[b][:], [f"mt{b}"], [f"mb{b}"])
        for k in range(16):
            pb = k % 2
            tr(psb[pb][:, 0:128], mb[b][:, k * 128:(k + 1) * 128], ident_b[:], [f"mb{b}", "identb"], [f"psb{pb}"])
            cp(mTs[b][:, k, :], psb[pb][:, 0:128], [f"psb{pb}"], [f"mTs{b}"], q=("act" if k % 2 else "dve"))
        for cg in range(4):
            for k in range(16):
                mm(ps[cg][:, :], mTs[b][:, k, :], wo[:, k, cg * 512:(cg + 1) * 512], k == 0, k == 15, [f"mTs{b}", "wo"], [f"ps{cg}"])
            cp(mo[b][:, cg * 512:(cg + 1) * 512], ps[cg][:, :], [f"ps{cg}"], [f"mo{b}"], q=("act" if cg % 2 else "dve"))
        act(junk5[:], mo[b][:], AF.Square, [f"mo{b}"], ["junk5", f"ss5{b}"], accum_out=ss5[b][:])
        ts(ss5[b][:], ss5[b][:], 1.0 / D, 1e-6, ALU.mult, ALU.add, [f"ss5{b}"], [f"ss5{b}"])
        act(ss5[b][:], ss5[b][:], AF.Sqrt, [f"ss5{b}"], [f"ss5{b}"])
        P.add("dve", lambda e, b=b: e.reciprocal(out=ss5[b][:], in_=ss5[b][:]), [f"ss5{b}"], [f"ss5{b}"])
        stt(mo[b][:], mo[b][:], ss5[b][:], gn1[s_][:], ALU.mult, ALU.mult, [f"mo{b}", f"ss5{b}", f"gn1_{s_}"], [f"mo{b}"])
        tt(mo[b][:], mo[b][:], xr[b][:], ALU.add, [f"mo{b}", f"xr{b}"], [f"mo{b}"])
        dma("dsp", x1_d[t * 128:(t + 1) * 128, :], mo[b][:], [f"mo{b}"], ["x1_d"])
    P.barrier()
    P.pop()

    w_router = din("w_router", [D, NE])
    e_bias = din("e_bias", [1, NE])
    h2T_d = dscr("h2T_d", [16, 128, T], BF16)
    wselT_d = dscr("wselT_d", [NE, T])
    P.push()
    g2 = [P.sb([128, D], F32, f"g2_{s_}") for s_ in range(2)]
    s2 = [P.sb([128, D], F32, f"s2_{s_}") for s_ in range(2)]
    for s_ in range(2):
        dma("dsp", g2[s_][:], vec_d[3, s_:s_ + 1, :].partition_broadcast(128), (), [f"g2_{s_}"])
        dma("dsp", s2[s_][:], vec_d[4, s_:s_ + 1, :].partition_broadcast(128), (), [f"s2_{s_}"])
    wr = P.sb([128, 16, NE], F32, "wr")
    dma("dsp", wr[:], w_router.rearrange("(k p) e -> p k e", p=128), (), ["wr"])
    eb = P.sb([128, NE], F32, "eb")
    dma("dsp", eb[:], e_bias[0:1, :].partition_broadcast(128), (), ["eb"])
    x6 = [P.sb([128, D], F32, f"x6{i}") for i in range(2)]
    junk6 = P.sb([128, D], F32, "junk6")
    hTf = [P.sb([128, 16, 128], F32, f"hTf{i}") for i in range(2)]
    hTb6 = [P.sb([128, 16, 128], BF16, f"hTb6{i}") for i in range(2)]
    ss6 = [P.sb([128, 1], F32, f"ss6{i}") for i in range(2)]
    scr = P.sb([128, NE], F32, "scr")
    bi = P.sb([128, NE], F32, "bi")
    m8 = P.sb([128, 8, 8], F32, "m8")
    gs = P.sb([128, 8], F32, "gs")
    g8 = P.sb([128, 8], F32, "g8")
    gmask = P.sb([128, 8], F32, "gmask")
    emask = P.sb([128, NE], F32, "emask")
    mk = P.sb([128, NE], F32, "mk")
    tmk = P.sb([128, NE], F32, "tmk")
    t8 = P.sb([128, 8], F32, "t8")
    sel = P.sb([128, NE], F32, "sel")
    wsl = P.sb([128, NE], F32, "wsl")
    wsum = P.sb([128, 1], F32, "wsum")
    wT = P.sb([64, 128], F32, "wT")
    for t in range(NT):
        b = t % 2
        s_ = seq_of_tile(t)
        dma("dsp", x6[b][:], x1_d[t * 128:(t + 1) * 128, :], (), [f"x6{b}"])
        act(junk6[:], x6[b][:], AF.Square, [f"x6{b}"], ["junk6", f"ss6{b}"], accum_out=ss6[b][:])
        ts(ss6[b][:], ss6[b][:], 1.0 / D, 1e-6, ALU.mult, ALU.add, [f"ss6{b}"], [f"ss6{b}"])
        act(ss6[b][:], ss6[b][:], AF.Sqrt, [f"ss6{b}"], [f"ss6{b}"])
        P.add("dve", lambda e, b=b: e.reciprocal(out=ss6[b][:], in_=ss6[b][:]), [f"ss6{b}"], [f"ss6{b}"])
        stt(x6[b][:], x6[b][:], ss6[b][:], g2[s_][:], ALU.mult, ALU.mult, [f"x6{b}", f"ss6{b}", f"g2_{s_}"], [f"x6{b}"])
        tt(x6[b][:], x6[b][:], s2[s_][:], ALU.add, [f"x6{b}", f"s2_{s_}"], [f"x6{b}"])
        for kg in range(4):
            for kk2 in range(4):
                k = kg * 4 + kk2
                tr(ps[kg][:, kk2 * 128:(kk2 + 1) * 128], x6[b][:, k * 128:(k + 1) * 128], ident_f[:], [f"x6{b}", "identf"], [f"ps{kg}"])
            cp(hTf[b][:, kg * 4:(kg + 1) * 4, :].rearrange("p a b -> p (a b)"), ps[kg][:, :], [f"ps{kg}"], [f"hTf{b}"], q="act")
            cp(hTb6[b][:, kg * 4:(kg + 1) * 4, :].rearrange("p a b -> p (a b)"), ps[kg][:, :], [f"ps{kg}"], [f"hTb6{b}"])
        dma("dsp", h2T_d[:, :, t * 128:(t + 1) * 128].rearrange("k p t -> p k t"), hTb6[b][:], [f"hTb6{b}"], ["h2T_d"])
        for k in range(16):
            mm(ps[4][:, 0:NE], hTf[b][:, k, :], wr[:, k, :], k == 0, k == 15, [f"hTf{b}", "wr"], ["ps4"])
        act(scr[:], ps[4][:, 0:NE], AF.Sigmoid, ["ps4"], ["scr"])
        tt(bi[:], scr[:], eb[:], ALU.add, ["scr", "eb"], ["bi"])
        for g_ in range(8):
            P.add("dve", lambda e, g_=g_: e.max(out=m8[:, g_, :], in_=bi[:, g_ * 8:(g_ + 1) * 8]), ["bi"], ["m8"])
        tt(gs[:], m8[:, :, 0], m8[:, :, 1], ALU.add, ["m8"], ["gs"])
        P.add("dve", lambda e: e.max(out=g8[:], in_=gs[:]), ["gs"], ["g8"])
        ts(gmask[:], gs[:], g8[:, 3:4], None, ALU.is_ge, None, ["gs", "g8"], ["gmask"])
        cp(emask[:].rearrange("p (g j) -> p g j", j=8), gmask[:].unsqueeze(2).to_broadcast([128, 8, 8]), ["gmask"], ["emask"])
        ts(tmk[:], emask[:], 10.0, -10.0, ALU.mult, ALU.add, ["emask"], ["tmk"])
        tt(mk[:], bi[:], emask[:], ALU.mult, ["bi", "emask"], ["mk"])
        tt(mk[:], mk[:], tmk[:], ALU.add, ["mk", "tmk"], ["mk"])
        P.add("dve", lambda e: e.max(out=t8[:], in_=mk[:]), ["mk"], ["t8"])
        ts(sel[:], mk[:], t8[:, 5:6], None, ALU.is_ge, None, ["mk", "t8"], ["sel"])
        tt(wsl[:], scr[:], sel[:], ALU.mult, ["scr", "sel"], ["wsl"])
        P.add("dve", lambda e: e.tensor_reduce(out=wsum[:], in_=wsl[:], axis=AX.X, op=ALU.add), ["wsl"], ["wsum"])
        P.add("dve", lambda e: e.reciprocal(out=wsum[:], in_=wsum[:]), ["wsum"], ["wsum"])
        ts(wsl[:], wsl[:], wsum[:], 2.5, ALU.mult, ALU.mult, ["wsl", "wsum"], ["wsl"])
        tr(ps[5][0:64, 0:128], wsl[:], ident_f[:], ["wsl", "identf"], ["ps5"])
        cp(wT[:], ps[5][0:64, 0:128], ["ps5"], ["wT"], q="act")
        dma("dsp", wselT_d[:, t * 128:(t + 1) * 128], wT[:], ["wT"], ["wselT_d"])
    P.barrier()
    P.pop()

    if DBG == "p6":
        dbg = dout("dbg", [NE, T])
        dma("dsp", dbg, wselT_d, (), ["dbg"])
        P.barrier()
        P.emit()
        return nc

    NEC = DE // 128
    TBM = TB
    NTI = TBM // 128
    P.push()
    h2b = P.sb([128, 16, TBM], BF16, "h2b")
    wgt = [P.sb([128, 16, DE], BF16, f"wgt{i}") for i in range(2)]
    wut = [P.sb([128, 16, DE], BF16, f"wut{i}") for i in range(2)]
    wdt = [P.sb([128, NEC, D], BF16, f"wdt{i}") for i in range(2)]
    wbt = [P.sb([128, TBM], F32, f"wbt{i}") for i in range(2)]
    yacc = P.sb([128, NTI, D], F32, "yacc")
    actT = [P.sb([128, NEC, TBM], BF16, f"actT{i}") for i in range(2)]
    sgt = [P.sb([128, TBM], F32, f"sgt{i}") for i in range(2)]
    a1t = [P.sb([128, TBM], F32, f"a1t{i}") for i in range(2)]
    gn2 = P.sb([128, D], F32, "gn2")
    x7 = P.sb([128, D], F32, "x7")
    ss7 = P.sb([128, 1], F32, "ss7")
    junk7 = P.sb([128, D], F32, "junk7")

    def wsrc(e):
        return ("persist:wg%d" % e, "persist:wu%d" % e, "persist:wd%d" % e)

    def load_w(e, par):
        kg_, ku_, kd_ = wsrc(e)
        dma("dsp", wgt[par][:].rearrange("p k n -> p (k n)"), wg_b[e], [kg_], [f"wgt{par}"])
        dma("dsp", wut[par][:].rearrange("p k n -> p (k n)"), wu_b[e], [ku_], [f"wut{par}"])
        dma("dsp", wdt[par][:].rearrange("p k n -> p (k n)"), wd_b[e], [kd_], [f"wdt{par}"])

    order = [NE] + list(range(NE))
    gi = 0
    for tb in range(T // TBM):
        t0 = tb * TBM
        s_ = 0 if t0 < LP else 1
        dma("dsp", h2b[:], h2T_d[:, :, t0:t0 + TBM].rearrange("k p t -> p k t"), (), ["h2b"])
        dma("dsp", gn2[:], vec_d[5, s_:s_ + 1, :].partition_broadcast(128), (), ["gn2"])
        load_w(order[0], gi % 2)

        def GU(i, e, par):
            if e < NE:
                dma("dsp", wbt[par][:], wselT_d[e:e + 1, t0:t0 + TBM].partition_broadcast(128), (), [f"wbt{par}"])
            for ec in range(NEC):
                pg = ps[ec % 2]
                pu = ps[2 + ec % 2]
                kg_ = f"ps{ec % 2}"
                ku_ = f"ps{2 + ec % 2}"
                for k in range(16):
                    mm(pg[:, 0:TBM], wgt[par][:, k, ec * 128:(ec + 1) * 128], h2b[:, k, :], k == 0, k == 15, [f"wgt{par}", "h2b"], [kg_])
                for k in range(16):
                    mm(pu[:, 0:TBM], wut[par][:, k, ec * 128:(ec + 1) * 128], h2b[:, k, :], k == 0, k == 15, [f"wut{par}", "h2b"], [ku_])
                sb_ = ec % 2
                act(sgt[sb_][:], pg[:, 0:TBM], AF.Silu, [kg_], [f"sgt{sb_}"])
                if e < NE:
                    tt(a1t[sb_][:], sgt[sb_][:], pu[:, 0:TBM], ALU.mult, [f"sgt{sb_}", ku_], [f"a1t{sb_}"])
                    tt(actT[par][:, ec, :], a1t[sb_][:], wbt[par][:], ALU.mult, [f"a1t{sb_}", f"wbt{par}"], [f"actT{par}"])
                else:
                    tt(actT[par][:, ec, :], sgt[sb_][:], pu[:, 0:TBM], ALU.mult, [f"sgt{sb_}", ku_], [f"actT{par}"])

        def DN(i, e, par, first):
            j = 0
            for ti in range(NTI):
                for cg in range(4):
                    pd = ps[4 + j % 2]
                    kd_ = f"ps{4 + j % 2}"
                    j += 1
                    for ec in range(NEC):
                        mm(pd[:, :], actT[par][:, ec, ti * 128:(ti + 1) * 128], wdt[par][:, ec, cg * 512:(cg + 1) * 512],
                           ec == 0, ec == NEC - 1, [f"actT{par}", f"wdt{par}"], [kd_])
                    if first:
                        cp(yacc[:, ti, cg * 512:(cg + 1) * 512], pd[:, :], [kd_], ["yacc"])
                    else:
                        tt(yacc[:, ti, cg * 512:(cg + 1) * 512], yacc[:, ti, cg * 512:(cg + 1) * 512], pd[:, :], ALU.add, ["yacc", kd_], ["yacc"])

        n_e = len(order)
        for i, e in enumerate(order):
            par = (gi + i) % 2
            GU(i, e, par)
            if i > 0:
                DN(i - 1, order[i - 1], (gi + i - 1) % 2, i - 1 == 0)
            if i + 1 < n_e:
                load_w(order[i + 1], (gi + i + 1) % 2)
        DN(n_e - 1, order[-1], (gi + n_e - 1) % 2, False)
        gi += n_e
        for ti in range(NTI):
            r0 = t0 + ti * 128
            dma("dsp", x7[:], x1_d[r0:r0 + 128, :], (), ["x7"])
            P.add("dve", lambda e, ti=ti: e.tensor_tensor(out=junk7[:], in0=yacc[:, ti, :], in1=yacc[:, ti, :], op=ALU.mult), ["yacc"], ["junk7"])
            P.add("dve", lambda e: e.tensor_reduce(out=ss7[:], in_=junk7[:], axis=AX.X, op=ALU.add), ["junk7"], ["ss7"])
            ts(ss7[:], ss7[:], 1.0 / D, 1e-6, ALU.mult, ALU.add, ["ss7"], ["ss7"])
            act(ss7[:], ss7[:], AF.Sqrt, ["ss7"], ["ss7"])
            P.add("dve", lambda e: e.reciprocal(out=ss7[:], in_=ss7[:]), ["ss7"], ["ss7"])
            stt(junk7[:], yacc[:, ti, :], ss7[:], gn2[:], ALU.mult, ALU.mult, ["yacc", "ss7", "gn2"], ["junk7"])
            tt(junk7[:], junk7[:], x7[:], ALU.add, ["junk7", "x7"], ["junk7"])
            dma("dsp", y[r0:r0 + 128, :], junk7[:], ["junk7"], ["y"])
    P.barrier()
    P.pop()


    P.barrier()
    P.emit()
    return nc


def kernel(**inp):
    from concourse.bass_utils import run_bass_kernel_spmd
    n = 8
    LP = inp["x_prompt"].shape[1]
    LS = inp["x_sample"].shape[1]
    nc = build_program(dict(LP=LP, LS=LS, DE=int(inp["w_exp_gate"].shape[-1])))
    f = lambda a: np.ascontiguousarray(np.asarray(a, dtype=np.float32))
    shared = dict(
        w_ada=f(inp["w_ada"][0]), b_ada=f(inp["b_ada"][0:1]),
        nrm=f(np.stack([inp["norm_pre_mix"][0], inp["norm_post_mix"][0], inp["norm_pre_ffn"][0], inp["norm_post_ffn"][0]])),
        w_in=f(inp["w_in"][0]), rw_mu=f(inp["rw_mu"][0]), rw_w0=f(inp["rw_w0"][0]), rw_a0=f(inp["rw_a0"][0]),
        rw_w_up=f(inp["rw_w_up"][0]), rw_a_up=f(inp["rw_a_up"][0]), rw_g_up=f(inp["rw_g_up"][0]),
        rw_vecs=f(np.stack([inp["rw_k_k"][0], inp["rw_k_a"][0], inp["rw_r_k"][0].reshape(-1), inp["rw_ln_w"][0], inp["rw_ln_b"][0]])),
        hg_gamma=f(inp["hg_lb_gamma"]), hg_norm_w=f(inp["hg_norm_w"][0:1]), w_out=f(inp["w_out"][0]),
        w_router=f(inp["w_router"][0]), e_bias=f(inp["e_bias"][0:1]),
        w_exp_gate=f(inp["w_exp_gate"][0]), w_exp_up=f(inp["w_exp_up"][0]), w_exp_down=f(inp["w_exp_down"][0]),
        w_sh_gate=f(inp["w_sh_gate"][0]), w_sh_up=f(inp["w_sh_up"][0]), w_sh_down=f(inp["w_sh_down"][0]),
    )
    in_maps = []
    for b in range(n):
        m = dict(shared)
        m["x"] = f(np.concatenate([inp["x_prompt"][b], inp["x_sample"][b]], axis=0))
        m["c"] = f(np.stack([inp["c_prompt"][b], inp["c_sample"][b]]))
        in_maps.append(m)
    res = run_bass_kernel_spmd(nc, in_maps, core_ids=list(range(n)))
    ys = [r["y"] for r in res.results]
    y_prompt = np.stack([yy[:LP] for yy in ys]).astype(np.float32)
    y_sample = np.stack([yy[LP:] for yy in ys]).astype(np.float32)
    return (y_prompt, y_sample)
```

```python
import numpy as np
import concourse.bass as bass
import concourse.mybir as mybir

F32 = mybir.dt.float32
BF16 = mybir.dt.bfloat16
I32 = mybir.dt.int32
AF = mybir.ActivationFunctionType
ALU = mybir.AluOpType
AX = mybir.AxisListType

NDMASEM = 8


class Op:
    __slots__ = ("q", "fn", "deps", "signals", "sem", "ticket", "idx", "prewait", "isbar")

    def __init__(self, q, fn):
        self.q = q
        self.fn = fn
        self.deps = []
        self.signals = False
        self.sem = None
        self.ticket = 0
        self.prewait = None
        self.isbar = False


class Prog:
    ENG = {"pe": "pe", "act": "act", "dve": "dve", "pool": "pool",
           "dsp": "sp", "dpool": "pool", "dact": "act", "dpool2": "pool"}
    DMAQ = ("dsp", "dpool", "dact", "dpool2")

    def __init__(self, nc):
        self.nc = nc
        self.ops = []
        self.last_w = {}
        self.readers = {}
        self.sb_cur = 16512
        self.sb_mark = []
        self.uid = 0
        self.limit = None
        self.cap = None
        self.kmap = None

    def sb(self, shape, dtype, name=None):
        self.uid += 1
        nm = f"{name or 't'}_{self.uid}"
        esz = {F32: 4, BF16: 2, I32: 4, mybir.dt.uint32: 4, mybir.dt.uint16: 2}[dtype]
        per = int(np.prod(shape[1:])) * esz
        per = (per + 31) // 32 * 32
        off = self.sb_cur
        assert off + per <= 229344, f"SBUF overflow allocating {nm}: {off}+{per}"
        self.sb_cur += per
        return self.nc.alloc_sbuf_tensor_at(nm, list(shape), dtype, offset=off)

    def push(self):
        self.sb_mark.append(self.sb_cur)

    def pop(self):
        self.sb_cur = self.sb_mark.pop()

    def add(self, q, fn, reads=(), writes=()):
        if self.kmap is not None:
            reads = [self.kmap.get(k, k) for k in reads]
            writes = [self.kmap.get(k, k) for k in writes]
        if self.cap is not None:
            self.cap.append((q, fn, tuple(reads), tuple(writes)))
            return None
        if self.limit is not None and len(self.ops) >= self.limit:
            return None
        op = Op(q, fn)
        op.idx = len(self.ops)
        if self.limit is not None:
            import sys as _s
            f = _s._getframe(1)
            ln = []
            while f is not None and len(ln) < 3:
                ln.append(f.f_lineno)
                f = f.f_back
            self.lines = getattr(self, "lines", {})
            self.lines[op.idx] = (q, ln)
        deps = set()
        for k in reads:
            if k in self.last_w:
                deps.add(self.last_w[k])
            if isinstance(k, str) and k.startswith("ps"):
                for r in self.readers.get(k, ()):
                    if self.ops[r].q != q:
                        deps.add(r)
        for k in writes:
            if k in self.last_w:
                deps.add(self.last_w[k])
            for r in self.readers.get(k, ()):
                deps.add(r)
        deps.discard(op.idx)
        if q == "pe":
            deps = {d for d in deps if self.ops[d].q != "pe"}
        op.deps = sorted(deps)
        for k in reads:
            self.readers.setdefault(k, []).append(op.idx)
        for k in writes:
            self.last_w[k] = op.idx
            self.readers[k] = []
        self.ops.append(op)
        return op

    def merge(self, lists):
        n = max(len(l) for l in lists)
        for i in range(n):
            for l in lists:
                if i < len(l):
                    self.add(*l[i])

    def barrier(self):
        op = Op("bar", None)
        op.idx = len(self.ops)
        op.isbar = True
        self.ops.append(op)
        self.last_w = {k: v for k, v in self.last_w.items() if isinstance(k, str) and k.startswith("persist:")}
        self.readers = {k: v for k, v in self.readers.items() if isinstance(k, str) and k.startswith("persist:")}

    def emit(self):
        nc = self.nc
        ops = self.ops
        queues = ["pe", "act", "dve", "pool", "dsp", "dpool", "dact", "dpool2"]
        hist = {q: [] for q in queues}
        bar_deps = {}
        for op in ops:
            if op.isbar:
                deps = []
                for q in ("pe", "act", "dve", "pool"):
                    deps += hist[q][-1:]
                for q in ("dsp", "dpool", "dact"):
                    deps += hist[q][-NDMASEM:]
                bar_deps[op.idx] = deps
                for d in deps:
                    ops[d].signals = True
            else:
                hist[op.q].append(op.idx)
        for op in ops:
            for d in op.deps:
                ops[d].signals = True
        sem_c = {q: nc.alloc_semaphore(f"s_{q}") for q in ("pe", "act", "dve", "pool")}
        sem_d = {q: [nc.alloc_semaphore(f"s_{q}{i}") for i in range(NDMASEM)]
                 for q in self.DMAQ}
        cnt_c = {q: 0 for q in sem_c}
        cnt_d = {q: [0] * NDMASEM for q in sem_d}
        rr = {q: 0 for q in sem_d}
        for op in ops:
            if op.isbar:
                continue
            if op.q in sem_c:
                if op.signals:
                    cnt_c[op.q] += 1
                    op.sem = sem_c[op.q]
                    op.ticket = cnt_c[op.q]
            else:
                i = rr[op.q]
                rr[op.q] = (i + 1) % NDMASEM
                op.prewait = (sem_d[op.q][i], cnt_d[op.q][i])
                cnt_d[op.q][i] += 16
                op.sem = sem_d[op.q][i]
                op.ticket = cnt_d[op.q][i]
                op.signals = True
        streams = {"pe": [], "act": [], "dve": [], "pool": [], "sp": []}
        for op in ops:
            if op.isbar:
                for e in streams:
                    streams[e].append(op)
            else:
                streams[self.ENG[op.q]].append(op)
        self.n_wait = 0

        def run_stream(eng, lst):
            waited = {}

            def w(sem, val):
                if val <= 0:
                    return
                if waited.get(sem.num, 0) >= val:
                    return
                eng.wait_ge(sem, val)
                self.n_wait += 1
                waited[sem.num] = val

            for op in lst:
                if op.isbar:
                    for d in bar_deps[op.idx]:
                        w(ops[d].sem, ops[d].ticket)
                    continue
                for d in op.deps:
                    w(ops[d].sem, ops[d].ticket)
                if op.prewait is not None:
                    w(*op.prewait)
                ins = op.fn(eng)
                if op.signals:
                    ins.then_inc(op.sem, 16 if op.q in self.DMAQ else 1)
            return waited

        with nc.Block() as block:
            @block.tensor
            def _(e):
                run_stream(e, streams["pe"])

            @block.scalar
            def _(e):
                run_stream(e, streams["act"])

            @block.vector
            def _(e):
                run_stream(e, streams["dve"])

            @block.gpsimd
            def _(e):
                run_stream(e, streams["pool"])

            @block.sync
            def _(e):
                run_stream(e, streams["sp"])
D = 2048
RW = 1024
HG = 1024
RWC = 3456
INC = 8576
NE = 64


def build_program(cfg):
    LP, LS = cfg["LP"], cfg["LS"]
    DE = cfg.get("DE", 512)
    DBG = cfg.get("debug", None)
    T = LP + LS
    NT = T // 128
    nc = bass.Bass("TRN2", target_bir_lowering=False)
    P = Prog(nc)
    P.limit = cfg.get("limit", None)
    global LAST_PROG
    LAST_PROG = P

    def din(name, shape, dt=F32):
        return nc.dram_tensor(name, list(shape), dt, kind="ExternalInput").ap()

    def dout(name, shape, dt=F32):
        return nc.dram_tensor(name, list(shape), dt, kind="ExternalOutput").ap()

    def dscr(name, shape, dt=F32):
        return nc.dram_tensor(name, list(shape), dt, kind="Internal").ap()

    x = din("x", [T, D])
    c = din("c", [2, D])
    w_ada = din("w_ada", [D, 6 * D])
    b_ada = din("b_ada", [1, 6 * D])
    nrm = din("nrm", [4, D])
    w_in = din("w_in", [D, INC])
    y = dout("y", [T, D])

    vec_d = dscr("vec_d", [6, 2, D])
    hT_d = dscr("hT_d", [16, 128, T], BF16)

    ident_f = P.sb([128, 128], F32, "identf")
    ident_b = P.sb([128, 128], BF16, "identb")
    ps = [nc.alloc_psum_tensor(f"ps{i}", [128, 512], F32) for i in range(8)]

    def dma(q, out, in_, reads=(), writes=(), **kw):
        return P.add(q, lambda e: e.dma_start(out=out, in_=in_, **kw), reads, writes)

    def act(out, in_, func, reads, writes, bias=None, scale=1.0, accum_out=None):
        kw = {}
        if bias is not None:
            kw["bias"] = bias
        if accum_out is not None:
            kw["accum_out"] = accum_out
        return P.add("act", lambda e: e.activation(out=out, in_=in_, func=func, scale=scale, **kw), reads, writes)

    def tt(out, in0, in1, op, reads, writes, q="dve"):
        return P.add(q, lambda e: e.tensor_tensor(out=out, in0=in0, in1=in1, op=op), reads, writes)

    def ts(out, in0, s1, s2, op0, op1, reads, writes, q="dve", accum_out=None):
        kw = {}
        if accum_out is not None:
            kw["accum_out"] = accum_out
        if op1 is None:
            return P.add(q, lambda e: e.tensor_scalar(out=out, in0=in0, scalar1=s1, scalar2=None, op0=op0, **kw), reads, writes)
        return P.add(q, lambda e: e.tensor_scalar(out=out, in0=in0, scalar1=s1, scalar2=s2, op0=op0, op1=op1, **kw), reads, writes)

    def stt(out, in0, scalar, in1, op0, op1, reads, writes):
        return P.add("dve", lambda e: e.scalar_tensor_tensor(out=out, in0=in0, scalar=scalar, in1=in1, op0=op0, op1=op1), reads, writes)

    def cp(out, in_, reads, writes, q="dve"):
        if q == "act":
            return P.add("act", lambda e: e.copy(out=out, in_=in_), reads, writes)
        return P.add(q, lambda e: e.tensor_copy(out=out, in_=in_), reads, writes)

    def mm(out, lhsT, rhs, start, stop, reads, writes):
        return P.add("pe", lambda e: e.matmul(out, lhsT, rhs, start=start, stop=stop), reads, writes)

    def tr(out, in_, ident, reads, writes):
        return P.add("pe", lambda e: e.transpose(out, in_, ident), reads, writes)

    def memset(ap, val, writes, q="pool"):
        return P.add(q, lambda e: e.memset(ap, val), (), writes)

    memset(ident_f[:], 0.0, ["identf"])
    P.add("pool", lambda e: e.affine_select(out=ident_f[:], in_=ident_f[:], pattern=[[-1, 128]],
                                            compare_op=ALU.not_equal, fill=1.0, base=0, channel_multiplier=1),
          ["identf"], ["identf"])
    cp(ident_b[:], ident_f[:], ["identf"], ["identb"])

    P.push()
    cT = P.sb([128, 16, 2], F32, "cT")
    scT = P.sb([128, 16, 2], F32, "scT")
    mod = P.sb([2, 6 * D], F32, "mod")
    bb = [P.sb([2, 512], F32, f"bb{i}") for i in range(2)]
    nr = P.sb([2, 4, D], F32, "nr")
    wa = [P.sb([128, 16, 512], F32, f"wa{i}") for i in range(2)]
    for s_ in range(2):
        dma("dsp", cT[:, :, s_], c[s_, :].rearrange("(k p) -> p k", p=128), (), ["cT"], allow_slow_non_contiguous=True)
    dma("dsp", nr[:], nrm.rearrange("(o f) d -> o f d", o=1).partition_broadcast(2), (), ["nr"])
    act(scT[:], cT[:], AF.Silu, ["cT"], ["scT"])
    for cg in range(24):
        b = cg % 2
        dma("dsp", wa[b][:], w_ada[:, cg * 512:(cg + 1) * 512].rearrange("(k p) n -> p k n", p=128), (), [f"wa{b}"])
        dma("dsp", bb[b][:], b_ada[0:1, cg * 512:(cg + 1) * 512].partition_broadcast(2), (), [f"bb{b}"])
        for k in range(16):
            mm(ps[0][0:2, :], scT[:, k, :], wa[b][:, k, :], k == 0, k == 15, ["scT", f"wa{b}"], ["ps0"])
        tt(mod[:, cg * 512:(cg + 1) * 512], ps[0][0:2, :], bb[b][:], ALU.add,
           ["ps0", f"bb{b}"], ["mod"])
    vecs = P.sb([2, 6, D], F32, "vecs")
    stt(vecs[:, 0, :], mod[:, D:2 * D], 1.0, nr[:, 0, :], ALU.add, ALU.mult, ["mod", "nr"], ["vecs"])
    cp(vecs[:, 1, :], mod[:, 0:D], ["mod"], ["vecs"])
    tt(vecs[:, 2, :], mod[:, 2 * D:3 * D], nr[:, 1, :], ALU.mult, ["mod", "nr"], ["vecs"])
    stt(vecs[:, 3, :], mod[:, 4 * D:5 * D], 1.0, nr[:, 2, :], ALU.add, ALU.mult, ["mod", "nr"], ["vecs"])
    cp(vecs[:, 4, :], mod[:, 3 * D:4 * D], ["mod"], ["vecs"])
    tt(vecs[:, 5, :], mod[:, 5 * D:6 * D], nr[:, 3, :], ALU.mult, ["mod", "nr"], ["vecs"])
    dma("dsp", vec_d.rearrange("f s d -> s f d"), vecs[:], ["vecs"], ["vec_d"])
    P.barrier()
    P.pop()

    def seq_of_tile(t):
        return 0 if t * 128 < LP else 1

    P.push()
    g1 = [P.sb([128, D], F32, f"g1_{s}") for s in range(2)]
    s1 = [P.sb([128, D], F32, f"s1_{s}") for s in range(2)]
    for s in range(2):
        dma("dsp", g1[s][:], vec_d[0, s:s + 1, :].partition_broadcast(128), (), [f"g1_{s}"])
        dma("dsp", s1[s][:], vec_d[1, s:s + 1, :].partition_broadcast(128), (), [f"s1_{s}"])
    xt = [P.sb([128, D], F32, f"xt{i}") for i in range(2)]
    junk = P.sb([128, D], F32, "junk")
    hTs = [P.sb([128, 16, 128], BF16, f"hTs{i}") for i in range(2)]
    ss = [P.sb([128, 1], F32, f"ss{i}") for i in range(2)]
    rs = [P.sb([128, 1], F32, f"rs{i}") for i in range(2)]
    for t in range(NT):
        b = t % 2
        s = seq_of_tile(t)
        dma("dsp", xt[b][:], x[t * 128:(t + 1) * 128, :], (), [f"xt{b}"])
        act(junk[:], xt[b][:], AF.Square, [f"xt{b}"], ["junk", f"ss{b}"], accum_out=ss[b][:])
        ts(rs[b][:], ss[b][:], 1.0 / D, 1e-6, ALU.mult, ALU.add, [f"ss{b}"], [f"rs{b}"])
        act(rs[b][:], rs[b][:], AF.Sqrt, [f"rs{b}"], [f"rs{b}"])
        P.add("dve", lambda e, b=b: e.reciprocal(out=rs[b][:], in_=rs[b][:]), [f"rs{b}"], [f"rs{b}"])
        stt(xt[b][:], xt[b][:], rs[b][:], g1[s][:], ALU.mult, ALU.mult, [f"xt{b}", f"rs{b}", f"g1_{s}"], [f"xt{b}"])
        tt(xt[b][:], xt[b][:], s1[s][:], ALU.add, [f"xt{b}", f"s1_{s}"], [f"xt{b}"])
        for kg in range(4):
            pk = (t * 4 + kg) % 8
            for k4 in range(4):
                k = kg * 4 + k4
                tr(ps[pk][:, k4 * 128:(k4 + 1) * 128], xt[b][:, k * 128:(k + 1) * 128], ident_f[:], [f"xt{b}", "identf"], [f"ps{pk}"])
            cp(hTs[b][:, kg * 4:(kg + 1) * 4, :].rearrange("p a b -> p (a b)"), ps[pk][:, :], [f"ps{pk}"], [f"hTs{b}"], q=("act" if kg % 2 else "dve"))
        dma("dsp", hT_d[:, :, t * 128:(t + 1) * 128].rearrange("k p t -> p k t"), hTs[b][:], [f"hTs{b}"], ["hT_d"])
    P.barrier()
    P.pop()

    if DBG == "p1":
        dbg = dout("dbg", [16, 128, T], BF16)
        dma("dsp", dbg, hT_d, (), ["dbg"])
        dbg2 = dout("dbg2", [6, 2, D])
        dma("dsp", dbg2, vec_d, (), ["dbg2"])
        P.barrier()
        P.emit()
        return nc

    TB = 512 if (LP % 512 == 0 and LS % 512 == 0) else 128
    NF = 6528
    win_b = dscr("win_b", [D, INC], BF16)
    zT_d = dscr("zT_d", [NF, T])
    ztok_d = dscr("ztok_d", [T, 2048])
    for k in range(16):
        dma("dpool", win_b[k * 128:(k + 1) * 128, :], w_in[k * 128:(k + 1) * 128, :], (), ["win_b"])
    P.barrier()
    DS = DE
    w_exp_gate = din("w_exp_gate", [NE, D, DE])
    w_exp_up = din("w_exp_up", [NE, D, DE])
    w_exp_down = din("w_exp_down", [NE, DE, D])
    w_sh_gate = din("w_sh_gate", [D, DS])
    w_sh_up = din("w_sh_up", [D, DS])
    w_sh_down = din("w_sh_down", [DS, D])
    wg_b = dscr("wg_b", [NE + 1, 128, 16 * DE], BF16)
    wu_b = dscr("wu_b", [NE + 1, 128, 16 * DE], BF16)
    wd_b = dscr("wd_b", [NE + 1, 128, (DE // 128) * D], BF16)
    for e_ in ([] if DBG in ("p2", "p3", "p4") else [NE] + list(range(NE))):
        sg_ = w_sh_gate if e_ == NE else w_exp_gate[e_]
        su_ = w_sh_up if e_ == NE else w_exp_up[e_]
        sd_ = w_sh_down if e_ == NE else w_exp_down[e_]
        dma("dpool2", wg_b[e_].rearrange("p (k n) -> p k n", k=16), sg_.rearrange("(k p) n -> p k n", p=128), (), ["persist:wg%d" % e_])
        dma("dpool2", wu_b[e_].rearrange("p (k n) -> p k n", k=16), su_.rearrange("(k p) n -> p k n", p=128), (), ["persist:wu%d" % e_])
        dma("dpool2", wd_b[e_].rearrange("p (k n) -> p k n", k=DE // 128), sd_.rearrange("(k p) n -> p k n", p=128), (), ["persist:wd%d" % e_])
    P.push()
    hTb = [P.sb([128, 16, TB], BF16, f"hTb{i}") for i in range(2)]
    wt = [P.sb([128, 16, 512], BF16, f"wt{i}") for i in range(2)]
    zs = [P.sb([128, 512], F32, f"zs{i}") for i in range(3)]
    groups = [(i * 512, 512, "F") for i in range(12)] + [(6144, 384, "F")] + [(6528 + i * 512, 512, "T") for i in range(4)]
    it = 0
    zi = 0
    for tb in range(T // TB):
        hb_ = tb % 2
        dma("dsp", hTb[hb_][:], hT_d[:, :, tb * TB:(tb + 1) * TB].rearrange("k p t -> p k t"), (), [f"hTb{hb_}"])
        for (c0, ncol, mode) in groups:
            wb = it % 2
            it += 1
            dma("dsp", wt[wb][:, :, 0:ncol], win_b[:, c0:c0 + ncol].rearrange("(k p) n -> p k n", p=128), (), [f"wt{wb}"])
            if mode == "F":
                for ci in range(ncol // 128):
                    pi = zi % 4
                    z_ = zi % 3
                    zi += 1
                    for k in range(16):
                        mm(ps[pi][:, 0:TB], wt[wb][:, k, ci * 128:(ci + 1) * 128], hTb[hb_][:, k, :], k == 0, k == 15,
                           [f"wt{wb}", f"hTb{hb_}"], [f"ps{pi}"])
                    cp(zs[z_][:, 0:TB], ps[pi][:, 0:TB], [f"ps{pi}"], [f"zs{z_}"], q=("act" if zi % 2 else "dve"))
                    dma("dsp", zT_d[c0 + ci * 128:c0 + (ci + 1) * 128, tb * TB:(tb + 1) * TB], zs[z_][:, 0:TB], [f"zs{z_}"], ["zT_d"])
            else:
                for ti in range(TB // 128):
                    pi = zi % 4
                    z_ = zi % 3
                    zi += 1
                    for k in range(16):
                        mm(ps[pi][:, :], hTb[hb_][:, k, ti * 128:(ti + 1) * 128], wt[wb][:, k, :], k == 0, k == 15,
                           [f"wt{wb}", f"hTb{hb_}"], [f"ps{pi}"])
                    cp(zs[z_][:, :], ps[pi][:, :], [f"ps{pi}"], [f"zs{z_}"], q=("act" if zi % 2 else "dve"))
                    dma("dsp", ztok_d[tb * TB + ti * 128:tb * TB + (ti + 1) * 128, c0 - 6528:c0 - 6528 + 512], zs[z_][:, :], [f"zs{z_}"], ["ztok_d"])
    P.barrier()
    P.pop()

    if DBG == "p2":
        dbg = dout("dbg", [NF, T])
        dma("dsp", dbg, zT_d, (), ["dbg"])
        dbg2 = dout("dbg2", [T, 2048])
        dma("dsp", dbg2, ztok_d, (), ["dbg2"])
        P.barrier()
        P.emit()
        return nc

    if DBG is not None:
        print("ops before P3:", len(P.ops))
    rw_mu = din("rw_mu", [RWC])
    rw_w0 = din("rw_w0", [2, RW])
    rw_a0 = din("rw_a0", [2, RW])
    rw_w_up = din("rw_w_up", [2, 64, RW])
    rw_a_up = din("rw_a_up", [2, 64, RW])
    rw_g_up = din("rw_g_up", [128, RW])
    rw_vecs = din("rw_vecs", [5, RW])
    m_d = dscr("m_d", [T, 2048])
    of_d = dscr("of_d", [T, RW])
    P.push()
    CH = 128
    TB_saved = TB
    TB = min(TB, 256)
    NCB = TB // CH
    muT = P.sb([64, 54], F32, "muT")
    ommT = P.sb([64, 54], F32, "ommT")
    hmuT = P.sb([64, 54], F32, "hmuT")
    dma("dsp", muT[:], rw_mu.rearrange("(j p) -> p j", p=64), (), ["muT"], allow_slow_non_contiguous=True)
    ts(ommT[:], muT[:], -1.0, 1.0, ALU.mult, ALU.add, ["muT"], ["ommT"])
    ts(hmuT[:], muT[:], 0.5, None, ALU.mult, None, ["muT"], ["hmuT"])
    mug = P.sb([128, 3], F32, "mug")
    dma("dsp", mug[:, 0:1], rw_mu[3328:3456].rearrange("(p o) -> p o", o=1), (), ["mug"])
    ts(mug[:, 1:2], mug[:, 0:1], -1.0, 1.0, ALU.mult, ALU.add, ["mug"], ["mug"])
    ts(mug[:, 2:3], mug[:, 0:1], 0.5, None, ALU.mult, None, ["mug"], ["mug"])
    w0T = P.sb([64, 2, 16], F32, "w0T")
    a0T = P.sb([64, 2, 16], F32, "a0T")
    for d_ in range(2):
        dma("dsp", w0T[:, d_, :], rw_w0[d_, :].rearrange("(h p) -> p h", p=64), (), ["w0T"], allow_slow_non_contiguous=True)
        dma("dsp", a0T[:, d_, :], rw_a0[d_, :].rearrange("(h p) -> p h", p=64), (), ["a0T"], allow_slow_non_contiguous=True)
    vT = P.sb([64, 5, 16], F32, "vT")
    for i_ in range(3):
        dma("dsp", vT[:, i_, :], rw_vecs[i_, :].rearrange("(h p) -> p h", p=64), (), ["vT"], allow_slow_non_contiguous=True)
    omka = P.sb([64, 16], F32, "omka")
    ts(omka[:], vT[:, 1, :], -1.0, 1.0, ALU.mult, ALU.add, ["vT"], ["omka"])
    lnw_b = P.sb([128, RW], F32, "lnw_b")
    lnb_b = P.sb([128, RW], F32, "lnb_b")
    rk_b = P.sb([128, RW], F32, "rk_b")
    dma("dsp", rk_b[:], rw_vecs[2:3, :].partition_broadcast(128), (), ["rk_b"])
    dma("dsp", lnw_b[:], rw_vecs[3:4, :].partition_broadcast(128), (), ["lnw_b"])
    dma("dsp", lnb_b[:], rw_vecs[4:5, :].partition_broadcast(128), (), ["lnb_b"])
    wup = P.sb([64, 2, RW], BF16, "wup")
    aup = P.sb([64, 2, RW], BF16, "aup")
    gup = P.sb([128, RW], BF16, "gup")
    for d_ in range(2):
        dma("dpool", wup[:, d_, :], rw_w_up[d_], (), ["wup"])
        dma("dpool", aup[:, d_, :], rw_a_up[d_], (), ["aup"])
    dma("dpool", gup[:], rw_g_up, (), ["gup"])
    ones_f = P.sb([128, 128], F32, "ones_f")
    memset(ones_f[:], 1.0, ["ones_f"])
    MASK2 = [P.sb([128, 256], F32, f"mask2_{d_}") for d_ in range(2)]
    MASKX = [P.sb([128, 128], F32, f"maskx_{d_}") for d_ in range(2)]
    for d_ in range(2):
        memset(MASK2[d_][:], 1.0, [f"mask2_{d_}"])
        memset(MASKX[d_][:], 1.0, [f"maskx_{d_}"])
        sgn = 1 if d_ == 0 else -1
        P.add("pool", lambda e, d_=d_, sgn=sgn: e.affine_select(out=MASK2[d_][:, 0:128], in_=MASK2[d_][:, 0:128], pattern=[[sgn, 128]],
              compare_op=ALU.is_gt, fill=0.0, base=0, channel_multiplier=-sgn), [f"mask2_{d_}"], [f"mask2_{d_}"])
        P.add("pool", lambda e, d_=d_, sgn=sgn: e.affine_select(out=MASK2[d_][:, 128:256], in_=MASK2[d_][:, 128:256], pattern=[[sgn, 128]],
              compare_op=ALU.is_ge, fill=0.0, base=0, channel_multiplier=-sgn), [f"mask2_{d_}"], [f"mask2_{d_}"])
        P.add("pool", lambda e, d_=d_, sgn=sgn: e.affine_select(out=MASKX[d_][:], in_=MASKX[d_][:], pattern=[[-sgn, 128]],
              compare_op=ALU.is_gt, fill=0.0, base=0, channel_multiplier=sgn), [f"maskx_{d_}"], [f"maskx_{d_}"])
    ST = P.sb([64, 16, 64], F32, "ST")
    STb = P.sb([64, 16, 64], F32 if cfg.get("rw_fp32", True) else BF16, "STb")
    zh = [P.sb([64, TB + 2], F32, f"zh{i}") for i in range(2)]
    sft = P.sb([64, TB], F32, "sft")
    lat = P.sb([64, 5, TB], BF16, "lat")
    sgl = P.sb([128, TB], BF16, "sgl")
    zg = P.sb([128, TB + 2], F32, "zg")
    sftg = P.sb([128, TB], F32, "sftg")
    CD = F32 if cfg.get("rw_fp32", True) else BF16
    ident_c = ident_f if CD == F32 else ident_b
    identc_k = "identf" if CD == F32 else "identb"
    def mk_stream(sid):
        S = {}
        S["zh"] = [P.sb([64, TB + 2], F32, f"zhs{i}") for i in range(2)]
        S["rr_"] = P.sb([64, TB], F32, "rr")
        S["kk_"] = P.sb([64, TB], F32, "kk")
        S["vv_"] = P.sb([64, TB], F32, "vv")
        S["kn"] = P.sb([64, TB], F32, "kn")
        S["t1"] = P.sb([64, TB], F32, "t1")
        S["t2"] = P.sb([64, TB], F32, "t2")
        S["lw"] = P.sb([64, TB], F32, "lw")
        S["aa"] = P.sb([64, TB], F32, "aa")
        S["kd"] = P.sb([64, TB], F32, "kd")
        S["bb_"] = P.sb([64, TB], F32, "bbk")
        S["cl"] = P.sb([64, TB], F32, "cl")
        S["ex"] = P.sb([64, TB], F32, "ex")
        S["ART"] = P.sb([64, NCB, 2, CH], CD, "ART")
        S["BT"] = P.sb([64, TB], CD, "BT")
        S["KT"] = P.sb([64, TB], CD, "KT")
        S["BhT"] = P.sb([64, TB], CD, "BhT")
        S["KhT"] = P.sb([64, TB], CD, "KhT")
        S["AfT"] = P.sb([64, TB], CD, "AfT")
        S["RfT"] = P.sb([64, TB], CD, "RfT")
        S["VT"] = P.sb([64, TB], CD, "VT")
        S["rkv32"] = P.sb([64, 2, TB], F32, "rkv32")
        S["pc"] = P.sb([64, NCB], F32, "pc")
        S["clc"] = P.sb([64, NCB], F32, "clc")
        S["tok"] = P.sb([128, 5, 64], CD, "tok")
        S["NN"] = [P.sb([128, 256], CD, f"NN{i}") for i in range(2)]
        S["NX"] = [P.sb([128, 256], CD, f"NX{i}") for i in range(2)]
        S["Wt"] = [P.sb([128, 128], CD, f"Wt{i}") for i in range(2)]
        S["RhT"] = P.sb([64, 128], CD, "RhT")
        S["GB"] = P.sb([64, 64], CD, "GB")
        S["zi"] = 0
        S["ps"] = [ps[4 * sid + j] for j in (0, 1, 2, 0, 2, 3)]
        return S
    STREAMS = [mk_stream(0), mk_stream(1)]
    o_tok = P.sb([128, NCB, RW], F32, "o_tok")
    g_tok = P.sb([128, NCB, RW], F32, "g_tok")
    kb_tok = P.sb([128, NCB, RW], F32, "kb_tok")
    v_tok32 = P.sb([128, NCB, RW], F32, "v_tok32")
    of_t = P.sb([128, RW], F32, "of_t")
    st8 = P.sb([128, 16], F32, "st8")
    st9 = P.sb([128, 16], F32, "st9")
    onr = P.sb([128, RW], F32, "onr")

    seq_bounds = [(0, LP), (LP, T)]
    OT = [f"o_tok{h}" for h in range(16)]
    KBT = [f"kb_tok{h}" for h in range(16)]
    VTT = [f"v_tok32{h}" for h in range(16)]
    STK = [f"ST{h}" for h in range(16)]
    STBK = [f"STb{h}" for h in range(16)]

    def mk_kmap(h, sid):
        m = {}
        for n_ in ("kn", "t1", "t2", "lw", "aa", "kd", "bbk", "cl", "ex", "ART", "BT", "KT", "BhT", "KhT", "AfT", "RfT",
                   "VT", "rkv32", "pc", "clc", "tok", "NN0", "NN1", "NX0", "NX1", "Wt0", "Wt1", "RhT", "GB", "zh0", "zh1"):
            m[n_] = f"{n_}@{sid}"
        for i_, j_ in enumerate((0, 1, 2, 0, 2, 3)):
            m[f"ps{i_}"] = f"ps{4 * sid + j_}"
        m["ST"] = f"ST{h}"
        m["STb"] = f"STb{h}"
        m["o_tok"] = f"o_tok{h}"
        m["kb_tok"] = f"kb_tok{h}"
        m["v_tok32"] = f"v_tok32{h}"
        return m

    def load_shift(dst_sft, ztile, row0, nrow, t0, s0, s1, colj, key_z, key_o, omm_ap, hmu_ap):
        lo = t0 - 1
        hi = t0 + TB + 1
        a = max(lo, s0)
        b = min(hi, s1)
        if lo < s0:
            memset(ztile[0:nrow, 0:1], 0.0, [key_z])
        if hi > s1:
            memset(ztile[0:nrow, TB + 1:TB + 2], 0.0, [key_z])
        dma("dsp", ztile[0:nrow, a - lo:b - lo], zT_d[row0:row0 + nrow, a:b], (), [key_z])
        tt(dst_sft, ztile[0:nrow, 0:TB], ztile[0:nrow, 2:TB + 2], ALU.add, [key_z], [key_o])
        ts(dst_sft, dst_sft, hmu_ap, None, ALU.mult, None, [key_o], [key_o])
        stt(dst_sft, ztile[0:nrow, 1:TB + 1], omm_ap, dst_sft, ALU.mult, ALU.add, [key_z, key_o], [key_o])

    zi = 0
    for d_ in range(2):
        for (s0, s1) in seq_bounds:
            nblk = (s1 - s0) // TB
            memset(ST[:], 0.0, STK)
            memset(STb[:], 0.0, STBK)
            blks = range(nblk) if d_ == 0 else range(nblk - 1, -1, -1)
            for bi in blks:
                t0 = s0 + bi * TB
                for j_ in range(4):
                    zb = zi % 2
                    zi += 1
                    load_shift(sft[:], zh[zb], 3072 + j_ * 64, 64, t0, s0, s1, 48 + j_, f"zh{zb}", "sft",
                               ommT[:, 48 + j_:49 + j_], hmuT[:, 48 + j_:49 + j_])
                    if j_ < 2:
                        act(lat[:, j_, :], sft[:], AF.Tanh, ["sft"], ["lat"])
                    else:
                        cp(lat[:, j_, :], sft[:], ["sft"], ["lat"])
                load_shift(sftg[:], zg, 3328, 128, t0, s0, s1, 0, "zg", "sftg", mug[:, 1:2], mug[:, 2:3])
                act(sgl[:], sftg[:], AF.Sigmoid, ["sftg"], ["sgl"])
                for ci in range(NCB):
                    for half in range(2):
                        pi = 4 + half
                        mm(ps[pi][:, :], sgl[:, ci * CH:(ci + 1) * CH], gup[:, half * 512:(half + 1) * 512], True, True,
                           ["sgl", "gup"], [f"ps{pi}"])
                        cp(g_tok[:, ci, half * 512:(half + 1) * 512], ps[pi][:, :], [f"ps{pi}"], ["g_tok"], q="act")
                def head_body(h, sid):
                    S = STREAMS[sid]
                    ps = S["ps"]
                    zh = S["zh"]
                    rr_ = S["rr_"]
                    kk_ = S["kk_"]
                    vv_ = S["vv_"]
                    kn = S["kn"]
                    t1 = S["t1"]
                    t2 = S["t2"]
                    lw = S["lw"]
                    aa = S["aa"]
                    kd = S["kd"]
                    bb_ = S["bb_"]
                    cl = S["cl"]
                    ex = S["ex"]
                    ART = S["ART"]
                    BT = S["BT"]
                    KT = S["KT"]
                    BhT = S["BhT"]
                    KhT = S["KhT"]
                    AfT = S["AfT"]
                    RfT = S["RfT"]
                    VT = S["VT"]
                    rkv32 = S["rkv32"]
                    pc = S["pc"]
                    clc = S["clc"]
                    tok = S["tok"]
                    NN = S["NN"]
                    NX = S["NX"]
                    Wt = S["Wt"]
                    RhT = S["RhT"]
                    GB = S["GB"]
                    for (dst, base, colj) in ((rr_, 0, h), (kk_, 1024, 16 + h), (vv_, 2048, 32 + h)):
                        zb = S["zi"] % 2
                        S["zi"] += 1
                        load_shift(dst[:], zh[zb], base + h * 64, 64, t0, s0, s1, colj, f"zh{zb}", dst.name,
                                   ommT[:, colj:colj + 1], hmuT[:, colj:colj + 1])
                    ts(kn[:], kk_[:], vT[:, 0, h:h + 1], None, ALU.mult, None, [kk_.name, "vT"], ["kn"])
                    tt(t1[:], kn[:], kn[:], ALU.mult, ["kn"], ["t1"])
                    mm(ps[0][0:64, 0:TB], ones_f[0:64, 0:64], t1[:], True, True, ["ones_f", "t1"], ["ps0"])
                    act(t2[:], ps[0][0:64, 0:TB], AF.Sqrt, ["ps0"], ["t2"])
                    ts(t2[:], t2[:], 1e-12, None, ALU.max, None, ["t2"], ["t2"])
                    P.add("dve", lambda e: e.reciprocal(out=t2[:], in_=t2[:]), ["t2"], ["t2"])
                    tt(kn[:], kn[:], t2[:], ALU.mult, ["kn", "t2"], ["kn"])
                    mm(ps[1][0:64, 0:TB], wup[:, d_, h * 64:(h + 1) * 64], lat[:, d_, :], True, True, ["wup", "lat"], ["ps1"])
                    act(lw[:], ps[1][0:64, 0:TB], AF.Sigmoid, ["ps1", "w0T"], ["lw"], bias=w0T[:, d_, h:h + 1])
                    ts(lw[:], lw[:], -0.6065306597126334, None, ALU.mult, None, ["lw"], ["lw"])
                    mm(ps[2][0:64, 0:TB], aup[:, d_, h * 64:(h + 1) * 64], lat[:, 2 + d_, :], True, True, ["aup", "lat"], ["ps2"])
                    act(aa[:], ps[2][0:64, 0:TB], AF.Sigmoid, ["ps2", "a0T"], ["aa"], bias=a0T[:, d_, h:h + 1])
                    ts(t1[:], aa[:], vT[:, 1, h:h + 1], omka[:, h:h + 1], ALU.mult, ALU.add, ["aa", "vT", "omka"], ["t1"])
                    tt(kd[:], kk_[:], t1[:], ALU.mult, [kk_.name, "t1"], ["kd"])
                    tt(bb_[:], kn[:], aa[:], ALU.mult, ["kn", "aa"], ["bbk"])
                    stt(rkv32[:, 1, :], rr_[:], 0.5, kd[:], ALU.mult, ALU.mult, [rr_.name, "kd"], ["rkv32"])
                    for ci in range(NCB):
                        sl = slice(ci * CH, (ci + 1) * CH)
                        P.add("dve", lambda e, sl=sl: e.tensor_tensor_scan(out=cl[:, sl], data0=ones_f[0:64, 0:CH], data1=lw[:, sl],
                              initial=0.0, op0=ALU.mult, op1=ALU.add), ["ones_f", "lw"], ["cl"])
                        if d_ == 1:
                            cp(clc[:, ci:ci + 1], cl[:, ci * CH + CH - 1:ci * CH + CH], ["cl"], ["clc"])
                            tt(cl[:, sl], lw[:, sl], cl[:, sl], ALU.subtract, ["lw", "cl"], ["cl"])
                            ts(cl[:, sl], cl[:, sl], clc[:, ci:ci + 1], None, ALU.add, None, ["cl", "clc"], ["cl"])
                        else:
                            cp(clc[:, ci:ci + 1], cl[:, ci * CH + CH - 1:ci * CH + CH], ["cl"], ["clc"])
                    act(pc[:], clc[:], AF.Exp, ["clc"], ["pc"])
                    act(ex[:], cl[:], AF.Exp, ["cl"], ["ex"])
                    tt(t1[:], rr_[:], ex[:], ALU.mult, [rr_.name, "ex"], ["t1"])
                    for ci in range(NCB):
                        cp(ART[:, ci, 1, :], t1[:, ci * CH:(ci + 1) * CH], ["t1"], ["ART"], q="act")
                    cp(RfT[:], t1[:], ["t1"], ["RfT"], q="act")
                    tt(t2[:], cl[:], lw[:], ALU.subtract, ["cl", "lw"], ["t2"])
                    act(ex[:], t2[:], AF.Exp, ["t2"], ["ex"])
                    stt(t1[:], kn[:], -1.0, ex[:], ALU.mult, ALU.mult, ["kn", "ex"], ["t1"])
                    for ci in range(NCB):
                        cp(ART[:, ci, 0, :], t1[:, ci * CH:(ci + 1) * CH], ["t1"], ["ART"], q="act")
                    cp(AfT[:], t1[:], ["t1"], ["AfT"], q="act")
                    act(ex[:], cl[:], AF.Exp, ["cl"], ["ex"], scale=-1.0)
                    tt(BT[:], bb_[:], ex[:], ALU.mult, ["bbk", "ex"], ["BT"])
                    tt(KT[:], kd[:], ex[:], ALU.mult, ["kd", "ex"], ["KT"])
                    for ci in range(NCB):
                        sl = slice(ci * CH, (ci + 1) * CH)
                        act(ex[:, sl], cl[:, sl], AF.Exp, ["cl", "clc"], ["ex"], scale=-1.0, bias=clc[:, ci:ci + 1])
                    tt(BhT[:], bb_[:], ex[:], ALU.mult, ["bbk", "ex"], ["BhT"])
                    tt(KhT[:], kd[:], ex[:], ALU.mult, ["kd", "ex"], ["KhT"])
                    cp(VT[:], vv_[:], [vv_.name], ["VT"], q="act")
                    cis = range(NCB) if d_ == 0 else range(NCB - 1, -1, -1)
                    for ci in cis:
                        sl = slice(ci * CH, (ci + 1) * CH)
                        for i_, (src, sk) in enumerate(((AfT, "AfT"), (BhT, "BhT"), (KhT, "KhT"), (RfT, "RfT"), (VT, "VT"))):
                            tr(ps[3][:, 192 + i_ * 64:192 + (i_ + 1) * 64], src[:, sl], ident_c[0:64, 0:64], [sk, identc_k], ["ps3"])
                        cp(tok[:].rearrange("p a b -> p (a b)"), ps[3][:, 192:512], ["ps3"], ["tok"])
                        tr(ps[3][:, 0:64], rkv32[:, 1, sl], ident_f[0:64, 0:64], ["rkv32", "identf"], ["ps3"])
                        tr(ps[3][:, 64:128], rr_[:, sl], ident_f[0:64, 0:64], [rr_.name, "identf"], ["ps3"])
                        tr(ps[3][:, 128:192], vv_[:, sl], ident_f[0:64, 0:64], [vv_.name, "identf"], ["ps3"])
                        tt(kb_tok[:, ci, h * 64:(h + 1) * 64], ps[3][:, 0:64], rk_b[:, h * 64:(h + 1) * 64], ALU.mult, ["ps3", "rk_b"], ["kb_tok"]) if d_ == 0 else \
                            stt(kb_tok[:, ci, h * 64:(h + 1) * 64], ps[3][:, 0:64], 1.0, rk_b[:, h * 64:(h + 1) * 64], ALU.mult, ALU.mult, ["ps3", "rk_b"], ["kb_tok"])
                        cp(v_tok32[:, ci, h * 64:(h + 1) * 64], ps[3][:, 128:192], ["ps3"], ["v_tok32"], q="act")
                        mm(ps[0][:, 0:256], BT[:, sl], ART[:, ci, :, :].rearrange("p a b -> p (a b)"), True, True, ["BT", "ART"], ["ps0"])
                        tt(NN[0][:], ps[0][:, 0:256], MASK2[d_][:], ALU.mult, ["ps0", f"mask2_{d_}"], ["NN0"])
                        mm(ps[1][:, 0:256], KT[:, sl], ART[:, ci, :, :].rearrange("p a b -> p (a b)"), True, True, ["KT", "ART"], ["ps1"])
                        tt(NN[1][:], ps[1][:, 0:256], MASK2[d_][:], ALU.mult, ["ps1", f"mask2_{d_}"], ["NN1"])
                        mm(ps[2][:, 0:128], ART[:, ci, 0, :], BT[:, sl], True, True, ["ART", "BT"], ["ps2"])
                        cur = 0
                        cp(NX[cur][:, 0:128], NN[0][:, 0:128], ["NN0"], [f"NX{cur}"], q="act")
                        tt(NX[cur][:, 128:256], ps[2][:, 0:128], MASKX[d_][:], ALU.mult, ["ps2", f"maskx_{d_}"], [f"NX{cur}"])
                        mm(ps[2][:, 128:192], NN[1][:, 0:128], tok[:, 4, :], True, True, ["NN1", "tok"], ["ps2"])
                        cp(Wt[0][:, 0:64], tok[:, 0, :], ["tok"], ["Wt0"], q="act")
                        cp(Wt[0][:, 64:128], ps[2][:, 128:192], ["ps2"], ["Wt0"])
                        wc = 0
                        for j_ in range(7):
                            mm(ps[4][:, 0:128], ident_c[:], Wt[wc][:], True, False, [identc_k, f"Wt{wc}"], ["ps4"])
                            mm(ps[4][:, 0:128], NX[cur][:, 0:128], Wt[wc][:], False, True, [f"NX{cur}", f"Wt{wc}"], ["ps4"])
                            cp(Wt[1 - wc][:], ps[4][:, 0:128], ["ps4"], [f"Wt{1 - wc}"], q="act")
                            wc = 1 - wc
                            if j_ < 6:
                                mm(ps[5][:, 0:128], NX[cur][:, 128:256], NX[cur][:, 0:128], True, True, [f"NX{cur}"], ["ps5"])
                                mm(ps[5][:, 128:256], NX[cur][:, 0:128], NX[cur][:, 128:256], True, True, [f"NX{cur}"], ["ps5"])
                                cp(NX[1 - cur][:], ps[5][:, 0:256], ["ps5"], [f"NX{1 - cur}"])
                                cur = 1 - cur
                        W = Wt[wc]
                        wk = f"Wt{wc}"
                        mm(ps[0][0:64, 0:128], tok[:, 3, :], ident_c[:], True, False, ["tok", identc_k], ["ps0"])
                        mm(ps[0][0:64, 0:128], W[:, 0:64], NN[0][:, 128:256], False, True, [wk, "NN0"], ["ps0"])
                        cp(RhT[:], ps[0][0:64, 0:128], ["ps0"], ["RhT"], q="act")
                        mm(ps[1][:, 0:64], RhT[:], STb[:, h, :], True, False, ["RhT", "STb"], ["ps1"])
                        mm(ps[1][:, 0:64], NN[0][:, 128:256], W[:, 64:128], False, False, ["NN0", wk], ["ps1"])
                        mm(ps[1][:, 0:64], NN[1][:, 128:256], tok[:, 4, :], False, True, ["NN1", "tok"], ["ps1"])
                        cp(o_tok[:, ci, h * 64:(h + 1) * 64], ps[1][:, 0:64], ["ps1"], ["o_tok"])
                        mm(ps[2][0:64, 0:64], W[:, 0:64], tok[:, 1, :], True, True, [wk, "tok"], ["ps2"])
                        cp(GB[:], ps[2][0:64, 0:64], ["ps2"], ["GB"], q="act")
                        mm(ps[4][0:64, 0:64], GB[:], STb[:, h, :], True, False, ["GB", "STb"], ["ps4"])
                        mm(ps[4][0:64, 0:64], tok[:, 1, :], W[:, 64:128], False, False, ["tok", wk], ["ps4"])
                        mm(ps[4][0:64, 0:64], tok[:, 2, :], tok[:, 4, :], False, True, ["tok"], ["ps4"])
                        stt(ST[:, h, :], ST[:, h, :], pc[:, ci:ci + 1], ps[4][0:64, 0:64], ALU.mult, ALU.add, ["ST", "pc", "ps4"], ["ST"])
                        cp(STb[:, h, :], ST[:, h, :], ["ST"], ["STb"], q="act")
                for h2 in range(0, 16, 2):
                    lists = []
                    for sid in range(2):
                        P.cap = []
                        P.kmap = mk_kmap(h2 + sid, sid)
                        head_body(h2 + sid, sid)
                        lists.append(P.cap)
                        P.cap = None
                        P.kmap = None
                    P.merge(lists)
                for ci in range(NCB):
                    r0 = t0 + ci * CH
                    if d_ == 0:
                        dma("dsp", of_d[r0:r0 + CH, :], o_tok[:, ci, :], [*OT], ["of_d"])
                        dma("dsp", m_d[r0:r0 + CH, 0:RW], kb_tok[:, ci, :], [*KBT], ["m_d"])
                    else:
                        dma("dsp", of_t[:], of_d[r0:r0 + CH, :], ["of_d"], ["of_t"])
                        tt(o_tok[:, ci, :], o_tok[:, ci, :], of_t[:], ALU.add, [*OT, "of_t"], [*OT])
                        dma("dsp", of_t[:], m_d[r0:r0 + CH, 0:RW], ["m_d"], ["of_t"])
                        tt(kb_tok[:, ci, :], kb_tok[:, ci, :], of_t[:], ALU.add, [*KBT, "of_t"], [*KBT])
                        o3 = o_tok[:, ci, :].rearrange("p (h v) -> p h v", v=64)
                        P.add("dve", lambda e, o3=o3: e.tensor_reduce(out=st8[:], in_=o3, axis=AX.X, op=ALU.add), [*OT], ["st8"])
                        ts(st8[:], st8[:], 1.0 / 64, None, ALU.mult, None, ["st8"], ["st8"])
                        tt(o3, o3, st8[:].unsqueeze(2).to_broadcast([128, 16, 64]), ALU.subtract, [*OT, "st8"], [*OT])
                        tt(onr[:], o_tok[:, ci, :], o_tok[:, ci, :], ALU.mult, [*OT], ["onr"])
                        P.add("dve", lambda e: e.tensor_reduce(out=st9[:], in_=onr[:].rearrange("p (h v) -> p h v", v=64), axis=AX.X, op=ALU.add), ["onr"], ["st9"])
                        ts(st9[:], st9[:], 1.0 / 64, 64e-5, ALU.mult, ALU.add, ["st9"], ["st9"])
                        act(st9[:], st9[:], AF.Sqrt, ["st9"], ["st9"])
                        P.add("dve", lambda e: e.reciprocal(out=st9[:], in_=st9[:]), ["st9"], ["st9"])
                        tt(o3, o3, st9[:].unsqueeze(2).to_broadcast([128, 16, 64]), ALU.mult, [*OT, "st9"], [*OT])
                        tt(o_tok[:, ci, :], o_tok[:, ci, :], lnw_b[:], ALU.mult, [*OT, "lnw_b"], [*OT])
                        tt(o_tok[:, ci, :], o_tok[:, ci, :], lnb_b[:], ALU.add, [*OT, "lnb_b"], [*OT])
                        P.add("dve", lambda e, ci=ci: e.tensor_reduce(out=st8[:], in_=kb_tok[:, ci, :].rearrange("p (h v) -> p h v", v=64), axis=AX.X, op=ALU.add), [*KBT], ["st8"])
                        tt(onr[:].rearrange("p (h v) -> p h v", v=64), v_tok32[:, ci, :].rearrange("p (h v) -> p h v", v=64),
                           st8[:].unsqueeze(2).to_broadcast([128, 16, 64]), ALU.mult, [*VTT, "st8"], ["onr"])
                        tt(o_tok[:, ci, :], o_tok[:, ci, :], onr[:], ALU.add, [*OT, "onr"], [*OT])
                        tt(o_tok[:, ci, :], o_tok[:, ci, :], g_tok[:, ci, :], ALU.mult, [*OT, "g_tok"], [*OT])
                        dma("dsp", m_d[r0:r0 + CH, 0:RW], o_tok[:, ci, :], [*OT], ["m_d"])
    P.barrier()
    P.pop()
    TB = TB_saved

    if DBG == "p3":
        print("ops after P3:", len(P.ops))
        P.limit = None
        dbg = dout("dbg", [T, 2048])
        dma("dsp", dbg, m_d, (), ["dbg"])
        P.barrier()
        P.emit()
        return nc

    hg_gamma = din("hg_gamma", [2, 2, HG])
    hg_norm_w = din("hg_norm_w", [1, HG])
    P.push()
    C2 = 64
    NC2 = TB // C2
    gm = P.sb([128, 2, 2, 8], F32, "gm")
    for l_ in range(2):
        for d_ in range(2):
            dma("dsp", gm[:, l_, d_, :], hg_gamma[l_, d_, :].rearrange("(h p) -> p h", p=128), (), ["gm"], allow_slow_non_contiguous=True)
    lbT = P.sb([128, 2, 8], F32, "lbT")
    omlb = P.sb([128, 2, 8], F32, "omlb")
    tt(lbT[:], gm[:, 0, :, :], gm[:, 1, :, :], ALU.subtract, ["gm"], ["lbT"])
    act(lbT[:], lbT[:], AF.Sigmoid, ["lbT"], ["lbT"])
    ts(omlb[:], lbT[:], -1.0, 1.0, ALU.mult, ALU.add, ["lbT"], ["omlb"])
    hnw = P.sb([64, HG], F32, "hnw")
    dma("dsp", hnw[:], hg_norm_w[0:1, :].partition_broadcast(64), (), ["hnw"])
    ones2 = P.sb([128, 64], F32, "ones2")
    memset(ones2[:], 1.0, ["ones2"])
    MI = [P.sb([64, 64], F32, f"mi{d_}") for d_ in range(2)]
    for d_ in range(2):
        sgn = 1 if d_ == 0 else -1
        memset(MI[d_][:], 1.0, [f"mi{d_}"])
        P.add("pool", lambda e, d_=d_, sgn=sgn: e.affine_select(out=MI[d_][:], in_=MI[d_][:], pattern=[[sgn, 64]],
              compare_op=ALU.is_ge, fill=0.0, base=0, channel_multiplier=-sgn), [f"mi{d_}"], [f"mi{d_}"])
    S2 = P.sb([128, 8, 128], F32, "S2")
    qz = P.sb([128, TB], F32, "qz")
    fz = P.sb([128, TB], F32, "fz")
    lf = P.sb([128, TB], F32, "lf")
    kq = P.sb([128, TB], F32, "kq")
    b2 = P.sb([128, TB], F32, "b2")
    e2 = P.sb([128, TB], F32, "e2")
    qT = P.sb([128, TB], F32, "qT")
    kT2 = P.sb([128, TB], F32, "kT2")
    khT = P.sb([128, TB], F32, "khT")
    bc2 = P.sb([128, NC2], F32, "bc2")
    pc2 = P.sb([128, NC2], F32, "pc2")
    vt2 = P.sb([64, NC2, 128], F32, "vt2")
    gt2_ = P.sb([64, NC2, HG], F32, "gt2")
    o2 = P.sb([64, NC2, HG], F32, "o2")
    of2 = P.sb([64, HG], F32, "of2")
    sc2_ = P.sb([64, 64], F32, "sc2")
    kh_tok = P.sb([64, 128], F32, "kh_tok")
    sq2 = P.sb([64, HG], F32, "sq2")
    st2 = P.sb([64, 8], F32, "st2")
    for d_ in range(2):
        for (s0, s1) in seq_bounds:
            nblk = (s1 - s0) // TB
            memset(S2[:], 0.0, ["S2"])
            blks = range(nblk) if d_ == 0 else range(nblk - 1, -1, -1)
            for bi in blks:
                t0 = s0 + bi * TB
                if d_ == 1:
                    for ci in range(NC2):
                        dma("dsp", gt2_[:, ci, :], ztok_d[t0 + ci * C2:t0 + (ci + 1) * C2, 1024:2048], (), ["gt2"])
                for h in range(8):
                    dma("dsp", qz[:], zT_d[3456 + h * 128:3456 + (h + 1) * 128, t0:t0 + TB], (), ["qz"])
                    dma("dsp", fz[:], zT_d[4480 + d_ * 1024 + h * 128:4480 + d_ * 1024 + (h + 1) * 128, t0:t0 + TB], (), ["fz"])
                    for ci in range(NC2):
                        dma("dsp", vt2[:, ci, :], ztok_d[t0 + ci * C2:t0 + (ci + 1) * C2, h * 128:(h + 1) * 128], (), ["vt2"])
                    act(qz[:], qz[:], AF.Silu, ["qz"], ["qz"])
                    act(fz[:], fz[:], AF.Sigmoid, ["fz"], ["fz"])
                    ts(fz[:], fz[:], omlb[:, d_, h:h + 1], lbT[:, d_, h:h + 1], ALU.mult, ALU.add, ["fz", "omlb", "lbT"], ["fz"])
                    act(lf[:], fz[:], AF.Ln, ["fz"], ["lf"])
                    ts(kq[:], fz[:], -1.0, 1.0, ALU.mult, ALU.add, ["fz"], ["kq"])
                    for ci in range(NC2):
                        sl = slice(ci * C2, (ci + 1) * C2)
                        P.add("dve", lambda e, sl=sl: e.tensor_tensor_scan(out=b2[:, sl], data0=ones2[:, 0:C2], data1=lf[:, sl],
                              initial=0.0, op0=ALU.mult, op1=ALU.add), ["ones2", "lf"], ["b2"])
                        cp(bc2[:, ci:ci + 1], b2[:, ci * C2 + C2 - 1:ci * C2 + C2], ["b2"], ["bc2"])
                        if d_ == 1:
                            tt(b2[:, sl], lf[:, sl], b2[:, sl], ALU.subtract, ["lf", "b2"], ["b2"])
                            ts(b2[:, sl], b2[:, sl], bc2[:, ci:ci + 1], None, ALU.add, None, ["b2", "bc2"], ["b2"])
                    act(pc2[:], bc2[:], AF.Exp, ["bc2"], ["pc2"])
                    act(e2[:], b2[:], AF.Exp, ["b2"], ["e2"])
                    tt(qT[:], qz[:], e2[:], ALU.mult, ["qz", "e2"], ["qT"])
                    act(e2[:], b2[:], AF.Exp, ["b2"], ["e2"], scale=-1.0)
                    tt(kT2[:], kq[:], e2[:], ALU.mult, ["kq", "e2"], ["kT2"])
                    for ci in range(NC2):
                        sl = slice(ci * C2, (ci + 1) * C2)
                        act(e2[:, sl], b2[:, sl], AF.Exp, ["b2", "bc2"], ["e2"], scale=-1.0, bias=bc2[:, ci:ci + 1])
                    tt(khT[:], kq[:], e2[:], ALU.mult, ["kq", "e2"], ["khT"])
                    cis = range(NC2) if d_ == 0 else range(NC2 - 1, -1, -1)
                    for ci in cis:
                        sl = slice(ci * C2, (ci + 1) * C2)
                        mm(ps[0][0:64, 0:64], kT2[:, sl], qT[:, sl], True, True, ["kT2", "qT"], ["ps0"])
                        tt(sc2_[:], ps[0][0:64, 0:64], MI[d_][:], ALU.mult, ["ps0", f"mi{d_}"], ["sc2"])
                        tr(ps[1][0:64, 0:128], khT[:, sl], ident_f[:], ["khT", "identf"], ["ps1"])
                        cp(kh_tok[:], ps[1][0:64, 0:128], ["ps1"], ["kh_tok"], q="act")
                        mm(ps[2][0:64, 0:128], sc2_[:], vt2[:, ci, :], True, False, ["sc2", "vt2"], ["ps2"])
                        mm(ps[2][0:64, 0:128], qT[:, sl], S2[:, h, :], False, True, ["qT", "S2"], ["ps2"])
                        cp(o2[:, ci, h * 128:(h + 1) * 128], ps[2][0:64, 0:128], ["ps2"], ["o2"], q="act")
                        mm(ps[4][:, 0:128], kh_tok[:], vt2[:, ci, :], True, True, ["kh_tok", "vt2"], ["ps4"])
                        stt(S2[:, h, :], S2[:, h, :], pc2[:, ci:ci + 1], ps[4][:, 0:128], ALU.mult, ALU.add, ["S2", "pc2", "ps4"], ["S2"])
                for ci in range(NC2):
                    r0 = t0 + ci * C2
                    if d_ == 0:
                        dma("dsp", of_d[r0:r0 + C2, :], o2[:, ci, :], ["o2"], ["of_d"])
                    else:
                        dma("dsp", of2[:], of_d[r0:r0 + C2, :], ["of_d"], ["of2"])
                        tt(o2[:, ci, :], o2[:, ci, :], of2[:], ALU.add, ["o2", "of2"], ["o2"])
                        tt(sq2[:], o2[:, ci, :], o2[:, ci, :], ALU.mult, ["o2"], ["sq2"])
                        P.add("dve", lambda e: e.tensor_reduce(out=st2[:], in_=sq2[:].rearrange("p (h v) -> p h v", v=128), axis=AX.X, op=ALU.add), ["sq2"], ["st2"])
                        ts(st2[:], st2[:], 1.0 / 128, 1e-6, ALU.mult, ALU.add, ["st2"], ["st2"])
                        act(st2[:], st2[:], AF.Sqrt, ["st2"], ["st2"])
                        P.add("dve", lambda e: e.reciprocal(out=st2[:], in_=st2[:]), ["st2"], ["st2"])
                        o3 = o2[:, ci, :].rearrange("p (h v) -> p h v", v=128)
                        tt(o3, o3, st2[:].unsqueeze(2).to_broadcast([64, 8, 128]), ALU.mult, ["o2", "st2"], ["o2"])
                        tt(o2[:, ci, :], o2[:, ci, :], hnw[:], ALU.mult, ["o2", "hnw"], ["o2"])
                        act(gt2_[:, ci, :], gt2_[:, ci, :], AF.Silu, ["gt2"], ["gt2"])
                        tt(o2[:, ci, :], o2[:, ci, :], gt2_[:, ci, :], ALU.mult, ["o2", "gt2"], ["o2"])
                        dma("dsp", m_d[r0:r0 + C2, 1024:2048], o2[:, ci, :], ["o2"], ["m_d"])
    P.barrier()
    P.pop()

    if DBG == "p4":
        dbg = dout("dbg", [T, 2048])
        dma("dsp", dbg, m_d, (), ["dbg"])
        P.barrier()
        P.emit()
        return nc

    w_out = din("w_out", [D, D])
    x1_d = dscr("x1_d", [T, D])
    P.push()
    wo = P.sb([128, 16, D], BF16, "wo")
    for k in range(16):
        dma("dpool", wo[:, k, :], w_out[k * 128:(k + 1) * 128, :], (), ["wo"])
    gn1 = [P.sb([128, D], F32, f"gn1_{s_}") for s_ in range(2)]
    for s_ in range(2):
        dma("dsp", gn1[s_][:], vec_d[2, s_:s_ + 1, :].partition_broadcast(128), (), [f"gn1_{s_}"])
    mt = [P.sb([128, D], F32, f"mt{i}") for i in range(2)]
    mTs = [P.sb([128, 16, 128], BF16, f"mTs{i}") for i in range(2)]
    mo = [P.sb([128, D], F32, f"mo{i}") for i in range(2)]
    xr = [P.sb([128, D], F32, f"xr{i}") for i in range(2)]
    junk5 = P.sb([128, D], F32, "junk5")
    ss5 = [P.sb([128, 1], F32, f"ss5{i}") for i in range(2)]
    for t in range(NT):
        b = t % 2
        s_ = seq_of_tile(t)
        dma("dsp", mt[b][:], m_d[t * 128:(t + 1) * 128, :], (), [f"mt{b}"])
        dma("dsp", xr[b][:], x[t * 128:(t + 1) * 128, :], (), [f"xr{b}"])
        for kg in range(4):
            pk = 4 + kg
            for k4 in range(4):
                k = kg * 4 + k4
                tr(ps[pk][:, k4 * 128:(k4 + 1) * 128], mt[b][:, k * 128:(k + 1) * 128], ident_f[:], [f"mt{b}", "identf"], [f"ps{pk}"])
            cp(mTs[b][:, kg * 4:(kg + 1) * 4, :].rearrange("p a b -> p (a b)"), ps[pk][:, :], [f"ps{pk}"], [f"mTs{b}"], q=("act" if kg % 2 else "dve"))
        for cg in range(4):
            for k in range(16):
                mm(ps[cg][:, :], mTs[b][:, k, :], wo[:, k, cg * 512:(cg + 1) * 512], k == 0, k == 15, [f"mTs{b}", "wo"], [f"ps{cg}"])
            cp(mo[b][:, cg * 512:(cg + 1) * 512], ps[cg][:, :], [f"ps{cg}"], [f"mo{b}"], q=("act" if cg % 2 else "dve"))
        act(junk5[:], mo[b][:], AF.Square, [f"mo{b}"], ["junk5", f"ss5{b}"], accum_out=ss5[b][:])
        ts(ss5[b][:], ss5[b][:], 1.0 / D, 1e-6, ALU.mult, ALU.add, [f"ss5{b}"], [f"ss5{b}"])
        act(ss5[b][:], ss5[b][:], AF.Sqrt, [f"ss5{b}"], [f"ss5{b}"])
        P.add("dve", lambda e, b=b: e.reciprocal(out=ss5[b][:], in_=ss5[b][:]), [f"ss5{b}"], [f"ss5{b}"])
        stt(mo[b][:], mo[b][:], ss5[b][:], gn1[s_][:], ALU.mult, ALU.mult, [f"mo{b}", f"ss5{b}", f"gn1_{s_}"], [f"mo{b}"])
        tt(mo[b][:], mo[b][:], xr[b][:], ALU.add, [f"mo{b}", f"xr{b}"], [f"mo{b}"])
        dma("dsp", x1_d[t * 128:(t + 1) * 128, :], mo[b][:], [f"mo{b}"], ["x1_d"])
    P.barrier()
    P.pop()

    w_router = din("w_router", [D, NE])
    e_bias = din("e_bias", [1, NE])
    h2T_d = dscr("h2T_d", [16, 128, T], BF16)
    wselT_d = dscr("wselT_d", [NE, T])
    P.push()
    g2 = [P.sb([128, D], F32, f"g2_{s_}") for s_ in range(2)]
    s2 = [P.sb([128, D], F32, f"s2_{s_}") for s_ in range(2)]
    for s_ in range(2):
        dma("dsp", g2[s_][:], vec_d[3, s_:s_ + 1, :].partition_broadcast(128), (), [f"g2_{s_}"])
        dma("dsp", s2[s_][:], vec_d[4, s_:s_ + 1, :].partition_broadcast(128), (), [f"s2_{s_}"])
    wr = P.sb([128, 16, NE], F32, "wr")
    dma("dsp", wr[:], w_router.rearrange("(k p) e -> p k e", p=128), (), ["wr"])
    eb = P.sb([128, NE], F32, "eb")
    dma("dsp", eb[:], e_bias[0:1, :].partition_broadcast(128), (), ["eb"])
    x6 = [P.sb([128, D], F32, f"x6{i}") for i in range(2)]
    junk6 = P.sb([128, D], F32, "junk6")
    hTf = [P.sb([128, 16, 128], F32, f"hTf{i}") for i in range(2)]
    hTb6 = [P.sb([128, 16, 128], BF16, f"hTb6{i}") for i in range(2)]
    ss6 = [P.sb([128, 1], F32, f"ss6{i}") for i in range(2)]
    scr = P.sb([128, NE], F32, "scr")
    bi = P.sb([128, NE], F32, "bi")
    m8 = P.sb([128, 8, 8], F32, "m8")
    gs = P.sb([128, 8], F32, "gs")
    g8 = P.sb([128, 8], F32, "g8")
    gmask = P.sb([128, 8], F32, "gmask")
    emask = P.sb([128, NE], F32, "emask")
    mk = P.sb([128, NE], F32, "mk")
    tmk = P.sb([128, NE], F32, "tmk")
    t8 = P.sb([128, 8], F32, "t8")
    sel = P.sb([128, NE], F32, "sel")
    wsl = P.sb([128, NE], F32, "wsl")
    wsum = P.sb([128, 1], F32, "wsum")
    wT = P.sb([64, 128], F32, "wT")
    for t in range(NT):
        b = t % 2
        s_ = seq_of_tile(t)
        dma("dsp", x6[b][:], x1_d[t * 128:(t + 1) * 128, :], (), [f"x6{b}"])
        act(junk6[:], x6[b][:], AF.Square, [f"x6{b}"], ["junk6", f"ss6{b}"], accum_out=ss6[b][:])
        ts(ss6[b][:], ss6[b][:], 1.0 / D, 1e-6, ALU.mult, ALU.add, [f"ss6{b}"], [f"ss6{b}"])
        act(ss6[b][:], ss6[b][:], AF.Sqrt, [f"ss6{b}"], [f"ss6{b}"])
        P.add("dve", lambda e, b=b: e.reciprocal(out=ss6[b][:], in_=ss6[b][:]), [f"ss6{b}"], [f"ss6{b}"])
        stt(x6[b][:], x6[b][:], ss6[b][:], g2[s_][:], ALU.mult, ALU.mult, [f"x6{b}", f"ss6{b}", f"g2_{s_}"], [f"x6{b}"])
        tt(x6[b][:], x6[b][:], s2[s_][:], ALU.add, [f"x6{b}", f"s2_{s_}"], [f"x6{b}"])
        for kg in range(4):
            for kk2 in range(4):
                k = kg * 4 + kk2
                tr(ps[kg][:, kk2 * 128:(kk2 + 1) * 128], x6[b][:, k * 128:(k + 1) * 128], ident_f[:], [f"x6{b}", "identf"], [f"ps{kg}"])
            cp(hTf[b][:, kg * 4:(kg + 1) * 4, :].rearrange("p a b -> p (a b)"), ps[kg][:, :], [f"ps{kg}"], [f"hTf{b}"], q="act")
            cp(hTb6[b][:, kg * 4:(kg + 1) * 4, :].rearrange("p a b -> p (a b)"), ps[kg][:, :], [f"ps{kg}"], [f"hTb6{b}"])
        dma("dsp", h2T_d[:, :, t * 128:(t + 1) * 128].rearrange("k p t -> p k t"), hTb6[b][:], [f"hTb6{b}"], ["h2T_d"])
        for k in range(16):
            mm(ps[4][:, 0:NE], hTf[b][:, k, :], wr[:, k, :], k == 0, k == 15, [f"hTf{b}", "wr"], ["ps4"])
        act(scr[:], ps[4][:, 0:NE], AF.Sigmoid, ["ps4"], ["scr"])
        tt(bi[:], scr[:], eb[:], ALU.add, ["scr", "eb"], ["bi"])
        for g_ in range(8):
            P.add("dve", lambda e, g_=g_: e.max(out=m8[:, g_, :], in_=bi[:, g_ * 8:(g_ + 1) * 8]), ["bi"], ["m8"])
        tt(gs[:], m8[:, :, 0], m8[:, :, 1], ALU.add, ["m8"], ["gs"])
        P.add("dve", lambda e: e.max(out=g8[:], in_=gs[:]), ["gs"], ["g8"])
        ts(gmask[:], gs[:], g8[:, 3:4], None, ALU.is_ge, None, ["gs", "g8"], ["gmask"])
        cp(emask[:].rearrange("p (g j) -> p g j", j=8), gmask[:].unsqueeze(2).to_broadcast([128, 8, 8]), ["gmask"], ["emask"])
        ts(tmk[:], emask[:], 10.0, -10.0, ALU.mult, ALU.add, ["emask"], ["tmk"])
        tt(mk[:], bi[:], emask[:], ALU.mult, ["bi", "emask"], ["mk"])
        tt(mk[:], mk[:], tmk[:], ALU.add, ["mk", "tmk"], ["mk"])
        P.add("dve", lambda e: e.max(out=t8[:], in_=mk[:]), ["mk"], ["t8"])
        ts(sel[:], mk[:], t8[:, 5:6], None, ALU.is_ge, None, ["mk", "t8"], ["sel"])
        tt(wsl[:], scr[:], sel[:], ALU.mult, ["scr", "sel"], ["wsl"])
        P.add("dve", lambda e: e.tensor_reduce(out=wsum[:], in_=wsl[:], axis=AX.X, op=ALU.add), ["wsl"], ["wsum"])
        P.add("dve", lambda e: e.reciprocal(out=wsum[:], in_=wsum[:]), ["wsum"], ["wsum"])
        ts(wsl[:], wsl[:], wsum[:], 2.5, ALU.mult, ALU.mult, ["wsl", "wsum"], ["wsl"])
        tr(ps[5][0:64, 0:128], wsl[:], ident_f[:], ["wsl", "identf"], ["ps5"])
        cp(wT[:], ps[5][0:64, 0:128], ["ps5"], ["wT"], q="act")
        dma("dsp", wselT_d[:, t * 128:(t + 1) * 128], wT[:], ["wT"], ["wselT_d"])
    P.barrier()
    P.pop()

    if DBG == "p6":
        dbg = dout("dbg", [NE, T])
        dma("dsp", dbg, wselT_d, (), ["dbg"])
        P.barrier()
        P.emit()
        return nc

    NEC = DE // 128
    TBM = TB
    NTI = TBM // 128
    P.push()
    h2b = P.sb([128, 16, TBM], BF16, "h2b")
    wgt = [P.sb([128, 16, DE], BF16, f"wgt{i}") for i in range(2)]
    wut = [P.sb([128, 16, DE], BF16, f"wut{i}") for i in range(2)]
    wdt = [P.sb([128, NEC, D], BF16, f"wdt{i}") for i in range(2)]
    wbt = [P.sb([128, TBM], F32, f"wbt{i}") for i in range(2)]
    yacc = P.sb([128, NTI, D], F32, "yacc")
    actT = [P.sb([128, NEC, TBM], BF16, f"actT{i}") for i in range(2)]
    sgt = [P.sb([128, TBM], F32, f"sgt{i}") for i in range(2)]
    a1t = [P.sb([128, TBM], F32, f"a1t{i}") for i in range(2)]
    gn2 = P.sb([128, D], F32, "gn2")
    x7 = P.sb([128, D], F32, "x7")
    ss7 = P.sb([128, 1], F32, "ss7")
    junk7 = P.sb([128, D], F32, "junk7")

    def wsrc(e):
        return ("persist:wg%d" % e, "persist:wu%d" % e, "persist:wd%d" % e)

    def load_w(e, par):
        kg_, ku_, kd_ = wsrc(e)
        dma("dsp", wgt[par][:].rearrange("p k n -> p (k n)"), wg_b[e], [kg_], [f"wgt{par}"])
        dma("dsp", wut[par][:].rearrange("p k n -> p (k n)"), wu_b[e], [ku_], [f"wut{par}"])
        dma("dsp", wdt[par][:].rearrange("p k n -> p (k n)"), wd_b[e], [kd_], [f"wdt{par}"])

    order = [NE] + list(range(NE))
    gi = 0
    for tb in range(T // TBM):
        t0 = tb * TBM
        s_ = 0 if t0 < LP else 1
        dma("dsp", h2b[:], h2T_d[:, :, t0:t0 + TBM].rearrange("k p t -> p k t"), (), ["h2b"])
        dma("dsp", gn2[:], vec_d[5, s_:s_ + 1, :].partition_broadcast(128), (), ["gn2"])
        load_w(order[0], gi % 2)

        def GU(i, e, par):
            if e < NE:
                dma("dsp", wbt[par][:], wselT_d[e:e + 1, t0:t0 + TBM].partition_broadcast(128), (), [f"wbt{par}"])
            for ec in range(NEC):
                pg = ps[ec % 2]
                pu = ps[2 + ec % 2]
                kg_ = f"ps{ec % 2}"
                ku_ = f"ps{2 + ec % 2}"
                for k in range(16):
                    mm(pg[:, 0:TBM], wgt[par][:, k, ec * 128:(ec + 1) * 128], h2b[:, k, :], k == 0, k == 15, [f"wgt{par}", "h2b"], [kg_])
                for k in range(16):
                    mm(pu[:, 0:TBM], wut[par][:, k, ec * 128:(ec + 1) * 128], h2b[:, k, :], k == 0, k == 15, [f"wut{par}", "h2b"], [ku_])
                sb_ = ec % 2
                act(sgt[sb_][:], pg[:, 0:TBM], AF.Silu, [kg_], [f"sgt{sb_}"])
                if e < NE:
                    tt(a1t[sb_][:], sgt[sb_][:], pu[:, 0:TBM], ALU.mult, [f"sgt{sb_}", ku_], [f"a1t{sb_}"])
                    tt(actT[par][:, ec, :], a1t[sb_][:], wbt[par][:], ALU.mult, [f"a1t{sb_}", f"wbt{par}"], [f"actT{par}"])
                else:
                    tt(actT[par][:, ec, :], sgt[sb_][:], pu[:, 0:TBM], ALU.mult, [f"sgt{sb_}", ku_], [f"actT{par}"])

        def DN(i, e, par, first):
            j = 0
            for ti in range(NTI):
                for cg in range(4):
                    pd = ps[4 + j % 2]
                    kd_ = f"ps{4 + j % 2}"
                    j += 1
                    for ec in range(NEC):
                        mm(pd[:, :], actT[par][:, ec, ti * 128:(ti + 1) * 128], wdt[par][:, ec, cg * 512:(cg + 1) * 512],
                           ec == 0, ec == NEC - 1, [f"actT{par}", f"wdt{par}"], [kd_])
                    if first:
                        cp(yacc[:, ti, cg * 512:(cg + 1) * 512], pd[:, :], [kd_], ["yacc"])
                    else:
                        tt(yacc[:, ti, cg * 512:(cg + 1) * 512], yacc[:, ti, cg * 512:(cg + 1) * 512], pd[:, :], ALU.add, ["yacc", kd_], ["yacc"])

        n_e = len(order)
        for i, e in enumerate(order):
            par = (gi + i) % 2
            GU(i, e, par)
            if i > 0:
                DN(i - 1, order[i - 1], (gi + i - 1) % 2, i - 1 == 0)
            if i + 1 < n_e:
                load_w(order[i + 1], (gi + i + 1) % 2)
        DN(n_e - 1, order[-1], (gi + n_e - 1) % 2, False)
        gi += n_e
        for ti in range(NTI):
            r0 = t0 + ti * 128
            dma("dsp", x7[:], x1_d[r0:r0 + 128, :], (), ["x7"])
            P.add("dve", lambda e, ti=ti: e.tensor_tensor(out=junk7[:], in0=yacc[:, ti, :], in1=yacc[:, ti, :], op=ALU.mult), ["yacc"], ["junk7"])
            P.add("dve", lambda e: e.tensor_reduce(out=ss7[:], in_=junk7[:], axis=AX.X, op=ALU.add), ["junk7"], ["ss7"])
            ts(ss7[:], ss7[:], 1.0 / D, 1e-6, ALU.mult, ALU.add, ["ss7"], ["ss7"])
            act(ss7[:], ss7[:], AF.Sqrt, ["ss7"], ["ss7"])
            P.add("dve", lambda e: e.reciprocal(out=ss7[:], in_=ss7[:]), ["ss7"], ["ss7"])
            stt(junk7[:], yacc[:, ti, :], ss7[:], gn2[:], ALU.mult, ALU.mult, ["yacc", "ss7", "gn2"], ["junk7"])
            tt(junk7[:], junk7[:], x7[:], ALU.add, ["junk7", "x7"], ["junk7"])
            dma("dsp", y[r0:r0 + 128, :], junk7[:], ["junk7"], ["y"])
    P.barrier()
    P.pop()


    P.barrier()
    P.emit()
    return nc


def kernel(**inp):
    from concourse.bass_utils import run_bass_kernel_spmd
    n = 8
    LP = inp["x_prompt"].shape[1]
    LS = inp["x_sample"].shape[1]
    nc = build_program(dict(LP=LP, LS=LS, DE=int(inp["w_exp_gate"].shape[-1])))
    f = lambda a: np.ascontiguousarray(np.asarray(a, dtype=np.float32))
    shared = dict(
        w_ada=f(inp["w_ada"][0]), b_ada=f(inp["b_ada"][0:1]),
        nrm=f(np.stack([inp["norm_pre_mix"][0], inp["norm_post_mix"][0], inp["norm_pre_ffn"][0], inp["norm_post_ffn"][0]])),
        w_in=f(inp["w_in"][0]), rw_mu=f(inp["rw_mu"][0]), rw_w0=f(inp["rw_w0"][0]), rw_a0=f(inp["rw_a0"][0]),
        rw_w_up=f(inp["rw_w_up"][0]), rw_a_up=f(inp["rw_a_up"][0]), rw_g_up=f(inp["rw_g_up"][0]),
        rw_vecs=f(np.stack([inp["rw_k_k"][0], inp["rw_k_a"][0], inp["rw_r_k"][0].reshape(-1), inp["rw_ln_w"][0], inp["rw_ln_b"][0]])),
        hg_gamma=f(inp["hg_lb_gamma"]), hg_norm_w=f(inp["hg_norm_w"][0:1]), w_out=f(inp["w_out"][0]),
        w_router=f(inp["w_router"][0]), e_bias=f(inp["e_bias"][0:1]),
        w_exp_gate=f(inp["w_exp_gate"][0]), w_exp_up=f(inp["w_exp_up"][0]), w_exp_down=f(inp["w_exp_down"][0]),
        w_sh_gate=f(inp["w_sh_gate"][0]), w_sh_up=f(inp["w_sh_up"][0]), w_sh_down=f(inp["w_sh_down"][0]),
    )
    in_maps = []
    for b in range(n):
        m = dict(shared)
        m["x"] = f(np.concatenate([inp["x_prompt"][b], inp["x_sample"][b]], axis=0))
        m["c"] = f(np.stack([inp["c_prompt"][b], inp["c_sample"][b]]))
        in_maps.append(m)
    res = run_bass_kernel_spmd(nc, in_maps, core_ids=list(range(n)))
    ys = [r["y"] for r in res.results]
    y_prompt = np.stack([yy[:LP] for yy in ys]).astype(np.float32)
    y_sample = np.stack([yy[LP:] for yy in ys]).astype(np.float32)
    return (y_prompt, y_sample)
```

```python
import numpy as np
import concourse.bass as bass
import concourse.mybir as mybir

F32 = mybir.dt.float32
BF16 = mybir.dt.bfloat16
I32 = mybir.dt.int32
AF = mybir.ActivationFunctionType
ALU = mybir.AluOpType
AX = mybir.AxisListType

NDMASEM = 8


class Op:
    __slots__ = ("q", "fn", "deps", "signals", "sem", "ticket", "idx", "prewait", "isbar")

    def __init__(self, q, fn):
        self.q = q
        self.fn = fn
        self.deps = []
        self.signals = False
        self.sem = None
        self.ticket = 0
        self.prewait = None
        self.isbar = False


class Prog:
    ENG = {"pe": "pe", "act": "act", "dve": "dve", "pool": "pool",
           "dsp": "sp", "dpool": "pool", "dact": "act", "dpool2": "pool"}
    DMAQ = ("dsp", "dpool", "dact", "dpool2")

    def __init__(self, nc):
        self.nc = nc
        self.ops = []
        self.last_w = {}
        self.readers = {}
        self.sb_cur = 16512
        self.sb_mark = []
        self.uid = 0
        self.limit = None
        self.cap = None
        self.kmap = None

    def sb(self, shape, dtype, name=None):
        self.uid += 1
        nm = f"{name or 't'}_{self.uid}"
        esz = {F32: 4, BF16: 2, I32: 4, mybir.dt.uint32: 4, mybir.dt.uint16: 2}[dtype]
        per = int(np.prod(shape[1:])) * esz
        per = (per + 31) // 32 * 32
        off = self.sb_cur
        assert off + per <= 229344, f"SBUF overflow allocating {nm}: {off}+{per}"
        self.sb_cur += per
        return self.nc.alloc_sbuf_tensor_at(nm, list(shape), dtype, offset=off)

    def push(self):
        self.sb_mark.append(self.sb_cur)

    def pop(self):
        self.sb_cur = self.sb_mark.pop()

    def add(self, q, fn, reads=(), writes=()):
        if self.kmap is not None:
            reads = [self.kmap.get(k, k) for k in reads]
            writes = [self.kmap.get(k, k) for k in writes]
        if self.cap is not None:
            self.cap.append((q, fn, tuple(reads), tuple(writes)))
            return None
        if self.limit is not None and len(self.ops) >= self.limit:
            return None
        op = Op(q, fn)
        op.idx = len(self.ops)
        if self.limit is not None:
            import sys as _s
            f = _s._getframe(1)
            ln = []
            while f is not None and len(ln) < 3:
                ln.append(f.f_lineno)
                f = f.f_back
            self.lines = getattr(self, "lines", {})
            self.lines[op.idx] = (q, ln)
        deps = set()
        for k in reads:
            if k in self.last_w:
                deps.add(self.last_w[k])
            if isinstance(k, str) and k.startswith("ps"):
                for r in self.readers.get(k, ()):
                    if self.ops[r].q != q:
                        deps.add(r)
        for k in writes:
            if k in self.last_w:
                deps.add(self.last_w[k])
            for r in self.readers.get(k, ()):
                deps.add(r)
        deps.discard(op.idx)
        if q == "pe":
            deps = {d for d in deps if self.ops[d].q != "pe"}
        op.deps = sorted(deps)
        for k in reads:
            self.readers.setdefault(k, []).append(op.idx)
        for k in writes:
            self.last_w[k] = op.idx
            self.readers[k] = []
        self.ops.append(op)
        return op

    def merge(self, lists):
        n = max(len(l) for l in lists)
        for i in range(n):
            for l in lists:
                if i < len(l):
                    self.add(*l[i])

    def barrier(self):
        op = Op("bar", None)
        op.idx = len(self.ops)
        op.isbar = True
        self.ops.append(op)
        self.last_w = {k: v for k, v in self.last_w.items() if isinstance(k, str) and k.startswith("persist:")}
        self.readers = {k: v for k, v in self.readers.items() if isinstance(k, str) and k.startswith("persist:")}

    def emit(self):
        nc = self.nc
        ops = self.ops
        queues = ["pe", "act", "dve", "pool", "dsp", "dpool", "dact", "dpool2"]
        hist = {q: [] for q in queues}
        bar_deps = {}
        for op in ops:
            if op.isbar:
                deps = []
                for q in ("pe", "act", "dve", "pool"):
                    deps += hist[q][-1:]
                for q in ("dsp", "dpool", "dact"):
                    deps += hist[q][-NDMASEM:]
                bar_deps[op.idx] = deps
                for d in deps:
                    ops[d].signals = True
            else:
                hist[op.q].append(op.idx)
        for op in ops:
            for d in op.deps:
                ops[d].signals = True
        sem_c = {q: nc.alloc_semaphore(f"s_{q}") for q in ("pe", "act", "dve", "pool")}
        sem_d = {q: [nc.alloc_semaphore(f"s_{q}{i}") for i in range(NDMASEM)]
                 for q in self.DMAQ}
        cnt_c = {q: 0 for q in sem_c}
        cnt_d = {q: [0] * NDMASEM for q in sem_d}
        rr = {q: 0 for q in sem_d}
        for op in ops:
            if op.isbar:
                continue
            if op.q in sem_c:
                if op.signals:
                    cnt_c[op.q] += 1
                    op.sem = sem_c[op.q]
                    op.ticket = cnt_c[op.q]
            else:
                i = rr[op.q]
                rr[op.q] = (i + 1) % NDMASEM
                op.prewait = (sem_d[op.q][i], cnt_d[op.q][i])
                cnt_d[op.q][i] += 16
                op.sem = sem_d[op.q][i]
                op.ticket = cnt_d[op.q][i]
                op.signals = True
        streams = {"pe": [], "act": [], "dve": [], "pool": [], "sp": []}
        for op in ops:
            if op.isbar:
                for e in streams:
                    streams[e].append(op)
            else:
                streams[self.ENG[op.q]].append(op)
        self.n_wait = 0

        def run_stream(eng, lst):
            waited = {}

            def w(sem, val):
                if val <= 0:
                    return
                if waited.get(sem.num, 0) >= val:
                    return
                eng.wait_ge(sem, val)
                self.n_wait += 1
                waited[sem.num] = val

            for op in lst:
                if op.isbar:
                    for d in bar_deps[op.idx]:
                        w(ops[d].sem, ops[d].ticket)
                    continue
                for d in op.deps:
                    w(ops[d].sem, ops[d].ticket)
                if op.prewait is not None:
                    w(*op.prewait)
                ins = op.fn(eng)
                if op.signals:
                    ins.then_inc(op.sem, 16 if op.q in self.DMAQ else 1)
            return waited

        with nc.Block() as block:
            @block.tensor
            def _(e):
                run_stream(e, streams["pe"])

            @block.scalar
            def _(e):
                run_stream(e, streams["act"])

            @block.vector
            def _(e):
                run_stream(e, streams["dve"])

            @block.gpsimd
            def _(e):
                run_stream(e, streams["pool"])

            @block.sync
            def _(e):
                run_stream(e, streams["sp"])
D = 2048
RW = 1024
HG = 1024
RWC = 3456
INC = 8576
NE = 64


def build_program(cfg):
    LP, LS = cfg["LP"], cfg["LS"]
    DE = cfg.get("DE", 512)
    DBG = cfg.get("debug", None)
    T = LP + LS
    NT = T // 128
    nc = bass.Bass("TRN2", target_bir_lowering=False)
    P = Prog(nc)
    P.limit = cfg.get("limit", None)
    global LAST_PROG
    LAST_PROG = P

    def din(name, shape, dt=F32):
        return nc.dram_tensor(name, list(shape), dt, kind="ExternalInput").ap()

    def dout(name, shape, dt=F32):
        return nc.dram_tensor(name, list(shape), dt, kind="ExternalOutput").ap()

    def dscr(name, shape, dt=F32):
        return nc.dram_tensor(name, list(shape), dt, kind="Internal").ap()

    x = din("x", [T, D])
    c = din("c", [2, D])
    w_ada = din("w_ada", [D, 6 * D])
    b_ada = din("b_ada", [1, 6 * D])
    nrm = din("nrm", [4, D])
    w_in = din("w_in", [D, INC])
    y = dout("y", [T, D])

    vec_d = dscr("vec_d", [6, 2, D])
    hT_d = dscr("hT_d", [16, 128, T], BF16)

    ident_f = P.sb([128, 128], F32, "identf")
    ident_b = P.sb([128, 128], BF16, "identb")
    ps = [nc.alloc_psum_tensor(f"ps{i}", [128, 512], F32) for i in range(8)]

    def dma(q, out, in_, reads=(), writes=(), **kw):
        return P.add(q, lambda e: e.dma_start(out=out, in_=in_, **kw), reads, writes)

    def act(out, in_, func, reads, writes, bias=None, scale=1.0, accum_out=None):
        kw = {}
        if bias is not None:
            kw["bias"] = bias
        if accum_out is not None:
            kw["accum_out"] = accum_out
        return P.add("act", lambda e: e.activation(out=out, in_=in_, func=func, scale=scale, **kw), reads, writes)

    def tt(out, in0, in1, op, reads, writes, q="dve"):
        return P.add(q, lambda e: e.tensor_tensor(out=out, in0=in0, in1=in1, op=op), reads, writes)

    def ts(out, in0, s1, s2, op0, op1, reads, writes, q="dve", accum_out=None):
        kw = {}
        if accum_out is not None:
            kw["accum_out"] = accum_out
        if op1 is None:
            return P.add(q, lambda e: e.tensor_scalar(out=out, in0=in0, scalar1=s1, scalar2=None, op0=op0, **kw), reads, writes)
        return P.add(q, lambda e: e.tensor_scalar(out=out, in0=in0, scalar1=s1, scalar2=s2, op0=op0, op1=op1, **kw), reads, writes)

    def stt(out, in0, scalar, in1, op0, op1, reads, writes):
        return P.add("dve", lambda e: e.scalar_tensor_tensor(out=out, in0=in0, scalar=scalar, in1=in1, op0=op0, op1=op1), reads, writes)

    def cp(out, in_, reads, writes, q="dve"):
        if q == "act":
            return P.add("act", lambda e: e.copy(out=out, in_=in_), reads, writes)
        return P.add(q, lambda e: e.tensor_copy(out=out, in_=in_), reads, writes)

    def mm(out, lhsT, rhs, start, stop, reads, writes):
        return P.add("pe", lambda e: e.matmul(out, lhsT, rhs, start=start, stop=stop), reads, writes)

    def tr(out, in_, ident, reads, writes):
        return P.add("pe", lambda e: e.transpose(out, in_, ident), reads, writes)

    def memset(ap, val, writes, q="pool"):
        return P.add(q, lambda e: e.memset(ap, val), (), writes)

    memset(ident_f[:], 0.0, ["identf"])
    P.add("pool", lambda e: e.affine_select(out=ident_f[:], in_=ident_f[:], pattern=[[-1, 128]],
                                            compare_op=ALU.not_equal, fill=1.0, base=0, channel_multiplier=1),
          ["identf"], ["identf"])
    cp(ident_b[:], ident_f[:], ["identf"], ["identb"])

    P.push()
    cT = P.sb([128, 16, 2], F32, "cT")
    scT = P.sb([128, 16, 2], F32, "scT")
    mod = P.sb([2, 6 * D], F32, "mod")
    bb = [P.sb([2, 512], F32, f"bb{i}") for i in range(2)]
    nr = P.sb([2, 4, D], F32, "nr")
    wa = [P.sb([128, 16, 512], F32, f"wa{i}") for i in range(2)]
    for s_ in range(2):
        dma("dsp", cT[:, :, s_], c[s_, :].rearrange("(k p) -> p k", p=128), (), ["cT"], allow_slow_non_contiguous=True)
    dma("dsp", nr[:], nrm.rearrange("(o f) d -> o f d", o=1).partition_broadcast(2), (), ["nr"])
    act(scT[:], cT[:], AF.Silu, ["cT"], ["scT"])
    for cg in range(24):
        b = cg % 2
        dma("dsp", wa[b][:], w_ada[:, cg * 512:(cg + 1) * 512].rearrange("(k p) n -> p k n", p=128), (), [f"wa{b}"])
        dma("dsp", bb[b][:], b_ada[0:1, cg * 512:(cg + 1) * 512].partition_broadcast(2), (), [f"bb{b}"])
        for k in range(16):
            mm(ps[0][0:2, :], scT[:, k, :], wa[b][:, k, :], k == 0, k == 15, ["scT", f"wa{b}"], ["ps0"])
        tt(mod[:, cg * 512:(cg + 1) * 512], ps[0][0:2, :], bb[b][:], ALU.add,
           ["ps0", f"bb{b}"], ["mod"])
    vecs = P.sb([2, 6, D], F32, "vecs")
    stt(vecs[:, 0, :], mod[:, D:2 * D], 1.0, nr[:, 0, :], ALU.add, ALU.mult, ["mod", "nr"], ["vecs"])
    cp(vecs[:, 1, :], mod[:, 0:D], ["mod"], ["vecs"])
    tt(vecs[:, 2, :], mod[:, 2 * D:3 * D], nr[:, 1, :], ALU.mult, ["mod", "nr"], ["vecs"])
    stt(vecs[:, 3, :], mod[:, 4 * D:5 * D], 1.0, nr[:, 2, :], ALU.add, ALU.mult, ["mod", "nr"], ["vecs"])
    cp(vecs[:, 4, :], mod[:, 3 * D:4 * D], ["mod"], ["vecs"])
    tt(vecs[:, 5, :], mod[:, 5 * D:6 * D], nr[:, 3, :], ALU.mult, ["mod", "nr"], ["vecs"])
    dma("dsp", vec_d.rearrange("f s d -> s f d"), vecs[:], ["vecs"], ["vec_d"])
    P.barrier()
    P.pop()

    def seq_of_tile(t):
        return 0 if t * 128 < LP else 1

    P.push()
    g1 = [P.sb([128, D], F32, f"g1_{s}") for s in range(2)]
    s1 = [P.sb([128, D], F32, f"s1_{s}") for s in range(2)]
    for s in range(2):
        dma("dsp", g1[s][:], vec_d[0, s:s + 1, :].partition_broadcast(128), (), [f"g1_{s}"])
        dma("dsp", s1[s][:], vec_d[1, s:s + 1, :].partition_broadcast(128), (), [f"s1_{s}"])
    xt = [P.sb([128, D], F32, f"xt{i}") for i in range(2)]
    junk = P.sb([128, D], F32, "junk")
    hTs = [P.sb([128, 16, 128], BF16, f"hTs{i}") for i in range(2)]
    ss = [P.sb([128, 1], F32, f"ss{i}") for i in range(2)]
    rs = [P.sb([128, 1], F32, f"rs{i}") for i in range(2)]
    for t in range(NT):
        b = t % 2
        s = seq_of_tile(t)
        dma("dsp", xt[b][:], x[t * 128:(t + 1) * 128, :], (), [f"xt{b}"])
        act(junk[:], xt[b][:], AF.Square, [f"xt{b}"], ["junk", f"ss{b}"], accum_out=ss[b][:])
        ts(rs[b][:], ss[b][:], 1.0 / D, 1e-6, ALU.mult, ALU.add, [f"ss{b}"], [f"rs{b}"])
        act(rs[b][:], rs[b][:], AF.Sqrt, [f"rs{b}"], [f"rs{b}"])
        P.add("dve", lambda e, b=b: e.reciprocal(out=rs[b][:], in_=rs[b][:]), [f"rs{b}"], [f"rs{b}"])
        stt(xt[b][:], xt[b][:], rs[b][:], g1[s][:], ALU.mult, ALU.mult, [f"xt{b}", f"rs{b}", f"g1_{s}"], [f"xt{b}"])
        tt(xt[b][:], xt[b][:], s1[s][:], ALU.add, [f"xt{b}", f"s1_{s}"], [f"xt{b}"])
        for kg in range(4):
            pk = (t * 4 + kg) % 8
            for k4 in range(4):
                k = kg * 4 + k4
                tr(ps[pk][:, k4 * 128:(k4 + 1) * 128], xt[b][:, k * 128:(k + 1) * 128], ident_f[:], [f"xt{b}", "identf"], [f"ps{pk}"])
            cp(hTs[b][:, kg * 4:(kg + 1) * 4, :].rearrange("p a b -> p (a b)"), ps[pk][:, :], [f"ps{pk}"], [f"hTs{b}"], q=("act" if kg % 2 else "dve"))
        dma("dsp", hT_d[:, :, t * 128:(t + 1) * 128].rearrange("k p t -> p k t"), hTs[b][:], [f"hTs{b}"], ["hT_d"])
    P.barrier()
    P.pop()

    if DBG == "p1":
        dbg = dout("dbg", [16, 128, T], BF16)
        dma("dsp", dbg, hT_d, (), ["dbg"])
        dbg2 = dout("dbg2", [6, 2, D])
        dma("dsp", dbg2, vec_d, (), ["dbg2"])
        P.barrier()
        P.emit()
        return nc

    TB = 512 if (LP % 512 == 0 and LS % 512 == 0) else 128
    NF = 6528
    win_b = dscr("win_b", [D, INC], BF16)
    zT_d = dscr("zT_d", [NF, T])
    ztok_d = dscr("ztok_d", [T, 2048])
    for k in range(16):
        dma("dpool", win_b[k * 128:(k + 1) * 128, :], w_in[k * 128:(k + 1) * 128, :], (), ["win_b"])
    P.barrier()
    DS = DE
    w_exp_gate = din("w_exp_gate", [NE, D, DE])
    w_exp_up = din("w_exp_up", [NE, D, DE])
    w_exp_down = din("w_exp_down", [NE, DE, D])
    w_sh_gate = din("w_sh_gate", [D, DS])
    w_sh_up = din("w_sh_up", [D, DS])
    w_sh_down = din("w_sh_down", [DS, D])
    wg_b = dscr("wg_b", [NE + 1, 128, 16 * DE], BF16)
    wu_b = dscr("wu_b", [NE + 1, 128, 16 * DE], BF16)
    wd_b = dscr("wd_b", [NE + 1, 128, (DE // 128) * D], BF16)
    for e_ in ([] if DBG in ("p2", "p3", "p4") else [NE] + list(range(NE))):
        sg_ = w_sh_gate if e_ == NE else w_exp_gate[e_]
        su_ = w_sh_up if e_ == NE else w_exp_up[e_]
        sd_ = w_sh_down if e_ == NE else w_exp_down[e_]
        dma("dpool2", wg_b[e_].rearrange("p (k n) -> p k n", k=16), sg_.rearrange("(k p) n -> p k n", p=128), (), ["persist:wg%d" % e_])
        dma("dpool2", wu_b[e_].rearrange("p (k n) -> p k n", k=16), su_.rearrange("(k p) n -> p k n", p=128), (), ["persist:wu%d" % e_])
        dma("dpool2", wd_b[e_].rearrange("p (k n) -> p k n", k=DE // 128), sd_.rearrange("(k p) n -> p k n", p=128), (), ["persist:wd%d" % e_])
    P.push()
    hTb = [P.sb([128, 16, TB], BF16, f"hTb{i}") for i in range(2)]
    wt = [P.sb([128, 16, 512], BF16, f"wt{i}") for i in range(2)]
    zs = [P.sb([128, 512], F32, f"zs{i}") for i in range(3)]
    groups = [(i * 512, 512, "F") for i in range(12)] + [(6144, 384, "F")] + [(6528 + i * 512, 512, "T") for i in range(4)]
    it = 0
    zi = 0
    for tb in range(T // TB):
        hb_ = tb % 2
        dma("dsp", hTb[hb_][:], hT_d[:, :, tb * TB:(tb + 1) * TB].rearrange("k p t -> p k t"), (), [f"hTb{hb_}"])
        for (c0, ncol, mode) in groups:
            wb = it % 2
            it += 1
            dma("dsp", wt[wb][:, :, 0:ncol], win_b[:, c0:c0 + ncol].rearrange("(k p) n -> p k n", p=128), (), [f"wt{wb}"])
            if mode == "F":
                for ci in range(ncol // 128):
                    pi = zi % 4
                    z_ = zi % 3
                    zi += 1
                    for k in range(16):
                        mm(ps[pi][:, 0:TB], wt[wb][:, k, ci * 128:(ci + 1) * 128], hTb[hb_][:, k, :], k == 0, k == 15,
                           [f"wt{wb}", f"hTb{hb_}"], [f"ps{pi}"])
                    cp(zs[z_][:, 0:TB], ps[pi][:, 0:TB], [f"ps{pi}"], [f"zs{z_}"], q=("act" if zi % 2 else "dve"))
                    dma("dsp", zT_d[c0 + ci * 128:c0 + (ci + 1) * 128, tb * TB:(tb + 1) * TB], zs[z_][:, 0:TB], [f"zs{z_}"], ["zT_d"])
            else:
                for ti in range(TB // 128):
                    pi = zi % 4
                    z_ = zi % 3
                    zi += 1
                    for k in range(16):
                        mm(ps[pi][:, :], hTb[hb_][:, k, ti * 128:(ti + 1) * 128], wt[wb][:, k, :], k == 0, k == 15,
                           [f"wt{wb}", f"hTb{hb_}"], [f"ps{pi}"])
                    cp(zs[z_][:, :], ps[pi][:, :], [f"ps{pi}"], [f"zs{z_}"], q=("act" if zi % 2 else "dve"))
                    dma("dsp", ztok_d[tb * TB + ti * 128:tb * TB + (ti + 1) * 128, c0 - 6528:c0 - 6528 + 512], zs[z_][:, :], [f"zs{z_}"], ["ztok_d"])
    P.barrier()
    P.pop()

    if DBG == "p2":
        dbg = dout("dbg", [NF, T])
        dma("dsp", dbg, zT_d, (), ["dbg"])
        dbg2 = dout("dbg2", [T, 2048])
        dma("dsp", dbg2, ztok_d, (), ["dbg2"])
        P.barrier()
        P.emit()
        return nc

    if DBG is not None:
        print("ops before P3:", len(P.ops))
    rw_mu = din("rw_mu", [RWC])
    rw_w0 = din("rw_w0", [2, RW])
    rw_a0 = din("rw_a0", [2, RW])
    rw_w_up = din("rw_w_up", [2, 64, RW])
    rw_a_up = din("rw_a_up", [2, 64, RW])
    rw_g_up = din("rw_g_up", [128, RW])
    rw_vecs = din("rw_vecs", [5, RW])
    m_d = dscr("m_d", [T, 2048])
    of_d = dscr("of_d", [T, RW])
    P.push()
    CH = 128
    TB_saved = TB
    TB = min(TB, 256)
    NCB = TB // CH
    muT = P.sb([64, 54], F32, "muT")
    ommT = P.sb([64, 54], F32, "ommT")
    hmuT = P.sb([64, 54], F32, "hmuT")
    dma("dsp", muT[:], rw_mu.rearrange("(j p) -> p j", p=64), (), ["muT"], allow_slow_non_contiguous=True)
    ts(ommT[:], muT[:], -1.0, 1.0, ALU.mult, ALU.add, ["muT"], ["ommT"])
    ts(hmuT[:], muT[:], 0.5, None, ALU.mult, None, ["muT"], ["hmuT"])
    mug = P.sb([128, 3], F32, "mug")
    dma("dsp", mug[:, 0:1], rw_mu[3328:3456].rearrange("(p o) -> p o", o=1), (), ["mug"])
    ts(mug[:, 1:2], mug[:, 0:1], -1.0, 1.0, ALU.mult, ALU.add, ["mug"], ["mug"])
    ts(mug[:, 2:3], mug[:, 0:1], 0.5, None, ALU.mult, None, ["mug"], ["mug"])
    w0T = P.sb([64, 2, 16], F32, "w0T")
    a0T = P.sb([64, 2, 16], F32, "a0T")
    for d_ in range(2):
        dma("dsp", w0T[:, d_, :], rw_w0[d_, :].rearrange("(h p) -> p h", p=64), (), ["w0T"], allow_slow_non_contiguous=True)
        dma("dsp", a0T[:, d_, :], rw_a0[d_, :].rearrange("(h p) -> p h", p=64), (), ["a0T"], allow_slow_non_contiguous=True)
    vT = P.sb([64, 5, 16], F32, "vT")
    for i_ in range(3):
        dma("dsp", vT[:, i_, :], rw_vecs[i_, :].rearrange("(h p) -> p h", p=64), (), ["vT"], allow_slow_non_contiguous=True)
    omka = P.sb([64, 16], F32, "omka")
    ts(omka[:], vT[:, 1, :], -1.0, 1.0, ALU.mult, ALU.add, ["vT"], ["omka"])
    lnw_b = P.sb([128, RW], F32, "lnw_b")
    lnb_b = P.sb([128, RW], F32, "lnb_b")
    rk_b = P.sb([128, RW], F32, "rk_b")
    dma("dsp", rk_b[:], rw_vecs[2:3, :].partition_broadcast(128), (), ["rk_b"])
    dma("dsp", lnw_b[:], rw_vecs[3:4, :].partition_broadcast(128), (), ["lnw_b"])
    dma("dsp", lnb_b[:], rw_vecs[4:5, :].partition_broadcast(128), (), ["lnb_b"])
    wup = P.sb([64, 2, RW], BF16, "wup")
    aup = P.sb([64, 2, RW], BF16, "aup")
    gup = P.sb([128, RW], BF16, "gup")
    for d_ in range(2):
        dma("dpool", wup[:, d_, :], rw_w_up[d_], (), ["wup"])
        dma("dpool", aup[:, d_, :], rw_a_up[d_], (), ["aup"])
    dma("dpool", gup[:], rw_g_up, (), ["gup"])
    ones_f = P.sb([128, 128], F32, "ones_f")
    memset(ones_f[:], 1.0, ["ones_f"])
    MASK2 = [P.sb([128, 256], F32, f"mask2_{d_}") for d_ in range(2)]
    MASKX = [P.sb([128, 128], F32, f"maskx_{d_}") for d_ in range(2)]
    for d_ in range(2):
        memset(MASK2[d_][:], 1.0, [f"mask2_{d_}"])
        memset(MASKX[d_][:], 1.0, [f"maskx_{d_}"])
        sgn = 1 if d_ == 0 else -1
        P.add("pool", lambda e, d_=d_, sgn=sgn: e.affine_select(out=MASK2[d_][:, 0:128], in_=MASK2[d_][:, 0:128], pattern=[[sgn, 128]],
              compare_op=ALU.is_gt, fill=0.0, base=0, channel_multiplier=-sgn), [f"mask2_{d_}"], [f"mask2_{d_}"])
        P.add("pool", lambda e, d_=d_, sgn=sgn: e.affine_select(out=MASK2[d_][:, 128:256], in_=MASK2[d_][:, 128:256], pattern=[[sgn, 128]],
              compare_op=ALU.is_ge, fill=0.0, base=0, channel_multiplier=-sgn), [f"mask2_{d_}"], [f"mask2_{d_}"])
        P.add("pool", lambda e, d_=d_, sgn=sgn: e.affine_select(out=MASKX[d_][:], in_=MASKX[d_][:], pattern=[[-sgn, 128]],
              compare_op=ALU.is_gt, fill=0.0, base=0, channel_multiplier=sgn), [f"maskx_{d_}"], [f"maskx_{d_}"])
    ST = P.sb([64, 16, 64], F32, "ST")
    STb = P.sb([64, 16, 64], F32 if cfg.get("rw_fp32", True) else BF16, "STb")
    zh = [P.sb([64, TB + 2], F32, f"zh{i}") for i in range(2)]
    sft = P.sb([64, TB], F32, "sft")
    lat = P.sb([64, 5, TB], BF16, "lat")
    sgl = P.sb([128, TB], BF16, "sgl")
    zg = P.sb([128, TB + 2], F32, "zg")
    sftg = P.sb([128, TB], F32, "sftg")
    CD = F32 if cfg.get("rw_fp32", True) else BF16
    ident_c = ident_f if CD == F32 else ident_b
    identc_k = "identf" if CD == F32 else "identb"
    def mk_stream(sid):
        S = {}
        S["zh"] = [P.sb([64, TB + 2], F32, f"zhs{i}") for i in range(2)]
        S["rr_"] = P.sb([64, TB], F32, "rr")
        S["kk_"] = P.sb([64, TB], F32, "kk")
        S["vv_"] = P.sb([64, TB], F32, "vv")
        S["kn"] = P.sb([64, TB], F32, "kn")
        S["t1"] = P.sb([64, TB], F32, "t1")
        S["t2"] = P.sb([64, TB], F32, "t2")
        S["lw"] = P.sb([64, TB], F32, "lw")
        S["aa"] = P.sb([64, TB], F32, "aa")
        S["kd"] = P.sb([64, TB], F32, "kd")
        S["bb_"] = P.sb([64, TB], F32, "bbk")
        S["cl"] = P.sb([64, TB], F32, "cl")
        S["ex"] = P.sb([64, TB], F32, "ex")
        S["ART"] = P.sb([64, NCB, 2, CH], CD, "ART")
        S["BT"] = P.sb([64, TB], CD, "BT")
        S["KT"] = P.sb([64, TB], CD, "KT")
        S["BhT"] = P.sb([64, TB], CD, "BhT")
        S["KhT"] = P.sb([64, TB], CD, "KhT")
        S["AfT"] = P.sb([64, TB], CD, "AfT")
        S["RfT"] = P.sb([64, TB], CD, "RfT")
        S["VT"] = P.sb([64, TB], CD, "VT")
        S["rkv32"] = P.sb([64, 2, TB], F32, "rkv32")
        S["pc"] = P.sb([64, NCB], F32, "pc")
        S["clc"] = P.sb([64, NCB], F32, "clc")
        S["tok"] = P.sb([128, 5, 64], CD, "tok")
        S["NN"] = [P.sb([128, 256], CD, f"NN{i}") for i in range(2)]
        S["NX"] = [P.sb([128, 256], CD, f"NX{i}") for i in range(2)]
        S["Wt"] = [P.sb([128, 128], CD, f"Wt{i}") for i in range(2)]
        S["RhT"] = P.sb([64, 128], CD, "RhT")
        S["GB"] = P.sb([64, 64], CD, "GB")
        S["zi"] = 0
        S["ps"] = [ps[4 * sid + j] for j in (0, 1, 2, 0, 2, 3)]
        return S
    STREAMS = [mk_stream(0), mk_stream(1)]
    o_tok = P.sb([128, NCB, RW], F32, "o_tok")
    g_tok = P.sb([128, NCB, RW], F32, "g_tok")
    kb_tok = P.sb([128, NCB, RW], F32, "kb_tok")
    v_tok32 = P.sb([128, NCB, RW], F32, "v_tok32")
    of_t = P.sb([128, RW], F32, "of_t")
    st8 = P.sb([128, 16], F32, "st8")
    st9 = P.sb([128, 16], F32, "st9")
    onr = P.sb([128, RW], F32, "onr")

    seq_bounds = [(0, LP), (LP, T)]
    OT = [f"o_tok{h}" for h in range(16)]
    KBT = [f"kb_tok{h}" for h in range(16)]
    VTT = [f"v_tok32{h}" for h in range(16)]
    STK = [f"ST{h}" for h in range(16)]
    STBK = [f"STb{h}" for h in range(16)]

    def mk_kmap(h, sid):
        m = {}
        for n_ in ("kn", "t1", "t2", "lw", "aa", "kd", "bbk", "cl", "ex", "ART", "BT", "KT", "BhT", "KhT", "AfT", "RfT",
                   "VT", "rkv32", "pc", "clc", "tok", "NN0", "NN1", "NX0", "NX1", "Wt0", "Wt1", "RhT", "GB", "zh0", "zh1"):
            m[n_] = f"{n_}@{sid}"
        for i_, j_ in enumerate((0, 1, 2, 0, 2, 3)):
            m[f"ps{i_}"] = f"ps{4 * sid + j_}"
        m["ST"] = f"ST{h}"
        m["STb"] = f"STb{h}"
        m["o_tok"] = f"o_tok{h}"
        m["kb_tok"] = f"kb_tok{h}"
        m["v_tok32"] = f"v_tok32{h}"
        return m

    def load_shift(dst_sft, ztile, row0, nrow, t0, s0, s1, colj, key_z, key_o, omm_ap, hmu_ap):
        lo = t0 - 1
        hi = t0 + TB + 1
        a = max(lo, s0)
        b = min(hi, s1)
        if lo < s0:
            memset(ztile[0:nrow, 0:1], 0.0, [key_z])
        if hi > s1:
            memset(ztile[0:nrow, TB + 1:TB + 2], 0.0, [key_z])
        dma("dsp", ztile[0:nrow, a - lo:b - lo], zT_d[row0:row0 + nrow, a:b], (), [key_z])
        tt(dst_sft, ztile[0:nrow, 0:TB], ztile[0:nrow, 2:TB + 2], ALU.add, [key_z], [key_o])
        ts(dst_sft, dst_sft, hmu_ap, None, ALU.mult, None, [key_o], [key_o])
        stt(dst_sft, ztile[0:nrow, 1:TB + 1], omm_ap, dst_sft, ALU.mult, ALU.add, [key_z, key_o], [key_o])

    zi = 0
    for d_ in range(2):
        for (s0, s1) in seq_bounds:
            nblk = (s1 - s0) // TB
            memset(ST[:], 0.0, STK)
            memset(STb[:], 0.0, STBK)
            blks = range(nblk) if d_ == 0 else range(nblk - 1, -1, -1)
            for bi in blks:
                t0 = s0 + bi * TB
                for j_ in range(4):
                    zb = zi % 2
                    zi += 1
                    load_shift(sft[:], zh[zb], 3072 + j_ * 64, 64, t0, s0, s1, 48 + j_, f"zh{zb}", "sft",
                               ommT[:, 48 + j_:49 + j_], hmuT[:, 48 + j_:49 + j_])
                    if j_ < 2:
                        act(lat[:, j_, :], sft[:], AF.Tanh, ["sft"], ["lat"])
                    else:
                        cp(lat[:, j_, :], sft[:], ["sft"], ["lat"])
                load_shift(sftg[:], zg, 3328, 128, t0, s0, s1, 0, "zg", "sftg", mug[:, 1:2], mug[:, 2:3])
                act(sgl[:], sftg[:], AF.Sigmoid, ["sftg"], ["sgl"])
                for ci in range(NCB):
                    for half in range(2):
                        pi = 4 + half
                        mm(ps[pi][:, :], sgl[:, ci * CH:(ci + 1) * CH], gup[:, half * 512:(half + 1) * 512], True, True,
                           ["sgl", "gup"], [f"ps{pi}"])
                        cp(g_tok[:, ci, half * 512:(half + 1) * 512], ps[pi][:, :], [f"ps{pi}"], ["g_tok"], q="act")
                def head_body(h, sid):
                    S = STREAMS[sid]
                    ps = S["ps"]
                    zh = S["zh"]
                    rr_ = S["rr_"]
                    kk_ = S["kk_"]
                    vv_ = S["vv_"]
                    kn = S["kn"]
                    t1 = S["t1"]
                    t2 = S["t2"]
                    lw = S["lw"]
                    aa = S["aa"]
                    kd = S["kd"]
                    bb_ = S["bb_"]
                    cl = S["cl"]
                    ex = S["ex"]
                    ART = S["ART"]
                    BT = S["BT"]
                    KT = S["KT"]
                    BhT = S["BhT"]
                    KhT = S["KhT"]
                    AfT = S["AfT"]
                    RfT = S["RfT"]
                    VT = S["VT"]
                    rkv32 = S["rkv32"]
                    pc = S["pc"]
                    clc = S["clc"]
                    tok = S["tok"]
                    NN = S["NN"]
                    NX = S["NX"]
                    Wt = S["Wt"]
                    RhT = S["RhT"]
                    GB = S["GB"]
                    for (dst, base, colj) in ((rr_, 0, h), (kk_, 1024, 16 + h), (vv_, 2048, 32 + h)):
                        zb = S["zi"] % 2
                        S["zi"] += 1
                        load_shift(dst[:], zh[zb], base + h * 64, 64, t0, s0, s1, colj, f"zh{zb}", dst.name,
                                   ommT[:, colj:colj + 1], hmuT[:, colj:colj + 1])
                    ts(kn[:], kk_[:], vT[:, 0, h:h + 1], None, ALU.mult, None, [kk_.name, "vT"], ["kn"])
                    tt(t1[:], kn[:], kn[:], ALU.mult, ["kn"], ["t1"])
                    mm(ps[0][0:64, 0:TB], ones_f[0:64, 0:64], t1[:], True, True, ["ones_f", "t1"], ["ps0"])
                    act(t2[:], ps[0][0:64, 0:TB], AF.Sqrt, ["ps0"], ["t2"])
                    ts(t2[:], t2[:], 1e-12, None, ALU.max, None, ["t2"], ["t2"])
                    P.add("dve", lambda e: e.reciprocal(out=t2[:], in_=t2[:]), ["t2"], ["t2"])
                    tt(kn[:], kn[:], t2[:], ALU.mult, ["kn", "t2"], ["kn"])
                    mm(ps[1][0:64, 0:TB], wup[:, d_, h * 64:(h + 1) * 64], lat[:, d_, :], True, True, ["wup", "lat"], ["ps1"])
                    act(lw[:], ps[1][0:64, 0:TB], AF.Sigmoid, ["ps1", "w0T"], ["lw"], bias=w0T[:, d_, h:h + 1])
                    ts(lw[:], lw[:], -0.6065306597126334, None, ALU.mult, None, ["lw"], ["lw"])
                    mm(ps[2][0:64, 0:TB], aup[:, d_, h * 64:(h + 1) * 64], lat[:, 2 + d_, :], True, True, ["aup", "lat"], ["ps2"])
                    act(aa[:], ps[2][0:64, 0:TB], AF.Sigmoid, ["ps2", "a0T"], ["aa"], bias=a0T[:, d_, h:h + 1])
                    ts(t1[:], aa[:], vT[:, 1, h:h + 1], omka[:, h:h + 1], ALU.mult, ALU.add, ["aa", "vT", "omka"], ["t1"])
                    tt(kd[:], kk_[:], t1[:], ALU.mult, [kk_.name, "t1"], ["kd"])
                    tt(bb_[:], kn[:], aa[:], ALU.mult, ["kn", "aa"], ["bbk"])
                    stt(rkv32[:, 1, :], rr_[:], 0.5, kd[:], ALU.mult, ALU.mult, [rr_.name, "kd"], ["rkv32"])
                    for ci in range(NCB):
                        sl = slice(ci * CH, (ci + 1) * CH)
                        P.add("dve", lambda e, sl=sl: e.tensor_tensor_scan(out=cl[:, sl], data0=ones_f[0:64, 0:CH], data1=lw[:, sl],
                              initial=0.0, op0=ALU.mult, op1=ALU.add), ["ones_f", "lw"], ["cl"])
                        if d_ == 1:
                            cp(clc[:, ci:ci + 1], cl[:, ci * CH + CH - 1:ci * CH + CH], ["cl"], ["clc"])
                            tt(cl[:, sl], lw[:, sl], cl[:, sl], ALU.subtract, ["lw", "cl"], ["cl"])
                            ts(cl[:, sl], cl[:, sl], clc[:, ci:ci + 1], None, ALU.add, None, ["cl", "clc"], ["cl"])
                        else:
                            cp(clc[:, ci:ci + 1], cl[:, ci * CH + CH - 1:ci * CH + CH], ["cl"], ["clc"])
                    act(pc[:], clc[:], AF.Exp, ["clc"], ["pc"])
                    act(ex[:], cl[:], AF.Exp, ["cl"], ["ex"])
                    tt(t1[:], rr_[:], ex[:], ALU.mult, [rr_.name, "ex"], ["t1"])
                    for ci in range(NCB):
                        cp(ART[:, ci, 1, :], t1[:, ci * CH:(ci + 1) * CH], ["t1"], ["ART"], q="act")
                    cp(RfT[:], t1[:], ["t1"], ["RfT"], q="act")
                    tt(t2[:], cl[:], lw[:], ALU.subtract, ["cl", "lw"], ["t2"])
                    act(ex[:], t2[:], AF.Exp, ["t2"], ["ex"])
                    stt(t1[:], kn[:], -1.0, ex[:], ALU.mult, ALU.mult, ["kn", "ex"], ["t1"])
                    for ci in range(NCB):
                        cp(ART[:, ci, 0, :], t1[:, ci * CH:(ci + 1) * CH], ["t1"], ["ART"], q="act")
                    cp(AfT[:], t1[:], ["t1"], ["AfT"], q="act")
                    act(ex[:], cl[:], AF.Exp, ["cl"], ["ex"], scale=-1.0)
                    tt(BT[:], bb_[:], ex[:], ALU.mult, ["bbk", "ex"], ["BT"])
                    tt(KT[:], kd[:], ex[:], ALU.mult, ["kd", "ex"], ["KT"])
                    for ci in range(NCB):
                        sl = slice(ci * CH, (ci + 1) * CH)
                        act(ex[:, sl], cl[:, sl], AF.Exp, ["cl", "clc"], ["ex"], scale=-1.0, bias=clc[:, ci:ci + 1])
                    tt(BhT[:], bb_[:], ex[:], ALU.mult, ["bbk", "ex"], ["BhT"])
                    tt(KhT[:], kd[:], ex[:], ALU.mult, ["kd", "ex"], ["KhT"])
                    cp(VT[:], vv_[:], [vv_.name], ["VT"], q="act")
                    cis = range(NCB) if d_ == 0 else range(NCB - 1, -1, -1)
                    for ci in cis:
                        sl = slice(ci * CH, (ci + 1) * CH)
                        for i_, (src, sk) in enumerate(((AfT, "AfT"), (BhT, "BhT"), (KhT, "KhT"), (RfT, "RfT"), (VT, "VT"))):
                            tr(ps[3][:, 192 + i_ * 64:192 + (i_ + 1) * 64], src[:, sl], ident_c[0:64, 0:64], [sk, identc_k], ["ps3"])
                        cp(tok[:].rearrange("p a b -> p (a b)"), ps[3][:, 192:512], ["ps3"], ["tok"])
                        tr(ps[3][:, 0:64], rkv32[:, 1, sl], ident_f[0:64, 0:64], ["rkv32", "identf"], ["ps3"])
                        tr(ps[3][:, 64:128], rr_[:, sl], ident_f[0:64, 0:64], [rr_.name, "identf"], ["ps3"])
                        tr(ps[3][:, 128:192], vv_[:, sl], ident_f[0:64, 0:64], [vv_.name, "identf"], ["ps3"])
                        tt(kb_tok[:, ci, h * 64:(h + 1) * 64], ps[3][:, 0:64], rk_b[:, h * 64:(h + 1) * 64], ALU.mult, ["ps3", "rk_b"], ["kb_tok"]) if d_ == 0 else \
                            stt(kb_tok[:, ci, h * 64:(h + 1) * 64], ps[3][:, 0:64], 1.0, rk_b[:, h * 64:(h + 1) * 64], ALU.mult, ALU.mult, ["ps3", "rk_b"], ["kb_tok"])
                        cp(v_tok32[:, ci, h * 64:(h + 1) * 64], ps[3][:, 128:192], ["ps3"], ["v_tok32"], q="act")
                        mm(ps[0][:, 0:256], BT[:, sl], ART[:, ci, :, :].rearrange("p a b -> p (a b)"), True, True, ["BT", "ART"], ["ps0"])
                        tt(NN[0][:], ps[0][:, 0:256], MASK2[d_][:], ALU.mult, ["ps0", f"mask2_{d_}"], ["NN0"])
                        mm(ps[1][:, 0:256], KT[:, sl], ART[:, ci, :, :].rearrange("p a b -> p (a b)"), True, True, ["KT", "ART"], ["ps1"])
                        tt(NN[1][:], ps[1][:, 0:256], MASK2[d_][:], ALU.mult, ["ps1", f"mask2_{d_}"], ["NN1"])
                        mm(ps[2][:, 0:128], ART[:, ci, 0, :], BT[:, sl], True, True, ["ART", "BT"], ["ps2"])
                        cur = 0
                        cp(NX[cur][:, 0:128], NN[0][:, 0:128], ["NN0"], [f"NX{cur}"], q="act")
                        tt(NX[cur][:, 128:256], ps[2][:, 0:128], MASKX[d_][:], ALU.mult, ["ps2", f"maskx_{d_}"], [f"NX{cur}"])
                        mm(ps[2][:, 128:192], NN[1][:, 0:128], tok[:, 4, :], True, True, ["NN1", "tok"], ["ps2"])
                        cp(Wt[0][:, 0:64], tok[:, 0, :], ["tok"], ["Wt0"], q="act")
                        cp(Wt[0][:, 64:128], ps[2][:, 128:192], ["ps2"], ["Wt0"])
                        wc = 0
                        for j_ in range(7):
                            mm(ps[4][:, 0:128], ident_c[:], Wt[wc][:], True, False, [identc_k, f"Wt{wc}"], ["ps4"])
                            mm(ps[4][:, 0:128], NX[cur][:, 0:128], Wt[wc][:], False, True, [f"NX{cur}", f"Wt{wc}"], ["ps4"])
                            cp(Wt[1 - wc][:], ps[4][:, 0:128], ["ps4"], [f"Wt{1 - wc}"], q="act")
                            wc = 1 - wc
                            if j_ < 6:
                                mm(ps[5][:, 0:128], NX[cur][:, 128:256], NX[cur][:, 0:128], True, True, [f"NX{cur}"], ["ps5"])
                                mm(ps[5][:, 128:256], NX[cur][:, 0:128], NX[cur][:, 128:256], True, True, [f"NX{cur}"], ["ps5"])
                                cp(NX[1 - cur][:], ps[5][:, 0:256], ["ps5"], [f"NX{1 - cur}"])
                                cur = 1 - cur
                        W = Wt[wc]
                        wk = f"Wt{wc}"
                        mm(ps[0][0:64, 0:128], tok[:, 3, :], ident_c[:], True, False, ["tok", identc_k], ["ps0"])
                        mm(ps[0][0:64, 0:128], W[:, 0:64], NN[0][:, 128:256], False, True, [wk, "NN0"], ["ps0"])
                        cp(RhT[:], ps[0][0:64, 0:128], ["ps0"], ["RhT"], q="act")
                        mm(ps[1][:, 0:64], RhT[:], STb[:, h, :], True, False, ["RhT", "STb"], ["ps1"])
                        mm(ps[1][:, 0:64], NN[0][:, 128:256], W[:, 64:128], False, False, ["NN0", wk], ["ps1"])
                        mm(ps[1][:, 0:64], NN[1][:, 128:256], tok[:, 4, :], False, True, ["NN1", "tok"], ["ps1"])
                        cp(o_tok[:, ci, h * 64:(h + 1) * 64], ps[1][:, 0:64], ["ps1"], ["o_tok"])
                        mm(ps[2][0:64, 0:64], W[:, 0:64], tok[:, 1, :], True, True, [wk, "tok"], ["ps2"])
                        cp(GB[:], ps[2][0:64, 0:64], ["ps2"], ["GB"], q="act")
                        mm(ps[4][0:64, 0:64], GB[:], STb[:, h, :], True, False, ["GB", "STb"], ["ps4"])
                        mm(ps[4][0:64, 0:64], tok[:, 1, :], W[:, 64:128], False, False, ["tok", wk], ["ps4"])
                        mm(ps[4][0:64, 0:64], tok[:, 2, :], tok[:, 4, :], False, True, ["tok"], ["ps4"])
                        stt(ST[:, h, :], ST[:, h, :], pc[:, ci:ci + 1], ps[4][0:64, 0:64], ALU.mult, ALU.add, ["ST", "pc", "ps4"], ["ST"])
                        cp(STb[:, h, :], ST[:, h, :], ["ST"], ["STb"], q="act")
                for h2 in range(0, 16, 2):
                    lists = []
                    for sid in range(2):
                        P.cap = []
                        P.kmap = mk_kmap(h2 + sid, sid)
                        head_body(h2 + sid, sid)
                        lists.append(P.cap)
                        P.cap = None
                        P.kmap = None
                    P.merge(lists)
                for ci in range(NCB):
                    r0 = t0 + ci * CH
                    if d_ == 0:
                        dma("dsp", of_d[r0:r0 + CH, :], o_tok[:, ci, :], [*OT], ["of_d"])
                        dma("dsp", m_d[r0:r0 + CH, 0:RW], kb_tok[:, ci, :], [*KBT], ["m_d"])
                    else:
                        dma("dsp", of_t[:], of_d[r0:r0 + CH, :], ["of_d"], ["of_t"])
                        tt(o_tok[:, ci, :], o_tok[:, ci, :], of_t[:], ALU.add, [*OT, "of_t"], [*OT])
                        dma("dsp", of_t[:], m_d[r0:r0 + CH, 0:RW], ["m_d"], ["of_t"])
                        tt(kb_tok[:, ci, :], kb_tok[:, ci, :], of_t[:], ALU.add, [*KBT, "of_t"], [*KBT])
                        o3 = o_tok[:, ci, :].rearrange("p (h v) -> p h v", v=64)
                        P.add("dve", lambda e, o3=o3: e.tensor_reduce(out=st8[:], in_=o3, axis=AX.X, op=ALU.add), [*OT], ["st8"])
                        ts(st8[:], st8[:], 1.0 / 64, None, ALU.mult, None, ["st8"], ["st8"])
                        tt(o3, o3, st8[:].unsqueeze(2).to_broadcast([128, 16, 64]), ALU.subtract, [*OT, "st8"], [*OT])
                        tt(onr[:], o_tok[:, ci, :], o_tok[:, ci, :], ALU.mult, [*OT], ["onr"])
                        P.add("dve", lambda e: e.tensor_reduce(out=st9[:], in_=onr[:].rearrange("p (h v) -> p h v", v=64), axis=AX.X, op=ALU.add), ["onr"], ["st9"])
                        ts(st9[:], st9[:], 1.0 / 64, 64e-5, ALU.mult, ALU.add, ["st9"], ["st9"])
                        act(st9[:], st9[:], AF.Sqrt, ["st9"], ["st9"])
                        P.add("dve", lambda e: e.reciprocal(out=st9[:], in_=st9[:]), ["st9"], ["st9"])
                        tt(o3, o3, st9[:].unsqueeze(2).to_broadcast([128, 16, 64]), ALU.mult, [*OT, "st9"], [*OT])
                        tt(o_tok[:, ci, :], o_tok[:, ci, :], lnw_b[:], ALU.mult, [*OT, "lnw_b"], [*OT])
                        tt(o_tok[:, ci, :], o_tok[:, ci, :], lnb_b[:], ALU.add, [*OT, "lnb_b"], [*OT])
                        P.add("dve", lambda e, ci=ci: e.tensor_reduce(out=st8[:], in_=kb_tok[:, ci, :].rearrange("p (h v) -> p h v", v=64), axis=AX.X, op=ALU.add), [*KBT], ["st8"])
                        tt(onr[:].rearrange("p (h v) -> p h v", v=64), v_tok32[:, ci, :].rearrange("p (h v) -> p h v", v=64),
                           st8[:].unsqueeze(2).to_broadcast([128, 16, 64]), ALU.mult, [*VTT, "st8"], ["onr"])
                        tt(o_tok[:, ci, :], o_tok[:, ci, :], onr[:], ALU.add, [*OT, "onr"], [*OT])
                        tt(o_tok[:, ci, :], o_tok[:, ci, :], g_tok[:, ci, :], ALU.mult, [*OT, "g_tok"], [*OT])
                        dma("dsp", m_d[r0:r0 + CH, 0:RW], o_tok[:, ci, :], [*OT], ["m_d"])
    P.barrier()
    P.pop()
    TB = TB_saved

    if DBG == "p3":
        print("ops after P3:", len(P.ops))
        P.limit = None
        dbg = dout("dbg", [T, 2048])
        dma("dsp", dbg, m_d, (), ["dbg"])
        P.barrier()
        P.emit()
        return nc

    hg_gamma = din("hg_gamma", [2, 2, HG])
    hg_norm_w = din("hg_norm_w", [1, HG])
    P.push()
    C2 = 64
    NC2 = TB // C2
    gm = P.sb([128, 2, 2, 8], F32, "gm")
    for l_ in range(2):
        for d_ in range(2):
            dma("dsp", gm[:, l_, d_, :], hg_gamma[l_, d_, :].rearrange("(h p) -> p h", p=128), (), ["gm"], allow_slow_non_contiguous=True)
    lbT = P.sb([128, 2, 8], F32, "lbT")
    omlb = P.sb([128, 2, 8], F32, "omlb")
    tt(lbT[:], gm[:, 0, :, :], gm[:, 1, :, :], ALU.subtract, ["gm"], ["lbT"])
    act(lbT[:], lbT[:], AF.Sigmoid, ["lbT"], ["lbT"])
    ts(omlb[:], lbT[:], -1.0, 1.0, ALU.mult, ALU.add, ["lbT"], ["omlb"])
    hnw = P.sb([64, HG], F32, "hnw")
    dma("dsp", hnw[:], hg_norm_w[0:1, :].partition_broadcast(64), (), ["hnw"])
    ones2 = P.sb([128, 64], F32, "ones2")
    memset(ones2[:], 1.0, ["ones2"])
    MI = [P.sb([64, 64], F32, f"mi{d_}") for d_ in range(2)]
    for d_ in range(2):
        sgn = 1 if d_ == 0 else -1
        memset(MI[d_][:], 1.0, [f"mi{d_}"])
        P.add("pool", lambda e, d_=d_, sgn=sgn: e.affine_select(out=MI[d_][:], in_=MI[d_][:], pattern=[[sgn, 64]],
              compare_op=ALU.is_ge, fill=0.0, base=0, channel_multiplier=-sgn), [f"mi{d_}"], [f"mi{d_}"])
    S2 = P.sb([128, 8, 128], F32, "S2")
    NHS = 4
    def mk_hstream(sid):
        S = {}
        S["qz"] = P.sb([128, TB], F32, "qz")
        S["fz"] = P.sb([128, TB], F32, "fz")
        S["lf"] = P.sb([128, TB], F32, "lf")
        S["kq"] = P.sb([128, TB], F32, "kq")
        S["b2"] = P.sb([128, TB], F32, "b2")
        S["e2"] = P.sb([128, TB], F32, "e2")
        S["qT"] = P.sb([128, TB], F32, "qT")
        S["kT2"] = P.sb([128, TB], F32, "kT2")
        S["khT"] = P.sb([128, TB], F32, "khT")
        S["bc2"] = P.sb([128, NC2], F32, "bc2")
        S["pc2"] = P.sb([128, NC2], F32, "pc2")
        S["vt2"] = P.sb([64, NC2, 128], F32, "vt2")
        S["sc2_"] = P.sb([64, 64], F32, "sc2")
        S["kh_tok"] = P.sb([64, 128], F32, "kh_tok")
        S["ps"] = [ps[2 * sid], ps[2 * sid + 1], ps[2 * sid], None, ps[2 * sid + 1]]
        return S
    HSTREAMS = [mk_hstream(i) for i in range(NHS)]
    O2K = [f"o2_{h}" for h in range(8)]
    S2K = [f"S2_{h}" for h in range(8)]

    def mk_hkmap(h, sid):
        m = {}
        for n_ in ("qz", "fz", "lf", "kq", "b2", "e2", "qT", "kT2", "khT", "bc2", "pc2", "vt2", "sc2", "kh_tok"):
            m[n_] = f"{n_}@{sid}"
        m["ps0"] = f"ps{2 * sid}"
        m["ps2"] = f"ps{2 * sid}"
        m["ps1"] = f"ps{2 * sid + 1}"
        m["ps4"] = f"ps{2 * sid + 1}"
        m["S2"] = f"S2_{h}"
        m["o2"] = f"o2_{h}"
        return m
    gt2_ = P.sb([64, NC2, HG], F32, "gt2")
    o2 = P.sb([64, NC2, HG], F32, "o2")
    of2 = P.sb([64, HG], F32, "of2")
    sq2 = P.sb([64, HG], F32, "sq2")
    st2 = P.sb([64, 8], F32, "st2")
    for d_ in range(2):
        for (s0, s1) in seq_bounds:
            nblk = (s1 - s0) // TB
            memset(S2[:], 0.0, S2K)
            blks = range(nblk) if d_ == 0 else range(nblk - 1, -1, -1)
            for bi in blks:
                t0 = s0 + bi * TB
                if d_ == 1:
                    for ci in range(NC2):
                        dma("dsp", gt2_[:, ci, :], ztok_d[t0 + ci * C2:t0 + (ci + 1) * C2, 1024:2048], (), ["gt2"])
                def hg_body(h, sid):
                    S = HSTREAMS[sid]
                    ps = S["ps"]
                    qz = S["qz"]
                    fz = S["fz"]
                    lf = S["lf"]
                    kq = S["kq"]
                    b2 = S["b2"]
                    e2 = S["e2"]
                    qT = S["qT"]
                    kT2 = S["kT2"]
                    khT = S["khT"]
                    bc2 = S["bc2"]
                    pc2 = S["pc2"]
                    vt2 = S["vt2"]
                    sc2_ = S["sc2_"]
                    kh_tok = S["kh_tok"]
                    dma("dsp", qz[:], zT_d[3456 + h * 128:3456 + (h + 1) * 128, t0:t0 + TB], (), ["qz"])
                    dma("dsp", fz[:], zT_d[4480 + d_ * 1024 + h * 128:4480 + d_ * 1024 + (h + 1) * 128, t0:t0 + TB], (), ["fz"])
                    for ci in range(NC2):
                        dma("dsp", vt2[:, ci, :], ztok_d[t0 + ci * C2:t0 + (ci + 1) * C2, h * 128:(h + 1) * 128], (), ["vt2"])
                    act(qz[:], qz[:], AF.Silu, ["qz"], ["qz"])
                    act(fz[:], fz[:], AF.Sigmoid, ["fz"], ["fz"])
                    ts(fz[:], fz[:], omlb[:, d_, h:h + 1], lbT[:, d_, h:h + 1], ALU.mult, ALU.add, ["fz", "omlb", "lbT"], ["fz"])
                    act(lf[:], fz[:], AF.Ln, ["fz"], ["lf"])
                    ts(kq[:], fz[:], -1.0, 1.0, ALU.mult, ALU.add, ["fz"], ["kq"])
                    for ci in range(NC2):
                        sl = slice(ci * C2, (ci + 1) * C2)
                        P.add("dve", lambda e, sl=sl: e.tensor_tensor_scan(out=b2[:, sl], data0=ones2[:, 0:C2], data1=lf[:, sl],
                              initial=0.0, op0=ALU.mult, op1=ALU.add), ["ones2", "lf"], ["b2"])
                        cp(bc2[:, ci:ci + 1], b2[:, ci * C2 + C2 - 1:ci * C2 + C2], ["b2"], ["bc2"])
                        if d_ == 1:
                            tt(b2[:, sl], lf[:, sl], b2[:, sl], ALU.subtract, ["lf", "b2"], ["b2"])
                            ts(b2[:, sl], b2[:, sl], bc2[:, ci:ci + 1], None, ALU.add, None, ["b2", "bc2"], ["b2"])
                    act(pc2[:], bc2[:], AF.Exp, ["bc2"], ["pc2"])
                    act(e2[:], b2[:], AF.Exp, ["b2"], ["e2"])
                    tt(qT[:], qz[:], e2[:], ALU.mult, ["qz", "e2"], ["qT"])
                    act(e2[:], b2[:], AF.Exp, ["b2"], ["e2"], scale=-1.0)
                    tt(kT2[:], kq[:], e2[:], ALU.mult, ["kq", "e2"], ["kT2"])
                    for ci in range(NC2):
                        sl = slice(ci * C2, (ci + 1) * C2)
                        act(e2[:, sl], b2[:, sl], AF.Exp, ["b2", "bc2"], ["e2"], scale=-1.0, bias=bc2[:, ci:ci + 1])
                    tt(khT[:], kq[:], e2[:], ALU.mult, ["kq", "e2"], ["khT"])
                    cis = range(NC2) if d_ == 0 else range(NC2 - 1, -1, -1)
                    for ci in cis:
                        sl = slice(ci * C2, (ci + 1) * C2)
                        mm(ps[0][0:64, 0:64], kT2[:, sl], qT[:, sl], True, True, ["kT2", "qT"], ["ps0"])
                        tt(sc2_[:], ps[0][0:64, 0:64], MI[d_][:], ALU.mult, ["ps0", f"mi{d_}"], ["sc2"])
                        tr(ps[1][0:64, 0:128], khT[:, sl], ident_f[:], ["khT", "identf"], ["ps1"])
                        cp(kh_tok[:], ps[1][0:64, 0:128], ["ps1"], ["kh_tok"], q="act")
                        mm(ps[2][0:64, 0:128], sc2_[:], vt2[:, ci, :], True, False, ["sc2", "vt2"], ["ps2"])
                        mm(ps[2][0:64, 0:128], qT[:, sl], S2[:, h, :], False, True, ["qT", "S2"], ["ps2"])
                        cp(o2[:, ci, h * 128:(h + 1) * 128], ps[2][0:64, 0:128], ["ps2"], ["o2"], q="act")
                        mm(ps[4][:, 0:128], kh_tok[:], vt2[:, ci, :], True, True, ["kh_tok", "vt2"], ["ps4"])
                        stt(S2[:, h, :], S2[:, h, :], pc2[:, ci:ci + 1], ps[4][:, 0:128], ALU.mult, ALU.add, ["S2", "pc2", "ps4"], ["S2"])
                for h4 in range(0, 8, NHS):
                    lists = []
                    for sid in range(NHS):
                        P.cap = []
                        P.kmap = mk_hkmap(h4 + sid, sid)
                        hg_body(h4 + sid, sid)
                        lists.append(P.cap)
                        P.cap = None
                        P.kmap = None
                    P.merge(lists)
                for ci in range(NC2):
                    r0 = t0 + ci * C2
                    if d_ == 0:
                        dma("dsp", of_d[r0:r0 + C2, :], o2[:, ci, :], [*O2K], ["of_d"])
                    else:
                        dma("dsp", of2[:], of_d[r0:r0 + C2, :], ["of_d"], ["of2"])
                        tt(o2[:, ci, :], o2[:, ci, :], of2[:], ALU.add, [*O2K, "of2"], [*O2K])
                        tt(sq2[:], o2[:, ci, :], o2[:, ci, :], ALU.mult, [*O2K], ["sq2"])
                        P.add("dve", lambda e: e.tensor_reduce(out=st2[:], in_=sq2[:].rearrange("p (h v) -> p h v", v=128), axis=AX.X, op=ALU.add), ["sq2"], ["st2"])
                        ts(st2[:], st2[:], 1.0 / 128, 1e-6, ALU.mult, ALU.add, ["st2"], ["st2"])
                        act(st2[:], st2[:], AF.Sqrt, ["st2"], ["st2"])
                        P.add("dve", lambda e: e.reciprocal(out=st2[:], in_=st2[:]), ["st2"], ["st2"])
                        o3 = o2[:, ci, :].rearrange("p (h v) -> p h v", v=128)
                        tt(o3, o3, st2[:].unsqueeze(2).to_broadcast([64, 8, 128]), ALU.mult, [*O2K, "st2"], [*O2K])
                        tt(o2[:, ci, :], o2[:, ci, :], hnw[:], ALU.mult, [*O2K, "hnw"], [*O2K])
                        act(gt2_[:, ci, :], gt2_[:, ci, :], AF.Silu, ["gt2"], ["gt2"])
                        tt(o2[:, ci, :], o2[:, ci, :], gt2_[:, ci, :], ALU.mult, [*O2K, "gt2"], [*O2K])
                        dma("dsp", m_d[r0:r0 + C2, 1024:2048], o2[:, ci, :], [*O2K], ["m_d"])
    P.barrier()
    P.pop()

    if DBG == "p4":
        dbg = dout("dbg", [T, 2048])
        dma("dsp", dbg, m_d, (), ["dbg"])
        P.barrier()
        P.emit()
        return nc

    w_out = din("w_out", [D, D])
    x1_d = dscr("x1_d", [T, D])
    P.push()
    wo = P.sb([128, 16, D], BF16, "wo")
    for k in range(16):
        dma("dpool", wo[:, k, :], w_out[k * 128:(k + 1) * 128, :], (), ["wo"])
    gn1 = [P.sb([128, D], F32, f"gn1_{s_}") for s_ in range(2)]
    for s_ in range(2):
        dma("dsp", gn1[s_][:], vec_d[2, s_:s_ + 1, :].partition_broadcast(128), (), [f"gn1_{s_}"])
    mt = [P.sb([128, D], F32, f"mt{i}") for i in range(2)]
    mTs = [P.sb([128, 16, 128], BF16, f"mTs{i}") for i in range(2)]
    mo = [P.sb([128, D], F32, f"mo{i}") for i in range(2)]
    xr = [P.sb([128, D], F32, f"xr{i}") for i in range(2)]
    junk5 = P.sb([128, D], F32, "junk5")
    ss5 = [P.sb([128, 1], F32, f"ss5{i}") for i in range(2)]
    for t in range(NT):
        b = t % 2
        s_ = seq_of_tile(t)
        dma("dsp", mt[b][:], m_d[t * 128:(t + 1) * 128, :], (), [f"mt{b}"])
        dma("dsp", xr[b][:], x[t * 128:(t + 1) * 128, :], (), [f"xr{b}"])
        for kg in range(4):
            pk = 4 + kg
            for k4 in range(4):
                k = kg * 4 + k4
                tr(ps[pk][:, k4 * 128:(k4 + 1) * 128], mt[b][:, k * 128:(k + 1) * 128], ident_f[:], [f"mt{b}", "identf"], [f"ps{pk}"])
            cp(mTs[b][:, kg * 4:(kg + 1) * 4, :].rearrange("p a b -> p (a b)"), ps[pk][:, :], [f"ps{pk}"], [f"mTs{b}"], q=("act" if kg % 2 else "dve"))
        for cg in range(4):
            for k in range(16):
                mm(ps[cg][:, :], mTs[b][:, k, :], wo[:, k, cg * 512:(cg + 1) * 512], k == 0, k == 15, [f"mTs{b}", "wo"], [f"ps{cg}"])
            cp(mo[b][:, cg * 512:(cg + 1) * 512], ps[cg][:, :], [f"ps{cg}"], [f"mo{b}"], q=("act" if cg % 2 else "dve"))
        act(junk5[:], mo[b][:], AF.Square, [f"mo{b}"], ["junk5", f"ss5{b}"], accum_out=ss5[b][:])
        ts(ss5[b][:], ss5[b][:], 1.0 / D, 1e-6, ALU.mult, ALU.add, [f"ss5{b}"], [f"ss5{b}"])
        act(ss5[b][:], ss5[b][:], AF.Sqrt, [f"ss5{b}"], [f"ss5{b}"])
        P.add("dve", lambda e, b=b: e.reciprocal(out=ss5[b][:], in_=ss5[b][:]), [f"ss5{b}"], [f"ss5{b}"])
        stt(mo[b][:], mo[b][:], ss5[b][:], gn1[s_][:], ALU.mult, ALU.mult, [f"mo{b}", f"ss5{b}", f"gn1_{s_}"], [f"mo{b}"])
        tt(mo[b][:], mo[b][:], xr[b][:], ALU.add, [f"mo{b}", f"xr{b}"], [f"mo{b}"])
        dma("dsp", x1_d[t * 128:(t + 1) * 128, :], mo[b][:], [f"mo{b}"], ["x1_d"])
    P.barrier()
    P.pop()

    w_router = din("w_router", [D, NE])
    e_bias = din("e_bias", [1, NE])
    h2T_d = dscr("h2T_d", [16, 128, T], BF16)
    wselT_d = dscr("wselT_d", [NE, T])
    P.push()
    g2 = [P.sb([128, D], F32, f"g2_{s_}") for s_ in range(2)]
    s2 = [P.sb([128, D], F32, f"s2_{s_}") for s_ in range(2)]
    for s_ in range(2):
        dma("dsp", g2[s_][:], vec_d[3, s_:s_ + 1, :].partition_broadcast(128), (), [f"g2_{s_}"])
        dma("dsp", s2[s_][:], vec_d[4, s_:s_ + 1, :].partition_broadcast(128), (), [f"s2_{s_}"])
    wr = P.sb([128, 16, NE], F32, "wr")
    dma("dsp", wr[:], w_router.rearrange("(k p) e -> p k e", p=128), (), ["wr"])
    eb = P.sb([128, NE], F32, "eb")
    dma("dsp", eb[:], e_bias[0:1, :].partition_broadcast(128), (), ["eb"])
    x6 = [P.sb([128, D], F32, f"x6{i}") for i in range(2)]
    junk6 = P.sb([128, D], F32, "junk6")
    hTf = [P.sb([128, 16, 128], F32, f"hTf{i}") for i in range(2)]
    hTb6 = [P.sb([128, 16, 128], BF16, f"hTb6{i}") for i in range(2)]
    ss6 = [P.sb([128, 1], F32, f"ss6{i}") for i in range(2)]
    scr = P.sb([128, NE], F32, "scr")
    bi = P.sb([128, NE], F32, "bi")
    m8 = P.sb([128, 8, 8], F32, "m8")
    gs = P.sb([128, 8], F32, "gs")
    g8 = P.sb([128, 8], F32, "g8")
    gmask = P.sb([128, 8], F32, "gmask")
    emask = P.sb([128, NE], F32, "emask")
    mk = P.sb([128, NE], F32, "mk")
    tmk = P.sb([128, NE], F32, "tmk")
    t8 = P.sb([128, 8], F32, "t8")
    sel = P.sb([128, NE], F32, "sel")
    wsl = P.sb([128, NE], F32, "wsl")
    wsum = P.sb([128, 1], F32, "wsum")
    wT = P.sb([64, 128], F32, "wT")
    for t in range(NT):
        b = t % 2
        s_ = seq_of_tile(t)
        dma("dsp", x6[b][:], x1_d[t * 128:(t + 1) * 128, :], (), [f"x6{b}"])
        act(junk6[:], x6[b][:], AF.Square, [f"x6{b}"], ["junk6", f"ss6{b}"], accum_out=ss6[b][:])
        ts(ss6[b][:], ss6[b][:], 1.0 / D, 1e-6, ALU.mult, ALU.add, [f"ss6{b}"], [f"ss6{b}"])
        act(ss6[b][:], ss6[b][:], AF.Sqrt, [f"ss6{b}"], [f"ss6{b}"])
        P.add("dve", lambda e, b=b: e.reciprocal(out=ss6[b][:], in_=ss6[b][:]), [f"ss6{b}"], [f"ss6{b}"])
        stt(x6[b][:], x6[b][:], ss6[b][:], g2[s_][:], ALU.mult, ALU.mult, [f"x6{b}", f"ss6{b}", f"g2_{s_}"], [f"x6{b}"])
        tt(x6[b][:], x6[b][:], s2[s_][:], ALU.add, [f"x6{b}", f"s2_{s_}"], [f"x6{b}"])
        for kg in range(4):
            for kk2 in range(4):
                k = kg * 4 + kk2
                tr(ps[kg][:, kk2 * 128:(kk2 + 1) * 128], x6[b][:, k * 128:(k + 1) * 128], ident_f[:], [f"x6{b}", "identf"], [f"ps{kg}"])
            cp(hTf[b][:, kg * 4:(kg + 1) * 4, :].rearrange("p a b -> p (a b)"), ps[kg][:, :], [f"ps{kg}"], [f"hTf{b}"], q="act")
            cp(hTb6[b][:, kg * 4:(kg + 1) * 4, :].rearrange("p a b -> p (a b)"), ps[kg][:, :], [f"ps{kg}"], [f"hTb6{b}"])
        dma("dsp", h2T_d[:, :, t * 128:(t + 1) * 128].rearrange("k p t -> p k t"), hTb6[b][:], [f"hTb6{b}"], ["h2T_d"])
        for k in range(16):
            mm(ps[4][:, 0:NE], hTf[b][:, k, :], wr[:, k, :], k == 0, k == 15, [f"hTf{b}", "wr"], ["ps4"])
        act(scr[:], ps[4][:, 0:NE], AF.Sigmoid, ["ps4"], ["scr"])
        tt(bi[:], scr[:], eb[:], ALU.add, ["scr", "eb"], ["bi"])
        for g_ in range(8):
            P.add("dve", lambda e, g_=g_: e.max(out=m8[:, g_, :], in_=bi[:, g_ * 8:(g_ + 1) * 8]), ["bi"], ["m8"])
        tt(gs[:], m8[:, :, 0], m8[:, :, 1], ALU.add, ["m8"], ["gs"])
        P.add("dve", lambda e: e.max(out=g8[:], in_=gs[:]), ["gs"], ["g8"])
        ts(gmask[:], gs[:], g8[:, 3:4], None, ALU.is_ge, None, ["gs", "g8"], ["gmask"])
        cp(emask[:].rearrange("p (g j) -> p g j", j=8), gmask[:].unsqueeze(2).to_broadcast([128, 8, 8]), ["gmask"], ["emask"])
        ts(tmk[:], emask[:], 10.0, -10.0, ALU.mult, ALU.add, ["emask"], ["tmk"])
        tt(mk[:], bi[:], emask[:], ALU.mult, ["bi", "emask"], ["mk"])
        tt(mk[:], mk[:], tmk[:], ALU.add, ["mk", "tmk"], ["mk"])
        P.add("dve", lambda e: e.max(out=t8[:], in_=mk[:]), ["mk"], ["t8"])
        ts(sel[:], mk[:], t8[:, 5:6], None, ALU.is_ge, None, ["mk", "t8"], ["sel"])
        tt(wsl[:], scr[:], sel[:], ALU.mult, ["scr", "sel"], ["wsl"])
        P.add("dve", lambda e: e.tensor_reduce(out=wsum[:], in_=wsl[:], axis=AX.X, op=ALU.add), ["wsl"], ["wsum"])
        P.add("dve", lambda e: e.reciprocal(out=wsum[:], in_=wsum[:]), ["wsum"], ["wsum"])
        ts(wsl[:], wsl[:], wsum[:], 2.5, ALU.mult, ALU.mult, ["wsl", "wsum"], ["wsl"])
        tr(ps[5][0:64, 0:128], wsl[:], ident_f[:], ["wsl", "identf"], ["ps5"])
        cp(wT[:], ps[5][0:64, 0:128], ["ps5"], ["wT"], q="act")
        dma("dsp", wselT_d[:, t * 128:(t + 1) * 128], wT[:], ["wT"], ["wselT_d"])
    P.barrier()
    P.pop()

    if DBG == "p6":
        dbg = dout("dbg", [NE, T])
        dma("dsp", dbg, wselT_d, (), ["dbg"])
        P.barrier()
        P.emit()
        return nc

    NEC = DE // 128
    TBM = TB
    NTI = TBM // 128
    P.push()
    h2b = P.sb([128, 16, TBM], BF16, "h2b")
    wgt = [P.sb([128, 16, DE], BF16, f"wgt{i}") for i in range(2)]
    wut = [P.sb([128, 16, DE], BF16, f"wut{i}") for i in range(2)]
    wdt = [P.sb([128, NEC, D], BF16, f"wdt{i}") for i in range(2)]
    wbt = [P.sb([128, TBM], F32, f"wbt{i}") for i in range(2)]
    yacc = P.sb([128, NTI, D], F32, "yacc")
    actT = [P.sb([128, NEC, TBM], BF16, f"actT{i}") for i in range(2)]
    sgt = [P.sb([128, TBM], F32, f"sgt{i}") for i in range(2)]
    a1t = [P.sb([128, TBM], F32, f"a1t{i}") for i in range(2)]
    gn2 = P.sb([128, D], F32, "gn2")
    x7 = P.sb([128, D], F32, "x7")
    ss7 = P.sb([128, 1], F32, "ss7")
    junk7 = P.sb([128, D], F32, "junk7")

    def wsrc(e):
        return ("persist:wg%d" % e, "persist:wu%d" % e, "persist:wd%d" % e)

    def load_w(e, par):
        kg_, ku_, kd_ = wsrc(e)
        dma("dsp", wgt[par][:].rearrange("p k n -> p (k n)"), wg_b[e], [kg_], [f"wgt{par}"])
        dma("dsp", wut[par][:].rearrange("p k n -> p (k n)"), wu_b[e], [ku_], [f"wut{par}"])
        dma("dsp", wdt[par][:].rearrange("p k n -> p (k n)"), wd_b[e], [kd_], [f"wdt{par}"])

    order = [NE] + list(range(NE))
    gi = 0
    for tb in range(T // TBM):
        t0 = tb * TBM
        s_ = 0 if t0 < LP else 1
        dma("dsp", h2b[:], h2T_d[:, :, t0:t0 + TBM].rearrange("k p t -> p k t"), (), ["h2b"])
        dma("dsp", gn2[:], vec_d[5, s_:s_ + 1, :].partition_broadcast(128), (), ["gn2"])
        load_w(order[0], gi % 2)

        def GU(i, e, par):
            if e < NE:
                dma("dsp", wbt[par][:], wselT_d[e:e + 1, t0:t0 + TBM].partition_broadcast(128), (), [f"wbt{par}"])
            for ec in range(NEC):
                pg = ps[ec % 2]
                pu = ps[2 + ec % 2]
                kg_ = f"ps{ec % 2}"
                ku_ = f"ps{2 + ec % 2}"
                for k in range(16):
                    mm(pg[:, 0:TBM], wgt[par][:, k, ec * 128:(ec + 1) * 128], h2b[:, k, :], k == 0, k == 15, [f"wgt{par}", "h2b"], [kg_])
                for k in range(16):
                    mm(pu[:, 0:TBM], wut[par][:, k, ec * 128:(ec + 1) * 128], h2b[:, k, :], k == 0, k == 15, [f"wut{par}", "h2b"], [ku_])
                sb_ = ec % 2
                act(sgt[sb_][:], pg[:, 0:TBM], AF.Silu, [kg_], [f"sgt{sb_}"])
                if e < NE:
                    tt(a1t[sb_][:], sgt[sb_][:], pu[:, 0:TBM], ALU.mult, [f"sgt{sb_}", ku_], [f"a1t{sb_}"])
                    tt(actT[par][:, ec, :], a1t[sb_][:], wbt[par][:], ALU.mult, [f"a1t{sb_}", f"wbt{par}"], [f"actT{par}"])
                else:
                    tt(actT[par][:, ec, :], sgt[sb_][:], pu[:, 0:TBM], ALU.mult, [f"sgt{sb_}", ku_], [f"actT{par}"])

        def DN(i, e, par, first):
            j = 0
            for ti in range(NTI):
                for cg in range(4):
                    pd = ps[4 + j % 2]
                    kd_ = f"ps{4 + j % 2}"
                    j += 1
                    for ec in range(NEC):
                        mm(pd[:, :], actT[par][:, ec, ti * 128:(ti + 1) * 128], wdt[par][:, ec, cg * 512:(cg + 1) * 512],
                           ec == 0, ec == NEC - 1, [f"actT{par}", f"wdt{par}"], [kd_])
                    if first:
                        cp(yacc[:, ti, cg * 512:(cg + 1) * 512], pd[:, :], [kd_], ["yacc"])
                    else:
                        tt(yacc[:, ti, cg * 512:(cg + 1) * 512], yacc[:, ti, cg * 512:(cg + 1) * 512], pd[:, :], ALU.add, ["yacc", kd_], ["yacc"])

        n_e = len(order)
        for i, e in enumerate(order):
            par = (gi + i) % 2
            GU(i, e, par)
            if i > 0:
                DN(i - 1, order[i - 1], (gi + i - 1) % 2, i - 1 == 0)
            if i + 1 < n_e:
                load_w(order[i + 1], (gi + i + 1) % 2)
        DN(n_e - 1, order[-1], (gi + n_e - 1) % 2, False)
        gi += n_e
        for ti in range(NTI):
            r0 = t0 + ti * 128
            dma("dsp", x7[:], x1_d[r0:r0 + 128, :], (), ["x7"])
            P.add("dve", lambda e, ti=ti: e.tensor_tensor(out=junk7[:], in0=yacc[:, ti, :], in1=yacc[:, ti, :], op=ALU.mult), ["yacc"], ["junk7"])
            P.add("dve", lambda e: e.tensor_reduce(out=ss7[:], in_=junk7[:], axis=AX.X, op=ALU.add), ["junk7"], ["ss7"])
            ts(ss7[:], ss7[:], 1.0 / D, 1e-6, ALU.mult, ALU.add, ["ss7"], ["ss7"])
            act(ss7[:], ss7[:], AF.Sqrt, ["ss7"], ["ss7"])
            P.add("dve", lambda e: e.reciprocal(out=ss7[:], in_=ss7[:]), ["ss7"], ["ss7"])
            stt(junk7[:], yacc[:, ti, :], ss7[:], gn2[:], ALU.mult, ALU.mult, ["yacc", "ss7", "gn2"], ["junk7"])
            tt(junk7[:], junk7[:], x7[:], ALU.add, ["junk7", "x7"], ["junk7"])
            dma("dsp", y[r0:r0 + 128, :], junk7[:], ["junk7"], ["y"])
    P.barrier()
    P.pop()


    P.barrier()
    P.emit()
    return nc


def kernel(**inp):
    from concourse.bass_utils import run_bass_kernel_spmd
    n = 8
    LP = inp["x_prompt"].shape[1]
    LS = inp["x_sample"].shape[1]
    nc = build_program(dict(LP=LP, LS=LS, DE=int(inp["w_exp_gate"].shape[-1])))
    f = lambda a: np.ascontiguousarray(np.asarray(a, dtype=np.float32))
    shared = dict(
        w_ada=f(inp["w_ada"][0]), b_ada=f(inp["b_ada"][0:1]),
        nrm=f(np.stack([inp["norm_pre_mix"][0], inp["norm_post_mix"][0], inp["norm_pre_ffn"][0], inp["norm_post_ffn"][0]])),
        w_in=f(inp["w_in"][0]), rw_mu=f(inp["rw_mu"][0]), rw_w0=f(inp["rw_w0"][0]), rw_a0=f(inp["rw_a0"][0]),
        rw_w_up=f(inp["rw_w_up"][0]), rw_a_up=f(inp["rw_a_up"][0]), rw_g_up=f(inp["rw_g_up"][0]),
        rw_vecs=f(np.stack([inp["rw_k_k"][0], inp["rw_k_a"][0], inp["rw_r_k"][0].reshape(-1), inp["rw_ln_w"][0], inp["rw_ln_b"][0]])),
        hg_gamma=f(inp["hg_lb_gamma"]), hg_norm_w=f(inp["hg_norm_w"][0:1]), w_out=f(inp["w_out"][0]),
        w_router=f(inp["w_router"][0]), e_bias=f(inp["e_bias"][0:1]),
        w_exp_gate=f(inp["w_exp_gate"][0]), w_exp_up=f(inp["w_exp_up"][0]), w_exp_down=f(inp["w_exp_down"][0]),
        w_sh_gate=f(inp["w_sh_gate"][0]), w_sh_up=f(inp["w_sh_up"][0]), w_sh_down=f(inp["w_sh_down"][0]),
    )
    in_maps = []
    for b in range(n):
        m = dict(shared)
        m["x"] = f(np.concatenate([inp["x_prompt"][b], inp["x_sample"][b]], axis=0))
        m["c"] = f(np.stack([inp["c_prompt"][b], inp["c_sample"][b]]))
        in_maps.append(m)
    res = run_bass_kernel_spmd(nc, in_maps, core_ids=list(range(n)))
    ys = [r["y"] for r in res.results]
    y_prompt = np.stack([yy[:LP] for yy in ys]).astype(np.float32)
    y_sample = np.stack([yy[LP:] for yy in ys]).astype(np.float32)
    return (y_prompt, y_sample)
```

```python
import numpy as np
import concourse.bass as bass
import concourse.mybir as mybir

F32 = mybir.dt.float32
BF16 = mybir.dt.bfloat16
I32 = mybir.dt.int32
AF = mybir.ActivationFunctionType
ALU = mybir.AluOpType
AX = mybir.AxisListType

NDMASEM = 8


class Op:
    __slots__ = ("q", "fn", "deps", "signals", "sem", "ticket", "idx", "prewait", "isbar")

    def __init__(self, q, fn):
        self.q = q
        self.fn = fn
        self.deps = []
        self.signals = False
        self.sem = None
        self.ticket = 0
        self.prewait = None
        self.isbar = False


class Prog:
    ENG = {"pe": "pe", "act": "act", "dve": "dve", "pool": "pool",
           "dsp": "sp", "dpool": "pool", "dact": "act", "dpool2": "pool"}
    DMAQ = ("dsp", "dpool", "dact", "dpool2")

    def __init__(self, nc):
        self.nc = nc
        self.ops = []
        self.last_w = {}
        self.readers = {}
        self.sb_cur = 16512
        self.sb_mark = []
        self.uid = 0
        self.limit = None
        self.cap = None
        self.kmap = None

    def sb(self, shape, dtype, name=None):
        self.uid += 1
        nm = f"{name or 't'}_{self.uid}"
        esz = {F32: 4, BF16: 2, I32: 4, mybir.dt.uint32: 4, mybir.dt.uint16: 2}[dtype]
        per = int(np.prod(shape[1:])) * esz
        per = (per + 31) // 32 * 32
        off = self.sb_cur
        assert off + per <= 229344, f"SBUF overflow allocating {nm}: {off}+{per}"
        self.sb_cur += per
        return self.nc.alloc_sbuf_tensor_at(nm, list(shape), dtype, offset=off)

    def push(self):
        self.sb_mark.append(self.sb_cur)

    def pop(self):
        self.sb_cur = self.sb_mark.pop()

    def add(self, q, fn, reads=(), writes=()):
        if self.kmap is not None:
            reads = [self.kmap.get(k, k) for k in reads]
            writes = [self.kmap.get(k, k) for k in writes]
        if self.cap is not None:
            self.cap.append((q, fn, tuple(reads), tuple(writes)))
            return None
        if self.limit is not None and len(self.ops) >= self.limit:
            return None
        op = Op(q, fn)
        op.idx = len(self.ops)
        if self.limit is not None:
            import sys as _s
            f = _s._getframe(1)
            ln = []
            while f is not None and len(ln) < 3:
                ln.append(f.f_lineno)
                f = f.f_back
            self.lines = getattr(self, "lines", {})
            self.lines[op.idx] = (q, ln)
        deps = set()
        for k in reads:
            if k in self.last_w:
                deps.add(self.last_w[k])
            if isinstance(k, str) and k.startswith("ps"):
                for r in self.readers.get(k, ()):
                    if self.ops[r].q != q:
                        deps.add(r)
        for k in writes:
            if k in self.last_w:
                deps.add(self.last_w[k])
            for r in self.readers.get(k, ()):
                deps.add(r)
        deps.discard(op.idx)
        if q == "pe":
            deps = {d for d in deps if self.ops[d].q != "pe"}
        op.deps = sorted(deps)
        for k in reads:
            self.readers.setdefault(k, []).append(op.idx)
        for k in writes:
            self.last_w[k] = op.idx
            self.readers[k] = []
        self.ops.append(op)
        return op

    def merge(self, lists):
        n = max(len(l) for l in lists)
        for i in range(n):
            for l in lists:
                if i < len(l):
                    self.add(*l[i])

    def barrier(self):
        op = Op("bar", None)
        op.idx = len(self.ops)
        op.isbar = True
        self.ops.append(op)
        self.last_w = {k: v for k, v in self.last_w.items() if isinstance(k, str) and k.startswith("persist:")}
        self.readers = {k: v for k, v in self.readers.items() if isinstance(k, str) and k.startswith("persist:")}

    def emit(self):
        nc = self.nc
        ops = self.ops
        queues = ["pe", "act", "dve", "pool", "dsp", "dpool", "dact", "dpool2"]
        hist = {q: [] for q in queues}
        bar_deps = {}
        for op in ops:
            if op.isbar:
                deps = []
                for q in ("pe", "act", "dve", "pool"):
                    deps += hist[q][-1:]
                for q in ("dsp", "dpool", "dact"):
                    deps += hist[q][-NDMASEM:]
                bar_deps[op.idx] = deps
                for d in deps:
                    ops[d].signals = True
            else:
                hist[op.q].append(op.idx)
        for op in ops:
            for d in op.deps:
                ops[d].signals = True
        sem_c = {q: nc.alloc_semaphore(f"s_{q}") for q in ("pe", "act", "dve", "pool")}
        sem_d = {q: [nc.alloc_semaphore(f"s_{q}{i}") for i in range(NDMASEM)]
                 for q in self.DMAQ}
        cnt_c = {q: 0 for q in sem_c}
        cnt_d = {q: [0] * NDMASEM for q in sem_d}
        rr = {q: 0 for q in sem_d}
        for op in ops:
            if op.isbar:
                continue
            if op.q in sem_c:
                if op.signals:
                    cnt_c[op.q] += 1
                    op.sem = sem_c[op.q]
                    op.ticket = cnt_c[op.q]
            else:
                i = rr[op.q]
                rr[op.q] = (i + 1) % NDMASEM
                op.prewait = (sem_d[op.q][i], cnt_d[op.q][i])
                cnt_d[op.q][i] += 16
                op.sem = sem_d[op.q][i]
                op.ticket = cnt_d[op.q][i]
                op.signals = True
        streams = {"pe": [], "act": [], "dve": [], "pool": [], "sp": []}
        for op in ops:
            if op.isbar:
                for e in streams:
                    streams[e].append(op)
            else:
                streams[self.ENG[op.q]].append(op)
        self.n_wait = 0

        def run_stream(eng, lst):
            waited = {}

            def w(sem, val):
                if val <= 0:
                    return
                if waited.get(sem.num, 0) >= val:
                    return
                eng.wait_ge(sem, val)
                self.n_wait += 1
                waited[sem.num] = val

            for op in lst:
                if op.isbar:
                    for d in bar_deps[op.idx]:
                        w(ops[d].sem, ops[d].ticket)
                    continue
                for d in op.deps:
                    w(ops[d].sem, ops[d].ticket)
                if op.prewait is not None:
                    w(*op.prewait)
                ins = op.fn(eng)
                if op.signals:
                    ins.then_inc(op.sem, 16 if op.q in self.DMAQ else 1)
            return waited

        with nc.Block() as block:
            @block.tensor
            def _(e):
                run_stream(e, streams["pe"])

            @block.scalar
            def _(e):
                run_stream(e, streams["act"])

            @block.vector
            def _(e):
                run_stream(e, streams["dve"])

            @block.gpsimd
            def _(e):
                run_stream(e, streams["pool"])

            @block.sync
            def _(e):
                run_stream(e, streams["sp"])
D = 2048
RW = 1024
HG = 1024
RWC = 3456
INC = 8576
NE = 64


def build_program(cfg):
    LP, LS = cfg["LP"], cfg["LS"]
    DE = cfg.get("DE", 512)
    DBG = cfg.get("debug", None)
    T = LP + LS
    NT = T // 128
    nc = bass.Bass("TRN2", target_bir_lowering=False)
    P = Prog(nc)
    P.limit = cfg.get("limit", None)
    global LAST_PROG
    LAST_PROG = P

    def din(name, shape, dt=F32):
        return nc.dram_tensor(name, list(shape), dt, kind="ExternalInput").ap()

    def dout(name, shape, dt=F32):
        return nc.dram_tensor(name, list(shape), dt, kind="ExternalOutput").ap()

    def dscr(name, shape, dt=F32):
        return nc.dram_tensor(name, list(shape), dt, kind="Internal").ap()

    x = din("x", [T, D])
    c = din("c", [2, D])
    w_ada = din("w_ada", [D, 6 * D])
    b_ada = din("b_ada", [1, 6 * D])
    nrm = din("nrm", [4, D])
    w_in = din("w_in", [D, INC])
    y = dout("y", [T, D])

    vec_d = dscr("vec_d", [6, 2, D])
    hT_d = dscr("hT_d", [16, 128, T], BF16)

    ident_f = P.sb([128, 128], F32, "identf")
    ident_b = P.sb([128, 128], BF16, "identb")
    ps = [nc.alloc_psum_tensor(f"ps{i}", [128, 512], F32) for i in range(8)]

    def dma(q, out, in_, reads=(), writes=(), **kw):
        return P.add(q, lambda e: e.dma_start(out=out, in_=in_, **kw), reads, writes)

    def act(out, in_, func, reads, writes, bias=None, scale=1.0, accum_out=None):
        kw = {}
        if bias is not None:
            kw["bias"] = bias
        if accum_out is not None:
            kw["accum_out"] = accum_out
        return P.add("act", lambda e: e.activation(out=out, in_=in_, func=func, scale=scale, **kw), reads, writes)

    def tt(out, in0, in1, op, reads, writes, q="dve"):
        return P.add(q, lambda e: e.tensor_tensor(out=out, in0=in0, in1=in1, op=op), reads, writes)

    def ts(out, in0, s1, s2, op0, op1, reads, writes, q="dve", accum_out=None):
        kw = {}
        if accum_out is not None:
            kw["accum_out"] = accum_out
        if op1 is None:
            return P.add(q, lambda e: e.tensor_scalar(out=out, in0=in0, scalar1=s1, scalar2=None, op0=op0, **kw), reads, writes)
        return P.add(q, lambda e: e.tensor_scalar(out=out, in0=in0, scalar1=s1, scalar2=s2, op0=op0, op1=op1, **kw), reads, writes)

    def stt(out, in0, scalar, in1, op0, op1, reads, writes):
        return P.add("dve", lambda e: e.scalar_tensor_tensor(out=out, in0=in0, scalar=scalar, in1=in1, op0=op0, op1=op1), reads, writes)

    def cp(out, in_, reads, writes, q="dve"):
        if q == "act":
            return P.add("act", lambda e: e.copy(out=out, in_=in_), reads, writes)
        return P.add(q, lambda e: e.tensor_copy(out=out, in_=in_), reads, writes)

    def mm(out, lhsT, rhs, start, stop, reads, writes):
        return P.add("pe", lambda e: e.matmul(out, lhsT, rhs, start=start, stop=stop), reads, writes)

    def tr(out, in_, ident, reads, writes):
        return P.add("pe", lambda e: e.transpose(out, in_, ident), reads, writes)

    def memset(ap, val, writes, q="pool"):
        return P.add(q, lambda e: e.memset(ap, val), (), writes)

    memset(ident_f[:], 0.0, ["identf"])
    P.add("pool", lambda e: e.affine_select(out=ident_f[:], in_=ident_f[:], pattern=[[-1, 128]],
                                            compare_op=ALU.not_equal, fill=1.0, base=0, channel_multiplier=1),
          ["identf"], ["identf"])
    cp(ident_b[:], ident_f[:], ["identf"], ["identb"])

    P.push()
    cT = P.sb([128, 16, 2], F32, "cT")
    scT = P.sb([128, 16, 2], F32, "scT")
    mod = P.sb([2, 6 * D], F32, "mod")
    bb = [P.sb([2, 512], F32, f"bb{i}") for i in range(2)]
    nr = P.sb([2, 4, D], F32, "nr")
    wa = [P.sb([128, 16, 512], F32, f"wa{i}") for i in range(2)]
    for s_ in range(2):
        dma("dsp", cT[:, :, s_], c[s_, :].rearrange("(k p) -> p k", p=128), (), ["cT"], allow_slow_non_contiguous=True)
    dma("dsp", nr[:], nrm.rearrange("(o f) d -> o f d", o=1).partition_broadcast(2), (), ["nr"])
    act(scT[:], cT[:], AF.Silu, ["cT"], ["scT"])
    for cg in range(24):
        b = cg % 2
        dma("dsp", wa[b][:], w_ada[:, cg * 512:(cg + 1) * 512].rearrange("(k p) n -> p k n", p=128), (), [f"wa{b}"])
        dma("dsp", bb[b][:], b_ada[0:1, cg * 512:(cg + 1) * 512].partition_broadcast(2), (), [f"bb{b}"])
        for k in range(16):
            mm(ps[0][0:2, :], scT[:, k, :], wa[b][:, k, :], k == 0, k == 15, ["scT", f"wa{b}"], ["ps0"])
        tt(mod[:, cg * 512:(cg + 1) * 512], ps[0][0:2, :], bb[b][:], ALU.add,
           ["ps0", f"bb{b}"], ["mod"])
    vecs = P.sb([2, 6, D], F32, "vecs")
    stt(vecs[:, 0, :], mod[:, D:2 * D], 1.0, nr[:, 0, :], ALU.add, ALU.mult, ["mod", "nr"], ["vecs"])
    cp(vecs[:, 1, :], mod[:, 0:D], ["mod"], ["vecs"])
    tt(vecs[:, 2, :], mod[:, 2 * D:3 * D], nr[:, 1, :], ALU.mult, ["mod", "nr"], ["vecs"])
    stt(vecs[:, 3, :], mod[:, 4 * D:5 * D], 1.0, nr[:, 2, :], ALU.add, ALU.mult, ["mod", "nr"], ["vecs"])
    cp(vecs[:, 4, :], mod[:, 3 * D:4 * D], ["mod"], ["vecs"])
    tt(vecs[:, 5, :], mod[:, 5 * D:6 * D], nr[:, 3, :], ALU.mult, ["mod", "nr"], ["vecs"])
    dma("dsp", vec_d.rearrange("f s d -> s f d"), vecs[:], ["vecs"], ["vec_d"])
    P.barrier()
    P.pop()

    def seq_of_tile(t):
        return 0 if t * 128 < LP else 1

    P.push()
    g1 = [P.sb([128, D], F32, f"g1_{s}") for s in range(2)]
    s1 = [P.sb([128, D], F32, f"s1_{s}") for s in range(2)]
    for s in range(2):
        dma("dsp", g1[s][:], vec_d[0, s:s + 1, :].partition_broadcast(128), (), [f"g1_{s}"])
        dma("dsp", s1[s][:], vec_d[1, s:s + 1, :].partition_broadcast(128), (), [f"s1_{s}"])
    xt = [P.sb([128, D], F32, f"xt{i}") for i in range(2)]
    junk = P.sb([128, D], F32, "junk")
    hTs = [P.sb([128, 16, 128], BF16, f"hTs{i}") for i in range(2)]
    ss = [P.sb([128, 1], F32, f"ss{i}") for i in range(2)]
    rs = [P.sb([128, 1], F32, f"rs{i}") for i in range(2)]
    for t in range(NT):
        b = t % 2
        s = seq_of_tile(t)
        dma("dsp", xt[b][:], x[t * 128:(t + 1) * 128, :], (), [f"xt{b}"])
        act(junk[:], xt[b][:], AF.Square, [f"xt{b}"], ["junk", f"ss{b}"], accum_out=ss[b][:])
        ts(rs[b][:], ss[b][:], 1.0 / D, 1e-6, ALU.mult, ALU.add, [f"ss{b}"], [f"rs{b}"])
        act(rs[b][:], rs[b][:], AF.Sqrt, [f"rs{b}"], [f"rs{b}"])
        P.add("dve", lambda e, b=b: e.reciprocal(out=rs[b][:], in_=rs[b][:]), [f"rs{b}"], [f"rs{b}"])
        stt(xt[b][:], xt[b][:], rs[b][:], g1[s][:], ALU.mult, ALU.mult, [f"xt{b}", f"rs{b}", f"g1_{s}"], [f"xt{b}"])
        tt(xt[b][:], xt[b][:], s1[s][:], ALU.add, [f"xt{b}", f"s1_{s}"], [f"xt{b}"])
        for kg in range(4):
            pk = (t * 4 + kg) % 8
            for k4 in range(4):
                k = kg * 4 + k4
                tr(ps[pk][:, k4 * 128:(k4 + 1) * 128], xt[b][:, k * 128:(k + 1) * 128], ident_f[:], [f"xt{b}", "identf"], [f"ps{pk}"])
            cp(hTs[b][:, kg * 4:(kg + 1) * 4, :].rearrange("p a b -> p (a b)"), ps[pk][:, :], [f"ps{pk}"], [f"hTs{b}"], q=("act" if kg % 2 else "dve"))
        dma("dsp", hT_d[:, :, t * 128:(t + 1) * 128].rearrange("k p t -> p k t"), hTs[b][:], [f"hTs{b}"], ["hT_d"])
    P.barrier()
    P.pop()

    if DBG == "p1":
        dbg = dout("dbg", [16, 128, T], BF16)
        dma("dsp", dbg, hT_d, (), ["dbg"])
        dbg2 = dout("dbg2", [6, 2, D])
        dma("dsp", dbg2, vec_d, (), ["dbg2"])
        P.barrier()
        P.emit()
        return nc

    TB = 512 if (LP % 512 == 0 and LS % 512 == 0) else 128
    NF = 6528
    win_b = dscr("win_b", [D, INC], BF16)
    zT_d = dscr("zT_d", [NF, T])
    ztok_d = dscr("ztok_d", [T, 2048])
    for k in range(16):
        dma("dpool", win_b[k * 128:(k + 1) * 128, :], w_in[k * 128:(k + 1) * 128, :], (), ["win_b"])
    P.barrier()
    DS = DE
    w_exp_gate = din("w_exp_gate", [NE, D, DE])
    w_exp_up = din("w_exp_up", [NE, D, DE])
    w_exp_down = din("w_exp_down", [NE, DE, D])
    w_sh_gate = din("w_sh_gate", [D, DS])
    w_sh_up = din("w_sh_up", [D, DS])
    w_sh_down = din("w_sh_down", [DS, D])
    wg_b = dscr("wg_b", [NE + 1, 128, 16 * DE], BF16)
    wu_b = dscr("wu_b", [NE + 1, 128, 16 * DE], BF16)
    wd_b = dscr("wd_b", [NE + 1, 128, (DE // 128) * D], BF16)
    for e_ in ([] if DBG in ("p2", "p3", "p4") else [NE] + list(range(NE))):
        sg_ = w_sh_gate if e_ == NE else w_exp_gate[e_]
        su_ = w_sh_up if e_ == NE else w_exp_up[e_]
        sd_ = w_sh_down if e_ == NE else w_exp_down[e_]
        dma("dpool2", wg_b[e_].rearrange("p (k n) -> p k n", k=16), sg_.rearrange("(k p) n -> p k n", p=128), (), ["persist:wg%d" % e_])
        dma("dpool2", wu_b[e_].rearrange("p (k n) -> p k n", k=16), su_.rearrange("(k p) n -> p k n", p=128), (), ["persist:wu%d" % e_])
        dma("dpool2", wd_b[e_].rearrange("p (k n) -> p k n", k=DE // 128), sd_.rearrange("(k p) n -> p k n", p=128), (), ["persist:wd%d" % e_])
    P.push()
    hTb = [P.sb([128, 16, TB], BF16, f"hTb{i}") for i in range(2)]
    wt = [P.sb([128, 16, 512], BF16, f"wt{i}") for i in range(2)]
    zs = [P.sb([128, 512], F32, f"zs{i}") for i in range(3)]
    groups = [(i * 512, 512, "F") for i in range(12)] + [(6144, 384, "F")] + [(6528 + i * 512, 512, "T") for i in range(4)]
    it = 0
    zi = 0
    for tb in range(T // TB):
        hb_ = tb % 2
        dma("dsp", hTb[hb_][:], hT_d[:, :, tb * TB:(tb + 1) * TB].rearrange("k p t -> p k t"), (), [f"hTb{hb_}"])
        for (c0, ncol, mode) in groups:
            wb = it % 2
            it += 1
            dma("dsp", wt[wb][:, :, 0:ncol], win_b[:, c0:c0 + ncol].rearrange("(k p) n -> p k n", p=128), (), [f"wt{wb}"])
            if mode == "F":
                for ci in range(ncol // 128):
                    pi = zi % 4
                    z_ = zi % 3
                    zi += 1
                    for k in range(16):
                        mm(ps[pi][:, 0:TB], wt[wb][:, k, ci * 128:(ci + 1) * 128], hTb[hb_][:, k, :], k == 0, k == 15,
                           [f"wt{wb}", f"hTb{hb_}"], [f"ps{pi}"])
                    cp(zs[z_][:, 0:TB], ps[pi][:, 0:TB], [f"ps{pi}"], [f"zs{z_}"], q=("act" if zi % 2 else "dve"))
                    dma("dsp", zT_d[c0 + ci * 128:c0 + (ci + 1) * 128, tb * TB:(tb + 1) * TB], zs[z_][:, 0:TB], [f"zs{z_}"], ["zT_d"])
            else:
                for ti in range(TB // 128):
                    pi = zi % 4
                    z_ = zi % 3
                    zi += 1
                    for k in range(16):
                        mm(ps[pi][:, :], hTb[hb_][:, k, ti * 128:(ti + 1) * 128], wt[wb][:, k, :], k == 0, k == 15,
                           [f"wt{wb}", f"hTb{hb_}"], [f"ps{pi}"])
                    cp(zs[z_][:, :], ps[pi][:, :], [f"ps{pi}"], [f"zs{z_}"], q=("act" if zi % 2 else "dve"))
                    dma("dsp", ztok_d[tb * TB + ti * 128:tb * TB + (ti + 1) * 128, c0 - 6528:c0 - 6528 + 512], zs[z_][:, :], [f"zs{z_}"], ["ztok_d"])
    P.barrier()
    P.pop()

    if DBG == "p2":
        dbg = dout("dbg", [NF, T])
        dma("dsp", dbg, zT_d, (), ["dbg"])
        dbg2 = dout("dbg2", [T, 2048])
        dma("dsp", dbg2, ztok_d, (), ["dbg2"])
        P.barrier()
        P.emit()
        return nc

    if DBG is not None:
        print("ops before P3:", len(P.ops))
    rw_mu = din("rw_mu", [RWC])
    rw_w0 = din("rw_w0", [2, RW])
    rw_a0 = din("rw_a0", [2, RW])
    rw_w_up = din("rw_w_up", [2, 64, RW])
    rw_a_up = din("rw_a_up", [2, 64, RW])
    rw_g_up = din("rw_g_up", [128, RW])
    rw_vecs = din("rw_vecs", [5, RW])
    m_d = dscr("m_d", [T, 2048])
    of_d = dscr("of_d", [T, RW])
    P.push()
    CH = 128
    TB_saved = TB
    TB = min(TB, 256)
    NCB = TB // CH
    muT = P.sb([64, 54], F32, "muT")
    ommT = P.sb([64, 54], F32, "ommT")
    hmuT = P.sb([64, 54], F32, "hmuT")
    dma("dsp", muT[:], rw_mu.rearrange("(j p) -> p j", p=64), (), ["muT"], allow_slow_non_contiguous=True)
    ts(ommT[:], muT[:], -1.0, 1.0, ALU.mult, ALU.add, ["muT"], ["ommT"])
    ts(hmuT[:], muT[:], 0.5, None, ALU.mult, None, ["muT"], ["hmuT"])
    mug = P.sb([128, 3], F32, "mug")
    dma("dsp", mug[:, 0:1], rw_mu[3328:3456].rearrange("(p o) -> p o", o=1), (), ["mug"])
    ts(mug[:, 1:2], mug[:, 0:1], -1.0, 1.0, ALU.mult, ALU.add, ["mug"], ["mug"])
    ts(mug[:, 2:3], mug[:, 0:1], 0.5, None, ALU.mult, None, ["mug"], ["mug"])
    w0T = P.sb([64, 2, 16], F32, "w0T")
    a0T = P.sb([64, 2, 16], F32, "a0T")
    for d_ in range(2):
        dma("dsp", w0T[:, d_, :], rw_w0[d_, :].rearrange("(h p) -> p h", p=64), (), ["w0T"], allow_slow_non_contiguous=True)
        dma("dsp", a0T[:, d_, :], rw_a0[d_, :].rearrange("(h p) -> p h", p=64), (), ["a0T"], allow_slow_non_contiguous=True)
    vT = P.sb([64, 5, 16], F32, "vT")
    for i_ in range(3):
        dma("dsp", vT[:, i_, :], rw_vecs[i_, :].rearrange("(h p) -> p h", p=64), (), ["vT"], allow_slow_non_contiguous=True)
    omka = P.sb([64, 16], F32, "omka")
    ts(omka[:], vT[:, 1, :], -1.0, 1.0, ALU.mult, ALU.add, ["vT"], ["omka"])
    lnw_b = P.sb([128, RW], F32, "lnw_b")
    lnb_b = P.sb([128, RW], F32, "lnb_b")
    rk_b = P.sb([128, RW], F32, "rk_b")
    dma("dsp", rk_b[:], rw_vecs[2:3, :].partition_broadcast(128), (), ["rk_b"])
    dma("dsp", lnw_b[:], rw_vecs[3:4, :].partition_broadcast(128), (), ["lnw_b"])
    dma("dsp", lnb_b[:], rw_vecs[4:5, :].partition_broadcast(128), (), ["lnb_b"])
    wup = P.sb([64, 2, RW], BF16, "wup")
    aup = P.sb([64, 2, RW], BF16, "aup")
    gup = P.sb([128, RW], BF16, "gup")
    for d_ in range(2):
        dma("dpool", wup[:, d_, :], rw_w_up[d_], (), ["wup"])
        dma("dpool", aup[:, d_, :], rw_a_up[d_], (), ["aup"])
    dma("dpool", gup[:], rw_g_up, (), ["gup"])
    ones_f = P.sb([128, 128], F32, "ones_f")
    memset(ones_f[:], 1.0, ["ones_f"])
    MASK2 = [P.sb([128, 256], F32, f"mask2_{d_}") for d_ in range(2)]
    MASKX = [P.sb([128, 128], F32, f"maskx_{d_}") for d_ in range(2)]
    for d_ in range(2):
        memset(MASK2[d_][:], 1.0, [f"mask2_{d_}"])
        memset(MASKX[d_][:], 1.0, [f"maskx_{d_}"])
        sgn = 1 if d_ == 0 else -1
        P.add("pool", lambda e, d_=d_, sgn=sgn: e.affine_select(out=MASK2[d_][:, 0:128], in_=MASK2[d_][:, 0:128], pattern=[[sgn, 128]],
              compare_op=ALU.is_gt, fill=0.0, base=0, channel_multiplier=-sgn), [f"mask2_{d_}"], [f"mask2_{d_}"])
        P.add("pool", lambda e, d_=d_, sgn=sgn: e.affine_select(out=MASK2[d_][:, 128:256], in_=MASK2[d_][:, 128:256], pattern=[[sgn, 128]],
              compare_op=ALU.is_ge, fill=0.0, base=0, channel_multiplier=-sgn), [f"mask2_{d_}"], [f"mask2_{d_}"])
        P.add("pool", lambda e, d_=d_, sgn=sgn: e.affine_select(out=MASKX[d_][:], in_=MASKX[d_][:], pattern=[[-sgn, 128]],
              compare_op=ALU.is_gt, fill=0.0, base=0, channel_multiplier=sgn), [f"maskx_{d_}"], [f"maskx_{d_}"])
    ST = P.sb([64, 16, 64], F32, "ST")
    STb = P.sb([64, 16, 64], F32 if cfg.get("rw_fp32", True) else BF16, "STb")
    zh = [P.sb([64, TB + 2], F32, f"zh{i}") for i in range(2)]
    sft = P.sb([64, TB], F32, "sft")
    lat = P.sb([64, 5, TB], BF16, "lat")
    sgl = P.sb([128, TB], BF16, "sgl")
    zg = P.sb([128, TB + 2], F32, "zg")
    sftg = P.sb([128, TB], F32, "sftg")
    CD = F32 if cfg.get("rw_fp32", True) else BF16
    ident_c = ident_f if CD == F32 else ident_b
    USE_R = cfg.get("fp32r", True) and CD == F32

    def RR(ap):
        return ap.bitcast(mybir.dt.float32r) if USE_R else ap

    identc_k = "identf" if CD == F32 else "identb"
    def mk_stream(sid):
        S = {}
        S["zh"] = [P.sb([64, TB + 2], F32, f"zhs{i}") for i in range(2)]
        S["rr_"] = P.sb([64, TB], F32, "rr")
        S["kk_"] = P.sb([64, TB], F32, "kk")
        S["vv_"] = P.sb([64, TB], F32, "vv")
        S["kn"] = P.sb([64, TB], F32, "kn")
        S["t1"] = P.sb([64, TB], F32, "t1")
        S["t2"] = P.sb([64, TB], F32, "t2")
        S["lw"] = P.sb([64, TB], F32, "lw")
        S["aa"] = P.sb([64, TB], F32, "aa")
        S["kd"] = P.sb([64, TB], F32, "kd")
        S["bb_"] = P.sb([64, TB], F32, "bbk")
        S["cl"] = P.sb([64, TB], F32, "cl")
        S["ex"] = P.sb([64, TB], F32, "ex")
        S["ART"] = P.sb([64, NCB, 2, CH], CD, "ART")
        S["BT"] = P.sb([64, TB], CD, "BT")
        S["KT"] = P.sb([64, TB], CD, "KT")
        S["BhT"] = P.sb([64, TB], CD, "BhT")
        S["KhT"] = P.sb([64, TB], CD, "KhT")
        S["AfT"] = P.sb([64, TB], CD, "AfT")
        S["RfT"] = P.sb([64, TB], CD, "RfT")
        S["VT"] = P.sb([64, TB], CD, "VT")
        S["rkv32"] = P.sb([64, 2, TB], F32, "rkv32")
        S["pc"] = P.sb([64, NCB], F32, "pc")
        S["clc"] = P.sb([64, NCB], F32, "clc")
        S["tok"] = P.sb([128, 5, 64], CD, "tok")
        S["NN"] = [P.sb([128, 256], CD, f"NN{i}") for i in range(2)]
        S["NX"] = [P.sb([128, 256], CD, f"NX{i}") for i in range(2)]
        S["Wt"] = [P.sb([128, 128], CD, f"Wt{i}") for i in range(2)]
        S["RhT"] = P.sb([64, 128], CD, "RhT")
        S["GB"] = P.sb([64, 64], CD, "GB")
        S["zi"] = 0
        S["ps"] = [ps[4 * sid + j] for j in (0, 1, 2, 0, 2, 3)]
        return S
    STREAMS = [mk_stream(0), mk_stream(1)]
    o_tok = P.sb([128, NCB, RW], F32, "o_tok")
    g_tok = P.sb([128, NCB, RW], F32, "g_tok")
    kb_tok = P.sb([128, NCB, RW], F32, "kb_tok")
    v_tok32 = P.sb([128, NCB, RW], F32, "v_tok32")
    of_t = P.sb([128, RW], F32, "of_t")
    st8 = P.sb([128, 16], F32, "st8")
    st9 = P.sb([128, 16], F32, "st9")
    onr = P.sb([128, RW], F32, "onr")

    seq_bounds = [(0, LP), (LP, T)]
    OT = [f"o_tok{h}" for h in range(16)]
    KBT = [f"kb_tok{h}" for h in range(16)]
    VTT = [f"v_tok32{h}" for h in range(16)]
    STK = [f"ST{h}" for h in range(16)]
    STBK = [f"STb{h}" for h in range(16)]

    def mk_kmap(h, sid):
        m = {}
        for n_ in ("kn", "t1", "t2", "lw", "aa", "kd", "bbk", "cl", "ex", "ART", "BT", "KT", "BhT", "KhT", "AfT", "RfT",
                   "VT", "rkv32", "pc", "clc", "tok", "NN0", "NN1", "NX0", "NX1", "Wt0", "Wt1", "RhT", "GB", "zh0", "zh1"):
            m[n_] = f"{n_}@{sid}"
        for i_, j_ in enumerate((0, 1, 2, 0, 2, 3)):
            m[f"ps{i_}"] = f"ps{4 * sid + j_}"
        m["ST"] = f"ST{h}"
        m["STb"] = f"STb{h}"
        m["o_tok"] = f"o_tok{h}"
        m["kb_tok"] = f"kb_tok{h}"
        m["v_tok32"] = f"v_tok32{h}"
        return m

    def load_shift(dst_sft, ztile, row0, nrow, t0, s0, s1, colj, key_z, key_o, omm_ap, hmu_ap):
        lo = t0 - 1
        hi = t0 + TB + 1
        a = max(lo, s0)
        b = min(hi, s1)
        if lo < s0:
            memset(ztile[0:nrow, 0:1], 0.0, [key_z])
        if hi > s1:
            memset(ztile[0:nrow, TB + 1:TB + 2], 0.0, [key_z])
        dma("dsp", ztile[0:nrow, a - lo:b - lo], zT_d[row0:row0 + nrow, a:b], (), [key_z])
        tt(dst_sft, ztile[0:nrow, 0:TB], ztile[0:nrow, 2:TB + 2], ALU.add, [key_z], [key_o])
        ts(dst_sft, dst_sft, hmu_ap, None, ALU.mult, None, [key_o], [key_o])
        stt(dst_sft, ztile[0:nrow, 1:TB + 1], omm_ap, dst_sft, ALU.mult, ALU.add, [key_z, key_o], [key_o])

    zi = 0
    for d_ in range(2):
        for (s0, s1) in seq_bounds:
            nblk = (s1 - s0) // TB
            memset(ST[:], 0.0, STK)
            memset(STb[:], 0.0, STBK)
            blks = range(nblk) if d_ == 0 else range(nblk - 1, -1, -1)
            for bi in blks:
                t0 = s0 + bi * TB
                for j_ in range(4):
                    zb = zi % 2
                    zi += 1
                    load_shift(sft[:], zh[zb], 3072 + j_ * 64, 64, t0, s0, s1, 48 + j_, f"zh{zb}", "sft",
                               ommT[:, 48 + j_:49 + j_], hmuT[:, 48 + j_:49 + j_])
                    if j_ < 2:
                        act(lat[:, j_, :], sft[:], AF.Tanh, ["sft"], ["lat"])
                    else:
                        cp(lat[:, j_, :], sft[:], ["sft"], ["lat"])
                load_shift(sftg[:], zg, 3328, 128, t0, s0, s1, 0, "zg", "sftg", mug[:, 1:2], mug[:, 2:3])
                act(sgl[:], sftg[:], AF.Sigmoid, ["sftg"], ["sgl"])
                for ci in range(NCB):
                    for half in range(2):
                        pi = 4 + half
                        mm(ps[pi][:, :], sgl[:, ci * CH:(ci + 1) * CH], gup[:, half * 512:(half + 1) * 512], True, True,
                           ["sgl", "gup"], [f"ps{pi}"])
                        cp(g_tok[:, ci, half * 512:(half + 1) * 512], ps[pi][:, :], [f"ps{pi}"], ["g_tok"], q="act")
                def head_body(h, sid):
                    S = STREAMS[sid]
                    ps = S["ps"]
                    zh = S["zh"]
                    rr_ = S["rr_"]
                    kk_ = S["kk_"]
                    vv_ = S["vv_"]
                    kn = S["kn"]
                    t1 = S["t1"]
                    t2 = S["t2"]
                    lw = S["lw"]
                    aa = S["aa"]
                    kd = S["kd"]
                    bb_ = S["bb_"]
                    cl = S["cl"]
                    ex = S["ex"]
                    ART = S["ART"]
                    BT = S["BT"]
                    KT = S["KT"]
                    BhT = S["BhT"]
                    KhT = S["KhT"]
                    AfT = S["AfT"]
                    RfT = S["RfT"]
                    VT = S["VT"]
                    rkv32 = S["rkv32"]
                    pc = S["pc"]
                    clc = S["clc"]
                    tok = S["tok"]
                    NN = S["NN"]
                    NX = S["NX"]
                    Wt = S["Wt"]
                    RhT = S["RhT"]
                    GB = S["GB"]
                    for (dst, base, colj) in ((rr_, 0, h), (kk_, 1024, 16 + h), (vv_, 2048, 32 + h)):
                        zb = S["zi"] % 2
                        S["zi"] += 1
                        load_shift(dst[:], zh[zb], base + h * 64, 64, t0, s0, s1, colj, f"zh{zb}", dst.name,
                                   ommT[:, colj:colj + 1], hmuT[:, colj:colj + 1])
                    ts(kn[:], kk_[:], vT[:, 0, h:h + 1], None, ALU.mult, None, [kk_.name, "vT"], ["kn"])
                    tt(t1[:], kn[:], kn[:], ALU.mult, ["kn"], ["t1"])
                    mm(ps[0][0:64, 0:TB], ones_f[0:64, 0:64], t1[:], True, True, ["ones_f", "t1"], ["ps0"])
                    act(t2[:], ps[0][0:64, 0:TB], AF.Sqrt, ["ps0"], ["t2"])
                    ts(t2[:], t2[:], 1e-12, None, ALU.max, None, ["t2"], ["t2"])
                    P.add("dve", lambda e: e.reciprocal(out=t2[:], in_=t2[:]), ["t2"], ["t2"])
                    tt(kn[:], kn[:], t2[:], ALU.mult, ["kn", "t2"], ["kn"])
                    mm(ps[1][0:64, 0:TB], wup[:, d_, h * 64:(h + 1) * 64], lat[:, d_, :], True, True, ["wup", "lat"], ["ps1"])
                    act(lw[:], ps[1][0:64, 0:TB], AF.Sigmoid, ["ps1", "w0T"], ["lw"], bias=w0T[:, d_, h:h + 1])
                    ts(lw[:], lw[:], -0.6065306597126334, None, ALU.mult, None, ["lw"], ["lw"])
                    mm(ps[2][0:64, 0:TB], aup[:, d_, h * 64:(h + 1) * 64], lat[:, 2 + d_, :], True, True, ["aup", "lat"], ["ps2"])
                    act(aa[:], ps[2][0:64, 0:TB], AF.Sigmoid, ["ps2", "a0T"], ["aa"], bias=a0T[:, d_, h:h + 1])
                    ts(t1[:], aa[:], vT[:, 1, h:h + 1], omka[:, h:h + 1], ALU.mult, ALU.add, ["aa", "vT", "omka"], ["t1"])
                    tt(kd[:], kk_[:], t1[:], ALU.mult, [kk_.name, "t1"], ["kd"])
                    tt(bb_[:], kn[:], aa[:], ALU.mult, ["kn", "aa"], ["bbk"])
                    stt(rkv32[:, 1, :], rr_[:], 0.5, kd[:], ALU.mult, ALU.mult, [rr_.name, "kd"], ["rkv32"])
                    for ci in range(NCB):
                        sl = slice(ci * CH, (ci + 1) * CH)
                        P.add("dve", lambda e, sl=sl: e.tensor_tensor_scan(out=cl[:, sl], data0=ones_f[0:64, 0:CH], data1=lw[:, sl],
                              initial=0.0, op0=ALU.mult, op1=ALU.add), ["ones_f", "lw"], ["cl"])
                        if d_ == 1:
                            cp(clc[:, ci:ci + 1], cl[:, ci * CH + CH - 1:ci * CH + CH], ["cl"], ["clc"])
                            tt(cl[:, sl], lw[:, sl], cl[:, sl], ALU.subtract, ["lw", "cl"], ["cl"])
                            ts(cl[:, sl], cl[:, sl], clc[:, ci:ci + 1], None, ALU.add, None, ["cl", "clc"], ["cl"])
                        else:
                            cp(clc[:, ci:ci + 1], cl[:, ci * CH + CH - 1:ci * CH + CH], ["cl"], ["clc"])
                    act(pc[:], clc[:], AF.Exp, ["clc"], ["pc"])
                    act(ex[:], cl[:], AF.Exp, ["cl"], ["ex"])
                    tt(t1[:], rr_[:], ex[:], ALU.mult, [rr_.name, "ex"], ["t1"])
                    for ci in range(NCB):
                        cp(RR(ART[:, ci, 1, :]), t1[:, ci * CH:(ci + 1) * CH], ["t1"], ["ART"], q="act")
                    cp(RfT[:], t1[:], ["t1"], ["RfT"], q="act")
                    tt(t2[:], cl[:], lw[:], ALU.subtract, ["cl", "lw"], ["t2"])
                    act(ex[:], t2[:], AF.Exp, ["t2"], ["ex"])
                    stt(t1[:], kn[:], -1.0, ex[:], ALU.mult, ALU.mult, ["kn", "ex"], ["t1"])
                    for ci in range(NCB):
                        cp(RR(ART[:, ci, 0, :]), t1[:, ci * CH:(ci + 1) * CH], ["t1"], ["ART"], q="act")
                    cp(AfT[:], t1[:], ["t1"], ["AfT"], q="act")
                    act(ex[:], cl[:], AF.Exp, ["cl"], ["ex"], scale=-1.0)
                    tt(RR(BT[:]), bb_[:], ex[:], ALU.mult, ["bbk", "ex"], ["BT"])
                    tt(RR(KT[:]), kd[:], ex[:], ALU.mult, ["kd", "ex"], ["KT"])
                    for ci in range(NCB):
                        sl = slice(ci * CH, (ci + 1) * CH)
                        act(ex[:, sl], cl[:, sl], AF.Exp, ["cl", "clc"], ["ex"], scale=-1.0, bias=clc[:, ci:ci + 1])
                    tt(BhT[:], bb_[:], ex[:], ALU.mult, ["bbk", "ex"], ["BhT"])
                    tt(KhT[:], kd[:], ex[:], ALU.mult, ["kd", "ex"], ["KhT"])
                    cp(VT[:], vv_[:], [vv_.name], ["VT"], q="act")
                    cis = range(NCB) if d_ == 0 else range(NCB - 1, -1, -1)
                    for ci in cis:
                        sl = slice(ci * CH, (ci + 1) * CH)
                        for i_, (src, sk) in enumerate(((AfT, "AfT"), (BhT, "BhT"), (KhT, "KhT"), (RfT, "RfT"), (VT, "VT"))):
                            tr(ps[3][:, 192 + i_ * 64:192 + (i_ + 1) * 64], src[:, sl], ident_c[0:64, 0:64], [sk, identc_k], ["ps3"])
                        cp(tok[:].rearrange("p a b -> p (a b)"), ps[3][:, 192:512], ["ps3"], ["tok"])
                        tr(ps[3][:, 0:64], rkv32[:, 1, sl], ident_f[0:64, 0:64], ["rkv32", "identf"], ["ps3"])
                        tr(ps[3][:, 64:128], rr_[:, sl], ident_f[0:64, 0:64], [rr_.name, "identf"], ["ps3"])
                        tr(ps[3][:, 128:192], vv_[:, sl], ident_f[0:64, 0:64], [vv_.name, "identf"], ["ps3"])
                        tt(kb_tok[:, ci, h * 64:(h + 1) * 64], ps[3][:, 0:64], rk_b[:, h * 64:(h + 1) * 64], ALU.mult, ["ps3", "rk_b"], ["kb_tok"]) if d_ == 0 else \
                            stt(kb_tok[:, ci, h * 64:(h + 1) * 64], ps[3][:, 0:64], 1.0, rk_b[:, h * 64:(h + 1) * 64], ALU.mult, ALU.mult, ["ps3", "rk_b"], ["kb_tok"])
                        cp(v_tok32[:, ci, h * 64:(h + 1) * 64], ps[3][:, 128:192], ["ps3"], ["v_tok32"], q="act")
                        mm(ps[0][:, 0:256], RR(BT[:, sl]), RR(ART[:, ci, :, :].rearrange("p a b -> p (a b)")), True, True, ["BT", "ART"], ["ps0"])
                        tt(NN[0][:], ps[0][:, 0:256], MASK2[d_][:], ALU.mult, ["ps0", f"mask2_{d_}"], ["NN0"])
                        mm(ps[1][:, 0:256], RR(KT[:, sl]), RR(ART[:, ci, :, :].rearrange("p a b -> p (a b)")), True, True, ["KT", "ART"], ["ps1"])
                        tt(NN[1][:], ps[1][:, 0:256], MASK2[d_][:], ALU.mult, ["ps1", f"mask2_{d_}"], ["NN1"])
                        mm(ps[2][:, 0:128], RR(ART[:, ci, 0, :]), RR(BT[:, sl]), True, True, ["ART", "BT"], ["ps2"])
                        cur = 0
                        cp(RR(NX[cur][:, 0:128]), NN[0][:, 0:128], ["NN0"], [f"NX{cur}"], q="act")
                        tt(RR(NX[cur][:, 128:256]), ps[2][:, 0:128], MASKX[d_][:], ALU.mult, ["ps2", f"maskx_{d_}"], [f"NX{cur}"])
                        mm(ps[2][:, 128:192], NN[1][:, 0:128], tok[:, 4, :], True, True, ["NN1", "tok"], ["ps2"])
                        cp(RR(Wt[0][:, 0:64]), tok[:, 0, :], ["tok"], ["Wt0"], q="act")
                        cp(RR(Wt[0][:, 64:128]), ps[2][:, 128:192], ["ps2"], ["Wt0"])
                        wc = 0
                        for j_ in range(7):
                            mm(ps[4][:, 0:128], RR(NX[cur][:, 0:128]), RR(Wt[wc][:]), True, True, [f"NX{cur}", f"Wt{wc}"], ["ps4"])
                            tt(RR(Wt[1 - wc][:]), ps[4][:, 0:128], Wt[wc][:], ALU.add, ["ps4", f"Wt{wc}"], [f"Wt{1 - wc}"])
                            wc = 1 - wc
                            if j_ < 6:
                                mm(ps[5][:, 0:128], RR(NX[cur][:, 128:256]), RR(NX[cur][:, 0:128]), True, True, [f"NX{cur}"], ["ps5"])
                                mm(ps[5][:, 128:256], RR(NX[cur][:, 0:128]), RR(NX[cur][:, 128:256]), True, True, [f"NX{cur}"], ["ps5"])
                                cp(RR(NX[1 - cur][:]), ps[5][:, 0:256], ["ps5"], [f"NX{1 - cur}"], q=("act" if j_ % 2 else "dve"))
                                cur = 1 - cur
                        W = Wt[wc]
                        wk = f"Wt{wc}"
                        mm(ps[0][0:64, 0:128], tok[:, 3, :], ident_c[:], True, False, ["tok", identc_k], ["ps0"])
                        mm(ps[0][0:64, 0:128], W[:, 0:64], NN[0][:, 128:256], False, True, [wk, "NN0"], ["ps0"])
                        cp(RhT[:], ps[0][0:64, 0:128], ["ps0"], ["RhT"], q="act")
                        mm(ps[1][:, 0:64], RhT[:], STb[:, h, :], True, False, ["RhT", "STb"], ["ps1"])
                        mm(ps[1][:, 0:64], NN[0][:, 128:256], W[:, 64:128], False, False, ["NN0", wk], ["ps1"])
                        mm(ps[1][:, 0:64], NN[1][:, 128:256], tok[:, 4, :], False, True, ["NN1", "tok"], ["ps1"])
                        cp(o_tok[:, ci, h * 64:(h + 1) * 64], ps[1][:, 0:64], ["ps1"], ["o_tok"])
                        mm(ps[2][0:64, 0:64], W[:, 0:64], tok[:, 1, :], True, True, [wk, "tok"], ["ps2"])
                        cp(GB[:], ps[2][0:64, 0:64], ["ps2"], ["GB"], q="act")
                        mm(ps[4][0:64, 0:64], GB[:], STb[:, h, :], True, False, ["GB", "STb"], ["ps4"])
                        mm(ps[4][0:64, 0:64], tok[:, 1, :], W[:, 64:128], False, False, ["tok", wk], ["ps4"])
                        mm(ps[4][0:64, 0:64], tok[:, 2, :], tok[:, 4, :], False, True, ["tok"], ["ps4"])
                        stt(ST[:, h, :], ST[:, h, :], pc[:, ci:ci + 1], ps[4][0:64, 0:64], ALU.mult, ALU.add, ["ST", "pc", "ps4"], ["ST"])
                        cp(STb[:, h, :], ST[:, h, :], ["ST"], ["STb"], q="act")
                for h2 in range(0, 16, 2):
                    lists = []
                    for sid in range(2):
                        P.cap = []
                        P.kmap = mk_kmap(h2 + sid, sid)
                        head_body(h2 + sid, sid)
                        lists.append(P.cap)
                        P.cap = None
                        P.kmap = None
                    P.merge(lists)
                for ci in range(NCB):
                    r0 = t0 + ci * CH
                    if d_ == 0:
                        dma("dsp", of_d[r0:r0 + CH, :], o_tok[:, ci, :], [*OT], ["of_d"])
                        dma("dsp", m_d[r0:r0 + CH, 0:RW], kb_tok[:, ci, :], [*KBT], ["m_d"])
                    else:
                        dma("dsp", of_t[:], of_d[r0:r0 + CH, :], ["of_d"], ["of_t"])
                        tt(o_tok[:, ci, :], o_tok[:, ci, :], of_t[:], ALU.add, [*OT, "of_t"], [*OT])
                        dma("dsp", of_t[:], m_d[r0:r0 + CH, 0:RW], ["m_d"], ["of_t"])
                        tt(kb_tok[:, ci, :], kb_tok[:, ci, :], of_t[:], ALU.add, [*KBT, "of_t"], [*KBT])
                        o3 = o_tok[:, ci, :].rearrange("p (h v) -> p h v", v=64)
                        P.add("dve", lambda e, o3=o3: e.tensor_reduce(out=st8[:], in_=o3, axis=AX.X, op=ALU.add), [*OT], ["st8"])
                        ts(st8[:], st8[:], 1.0 / 64, None, ALU.mult, None, ["st8"], ["st8"])
                        tt(o3, o3, st8[:].unsqueeze(2).to_broadcast([128, 16, 64]), ALU.subtract, [*OT, "st8"], [*OT])
                        tt(onr[:], o_tok[:, ci, :], o_tok[:, ci, :], ALU.mult, [*OT], ["onr"])
                        P.add("dve", lambda e: e.tensor_reduce(out=st9[:], in_=onr[:].rearrange("p (h v) -> p h v", v=64), axis=AX.X, op=ALU.add), ["onr"], ["st9"])
                        ts(st9[:], st9[:], 1.0 / 64, 64e-5, ALU.mult, ALU.add, ["st9"], ["st9"])
                        act(st9[:], st9[:], AF.Sqrt, ["st9"], ["st9"])
                        P.add("dve", lambda e: e.reciprocal(out=st9[:], in_=st9[:]), ["st9"], ["st9"])
                        tt(o3, o3, st9[:].unsqueeze(2).to_broadcast([128, 16, 64]), ALU.mult, [*OT, "st9"], [*OT])
                        tt(o_tok[:, ci, :], o_tok[:, ci, :], lnw_b[:], ALU.mult, [*OT, "lnw_b"], [*OT])
                        tt(o_tok[:, ci, :], o_tok[:, ci, :], lnb_b[:], ALU.add, [*OT, "lnb_b"], [*OT])
                        P.add("dve", lambda e, ci=ci: e.tensor_reduce(out=st8[:], in_=kb_tok[:, ci, :].rearrange("p (h v) -> p h v", v=64), axis=AX.X, op=ALU.add), [*KBT], ["st8"])
                        tt(onr[:].rearrange("p (h v) -> p h v", v=64), v_tok32[:, ci, :].rearrange("p (h v) -> p h v", v=64),
                           st8[:].unsqueeze(2).to_broadcast([128, 16, 64]), ALU.mult, [*VTT, "st8"], ["onr"])
                        tt(o_tok[:, ci, :], o_tok[:, ci, :], onr[:], ALU.add, [*OT, "onr"], [*OT])
                        tt(o_tok[:, ci, :], o_tok[:, ci, :], g_tok[:, ci, :], ALU.mult, [*OT, "g_tok"], [*OT])
                        dma("dsp", m_d[r0:r0 + CH, 0:RW], o_tok[:, ci, :], [*OT], ["m_d"])
    P.barrier()
    P.pop()
    TB = TB_saved

    if DBG == "p3":
        print("ops after P3:", len(P.ops))
        P.limit = None
        dbg = dout("dbg", [T, 2048])
        dma("dsp", dbg, m_d, (), ["dbg"])
        P.barrier()
        P.emit()
        return nc

    hg_gamma = din("hg_gamma", [2, 2, HG])
    hg_norm_w = din("hg_norm_w", [1, HG])
    P.push()
    C2 = 64
    NC2 = TB // C2
    gm = P.sb([128, 2, 2, 8], F32, "gm")
    for l_ in range(2):
        for d_ in range(2):
            dma("dsp", gm[:, l_, d_, :], hg_gamma[l_, d_, :].rearrange("(h p) -> p h", p=128), (), ["gm"], allow_slow_non_contiguous=True)
    lbT = P.sb([128, 2, 8], F32, "lbT")
    omlb = P.sb([128, 2, 8], F32, "omlb")
    tt(lbT[:], gm[:, 0, :, :], gm[:, 1, :, :], ALU.subtract, ["gm"], ["lbT"])
    act(lbT[:], lbT[:], AF.Sigmoid, ["lbT"], ["lbT"])
    ts(omlb[:], lbT[:], -1.0, 1.0, ALU.mult, ALU.add, ["lbT"], ["omlb"])
    hnw = P.sb([64, HG], F32, "hnw")
    dma("dsp", hnw[:], hg_norm_w[0:1, :].partition_broadcast(64), (), ["hnw"])
    ones2 = P.sb([128, 64], F32, "ones2")
    memset(ones2[:], 1.0, ["ones2"])
    MI = [P.sb([64, 64], F32, f"mi{d_}") for d_ in range(2)]
    for d_ in range(2):
        sgn = 1 if d_ == 0 else -1
        memset(MI[d_][:], 1.0, [f"mi{d_}"])
        P.add("pool", lambda e, d_=d_, sgn=sgn: e.affine_select(out=MI[d_][:], in_=MI[d_][:], pattern=[[sgn, 64]],
              compare_op=ALU.is_ge, fill=0.0, base=0, channel_multiplier=-sgn), [f"mi{d_}"], [f"mi{d_}"])
    S2 = P.sb([128, 8, 128], F32, "S2")
    NHS = 4
    def mk_hstream(sid):
        S = {}
        S["qz"] = P.sb([128, TB], F32, "qz")
        S["fz"] = P.sb([128, TB], F32, "fz")
        S["lf"] = P.sb([128, TB], F32, "lf")
        S["kq"] = P.sb([128, TB], F32, "kq")
        S["b2"] = P.sb([128, TB], F32, "b2")
        S["e2"] = P.sb([128, TB], F32, "e2")
        S["qT"] = P.sb([128, TB], F32, "qT")
        S["kT2"] = P.sb([128, TB], F32, "kT2")
        S["khT"] = P.sb([128, TB], F32, "khT")
        S["bc2"] = P.sb([128, NC2], F32, "bc2")
        S["pc2"] = P.sb([128, NC2], F32, "pc2")
        S["vt2"] = P.sb([64, NC2, 128], F32, "vt2")
        S["sc2_"] = P.sb([64, 64], F32, "sc2")
        S["kh_tok"] = P.sb([64, 128], F32, "kh_tok")
        S["ps"] = [ps[2 * sid], ps[2 * sid + 1], ps[2 * sid], None, ps[2 * sid + 1]]
        return S
    HSTREAMS = [mk_hstream(i) for i in range(NHS)]
    O2K = [f"o2_{h}" for h in range(8)]
    S2K = [f"S2_{h}" for h in range(8)]

    def mk_hkmap(h, sid):
        m = {}
        for n_ in ("qz", "fz", "lf", "kq", "b2", "e2", "qT", "kT2", "khT", "bc2", "pc2", "vt2", "sc2", "kh_tok"):
            m[n_] = f"{n_}@{sid}"
        m["ps0"] = f"ps{2 * sid}"
        m["ps2"] = f"ps{2 * sid}"
        m["ps1"] = f"ps{2 * sid + 1}"
        m["ps4"] = f"ps{2 * sid + 1}"
        m["S2"] = f"S2_{h}"
        m["o2"] = f"o2_{h}"
        return m
    gt2_ = P.sb([64, NC2, HG], F32, "gt2")
    o2 = P.sb([64, NC2, HG], F32, "o2")
    of2 = P.sb([64, HG], F32, "of2")
    sq2 = P.sb([64, HG], F32, "sq2")
    st2 = P.sb([64, 8], F32, "st2")
    for d_ in range(2):
        for (s0, s1) in seq_bounds:
            nblk = (s1 - s0) // TB
            memset(S2[:], 0.0, S2K)
            blks = range(nblk) if d_ == 0 else range(nblk - 1, -1, -1)
            for bi in blks:
                t0 = s0 + bi * TB
                if d_ == 1:
                    for ci in range(NC2):
                        dma("dsp", gt2_[:, ci, :], ztok_d[t0 + ci * C2:t0 + (ci + 1) * C2, 1024:2048], (), ["gt2"])
                def hg_body(h, sid):
                    S = HSTREAMS[sid]
                    ps = S["ps"]
                    qz = S["qz"]
                    fz = S["fz"]
                    lf = S["lf"]
                    kq = S["kq"]
                    b2 = S["b2"]
                    e2 = S["e2"]
                    qT = S["qT"]
                    kT2 = S["kT2"]
                    khT = S["khT"]
                    bc2 = S["bc2"]
                    pc2 = S["pc2"]
                    vt2 = S["vt2"]
                    sc2_ = S["sc2_"]
                    kh_tok = S["kh_tok"]
                    dma("dsp", qz[:], zT_d[3456 + h * 128:3456 + (h + 1) * 128, t0:t0 + TB], (), ["qz"])
                    dma("dsp", fz[:], zT_d[4480 + d_ * 1024 + h * 128:4480 + d_ * 1024 + (h + 1) * 128, t0:t0 + TB], (), ["fz"])
                    for ci in range(NC2):
                        dma("dsp", vt2[:, ci, :], ztok_d[t0 + ci * C2:t0 + (ci + 1) * C2, h * 128:(h + 1) * 128], (), ["vt2"])
                    act(qz[:], qz[:], AF.Silu, ["qz"], ["qz"])
                    act(fz[:], fz[:], AF.Sigmoid, ["fz"], ["fz"])
                    ts(fz[:], fz[:], omlb[:, d_, h:h + 1], lbT[:, d_, h:h + 1], ALU.mult, ALU.add, ["fz", "omlb", "lbT"], ["fz"])
                    act(lf[:], fz[:], AF.Ln, ["fz"], ["lf"])
                    ts(kq[:], fz[:], -1.0, 1.0, ALU.mult, ALU.add, ["fz"], ["kq"])
                    for ci in range(NC2):
                        sl = slice(ci * C2, (ci + 1) * C2)
                        P.add("dve", lambda e, sl=sl: e.tensor_tensor_scan(out=b2[:, sl], data0=ones2[:, 0:C2], data1=lf[:, sl],
                              initial=0.0, op0=ALU.mult, op1=ALU.add), ["ones2", "lf"], ["b2"])
                        cp(bc2[:, ci:ci + 1], b2[:, ci * C2 + C2 - 1:ci * C2 + C2], ["b2"], ["bc2"])
                        if d_ == 1:
                            tt(b2[:, sl], lf[:, sl], b2[:, sl], ALU.subtract, ["lf", "b2"], ["b2"])
                            ts(b2[:, sl], b2[:, sl], bc2[:, ci:ci + 1], None, ALU.add, None, ["b2", "bc2"], ["b2"])
                    act(pc2[:], bc2[:], AF.Exp, ["bc2"], ["pc2"])
                    act(e2[:], b2[:], AF.Exp, ["b2"], ["e2"])
                    tt(qT[:], qz[:], e2[:], ALU.mult, ["qz", "e2"], ["qT"])
                    act(e2[:], b2[:], AF.Exp, ["b2"], ["e2"], scale=-1.0)
                    tt(kT2[:], kq[:], e2[:], ALU.mult, ["kq", "e2"], ["kT2"])
                    for ci in range(NC2):
                        sl = slice(ci * C2, (ci + 1) * C2)
                        act(e2[:, sl], b2[:, sl], AF.Exp, ["b2", "bc2"], ["e2"], scale=-1.0, bias=bc2[:, ci:ci + 1])
                    tt(khT[:], kq[:], e2[:], ALU.mult, ["kq", "e2"], ["khT"])
                    cis = range(NC2) if d_ == 0 else range(NC2 - 1, -1, -1)
                    for ci in cis:
                        sl = slice(ci * C2, (ci + 1) * C2)
                        mm(ps[0][0:64, 0:64], kT2[:, sl], qT[:, sl], True, True, ["kT2", "qT"], ["ps0"])
                        tt(sc2_[:], ps[0][0:64, 0:64], MI[d_][:], ALU.mult, ["ps0", f"mi{d_}"], ["sc2"])
                        tr(ps[1][0:64, 0:128], khT[:, sl], ident_f[:], ["khT", "identf"], ["ps1"])
                        cp(kh_tok[:], ps[1][0:64, 0:128], ["ps1"], ["kh_tok"], q="act")
                        mm(ps[2][0:64, 0:128], sc2_[:], vt2[:, ci, :], True, False, ["sc2", "vt2"], ["ps2"])
                        mm(ps[2][0:64, 0:128], qT[:, sl], S2[:, h, :], False, True, ["qT", "S2"], ["ps2"])
                        cp(o2[:, ci, h * 128:(h + 1) * 128], ps[2][0:64, 0:128], ["ps2"], ["o2"], q="act")
                        mm(ps[4][:, 0:128], kh_tok[:], vt2[:, ci, :], True, True, ["kh_tok", "vt2"], ["ps4"])
                        stt(S2[:, h, :], S2[:, h, :], pc2[:, ci:ci + 1], ps[4][:, 0:128], ALU.mult, ALU.add, ["S2", "pc2", "ps4"], ["S2"])
                for h4 in range(0, 8, NHS):
                    lists = []
                    for sid in range(NHS):
                        P.cap = []
                        P.kmap = mk_hkmap(h4 + sid, sid)
                        hg_body(h4 + sid, sid)
                        lists.append(P.cap)
                        P.cap = None
                        P.kmap = None
                    P.merge(lists)
                for ci in range(NC2):
                    r0 = t0 + ci * C2
                    if d_ == 0:
                        dma("dsp", of_d[r0:r0 + C2, :], o2[:, ci, :], [*O2K], ["of_d"])
                    else:
                        dma("dsp", of2[:], of_d[r0:r0 + C2, :], ["of_d"], ["of2"])
                        tt(o2[:, ci, :], o2[:, ci, :], of2[:], ALU.add, [*O2K, "of2"], [*O2K])
                        tt(sq2[:], o2[:, ci, :], o2[:, ci, :], ALU.mult, [*O2K], ["sq2"])
                        P.add("dve", lambda e: e.tensor_reduce(out=st2[:], in_=sq2[:].rearrange("p (h v) -> p h v", v=128), axis=AX.X, op=ALU.add), ["sq2"], ["st2"])
                        ts(st2[:], st2[:], 1.0 / 128, 1e-6, ALU.mult, ALU.add, ["st2"], ["st2"])
                        act(st2[:], st2[:], AF.Sqrt, ["st2"], ["st2"])
                        P.add("dve", lambda e: e.reciprocal(out=st2[:], in_=st2[:]), ["st2"], ["st2"])
                        o3 = o2[:, ci, :].rearrange("p (h v) -> p h v", v=128)
                        tt(o3, o3, st2[:].unsqueeze(2).to_broadcast([64, 8, 128]), ALU.mult, [*O2K, "st2"], [*O2K])
                        tt(o2[:, ci, :], o2[:, ci, :], hnw[:], ALU.mult, [*O2K, "hnw"], [*O2K])
                        act(gt2_[:, ci, :], gt2_[:, ci, :], AF.Silu, ["gt2"], ["gt2"])
                        tt(o2[:, ci, :], o2[:, ci, :], gt2_[:, ci, :], ALU.mult, [*O2K, "gt2"], [*O2K])
                        dma("dsp", m_d[r0:r0 + C2, 1024:2048], o2[:, ci, :], [*O2K], ["m_d"])
    P.barrier()
    P.pop()

    if DBG == "p4":
        dbg = dout("dbg", [T, 2048])
        dma("dsp", dbg, m_d, (), ["dbg"])
        P.barrier()
        P.emit()
        return nc

    w_out = din("w_out", [D, D])
    x1_d = dscr("x1_d", [T, D])
    P.push()
    wo = P.sb([128, 16, D], BF16, "wo")
    for k in range(16):
        dma("dpool", wo[:, k, :], w_out[k * 128:(k + 1) * 128, :], (), ["wo"])
    gn1 = [P.sb([128, D], F32, f"gn1_{s_}") for s_ in range(2)]
    for s_ in range(2):
        dma("dsp", gn1[s_][:], vec_d[2, s_:s_ + 1, :].partition_broadcast(128), (), [f"gn1_{s_}"])
    mt = [P.sb([128, D], F32, f"mt{i}") for i in range(2)]
    mTs = [P.sb([128, 16, 128], BF16, f"mTs{i}") for i in range(2)]
    mo = [P.sb([128, D], F32, f"mo{i}") for i in range(2)]
    xr = [P.sb([128, D], F32, f"xr{i}") for i in range(2)]
    junk5 = P.sb([128, D], F32, "junk5")
    ss5 = [P.sb([128, 1], F32, f"ss5{i}") for i in range(2)]
    for t in range(NT):
        b = t % 2
        s_ = seq_of_tile(t)
        dma("dsp", mt[b][:], m_d[t * 128:(t + 1) * 128, :], (), [f"mt{b}"])
        dma("dsp", xr[b][:], x[t * 128:(t + 1) * 128, :], (), [f"xr{b}"])
        for kg in range(4):
            pk = 4 + kg
            for k4 in range(4):
                k = kg * 4 + k4
                tr(ps[pk][:, k4 * 128:(k4 + 1) * 128], mt[b][:, k * 128:(k + 1) * 128], ident_f[:], [f"mt{b}", "identf"], [f"ps{pk}"])
            cp(mTs[b][:, kg * 4:(kg + 1) * 4, :].rearrange("p a b -> p (a b)"), ps[pk][:, :], [f"ps{pk}"], [f"mTs{b}"], q=("act" if kg % 2 else "dve"))
        for cg in range(4):
            for k in range(16):
                mm(ps[cg][:, :], mTs[b][:, k, :], wo[:, k, cg * 512:(cg + 1) * 512], k == 0, k == 15, [f"mTs{b}", "wo"], [f"ps{cg}"])
            cp(mo[b][:, cg * 512:(cg + 1) * 512], ps[cg][:, :], [f"ps{cg}"], [f"mo{b}"], q=("act" if cg % 2 else "dve"))
        act(junk5[:], mo[b][:], AF.Square, [f"mo{b}"], ["junk5", f"ss5{b}"], accum_out=ss5[b][:])
        ts(ss5[b][:], ss5[b][:], 1.0 / D, 1e-6, ALU.mult, ALU.add, [f"ss5{b}"], [f"ss5{b}"])
        act(ss5[b][:], ss5[b][:], AF.Sqrt, [f"ss5{b}"], [f"ss5{b}"])
        P.add("dve", lambda e, b=b: e.reciprocal(out=ss5[b][:], in_=ss5[b][:]), [f"ss5{b}"], [f"ss5{b}"])
        stt(mo[b][:], mo[b][:], ss5[b][:], gn1[s_][:], ALU.mult, ALU.mult, [f"mo{b}", f"ss5{b}", f"gn1_{s_}"], [f"mo{b}"])
        tt(mo[b][:], mo[b][:], xr[b][:], ALU.add, [f"mo{b}", f"xr{b}"], [f"mo{b}"])
        dma("dsp", x1_d[t * 128:(t + 1) * 128, :], mo[b][:], [f"mo{b}"], ["x1_d"])
    P.barrier()
    P.pop()

    w_router = din("w_router", [D, NE])
    e_bias = din("e_bias", [1, NE])
    h2T_d = dscr("h2T_d", [16, 128, T], BF16)
    wselT_d = dscr("wselT_d", [NE, T])
    P.push()
    g2 = [P.sb([128, D], F32, f"g2_{s_}") for s_ in range(2)]
    s2 = [P.sb([128, D], F32, f"s2_{s_}") for s_ in range(2)]
    for s_ in range(2):
        dma("dsp", g2[s_][:], vec_d[3, s_:s_ + 1, :].partition_broadcast(128), (), [f"g2_{s_}"])
        dma("dsp", s2[s_][:], vec_d[4, s_:s_ + 1, :].partition_broadcast(128), (), [f"s2_{s_}"])
    wr = P.sb([128, 16, NE], F32, "wr")
    dma("dsp", wr[:], w_router.rearrange("(k p) e -> p k e", p=128), (), ["wr"])
    eb = P.sb([128, NE], F32, "eb")
    dma("dsp", eb[:], e_bias[0:1, :].partition_broadcast(128), (), ["eb"])
    x6 = [P.sb([128, D], F32, f"x6{i}") for i in range(2)]
    junk6 = P.sb([128, D], F32, "junk6")
    hTf = [P.sb([128, 16, 128], F32, f"hTf{i}") for i in range(2)]
    hTb6 = [P.sb([128, 16, 128], BF16, f"hTb6{i}") for i in range(2)]
    ss6 = [P.sb([128, 1], F32, f"ss6{i}") for i in range(2)]
    scr = P.sb([128, NE], F32, "scr")
    bi = P.sb([128, NE], F32, "bi")
    m8 = P.sb([128, 8, 8], F32, "m8")
    gs = P.sb([128, 8], F32, "gs")
    g8 = P.sb([128, 8], F32, "g8")
    gmask = P.sb([128, 8], F32, "gmask")
    emask = P.sb([128, NE], F32, "emask")
    mk = P.sb([128, NE], F32, "mk")
    tmk = P.sb([128, NE], F32, "tmk")
    t8 = P.sb([128, 8], F32, "t8")
    sel = P.sb([128, NE], F32, "sel")
    wsl = P.sb([128, NE], F32, "wsl")
    wsum = P.sb([128, 1], F32, "wsum")
    wT = P.sb([64, 128], F32, "wT")
    for t in range(NT):
        b = t % 2
        s_ = seq_of_tile(t)
        dma("dsp", x6[b][:], x1_d[t * 128:(t + 1) * 128, :], (), [f"x6{b}"])
        act(junk6[:], x6[b][:], AF.Square, [f"x6{b}"], ["junk6", f"ss6{b}"], accum_out=ss6[b][:])
        ts(ss6[b][:], ss6[b][:], 1.0 / D, 1e-6, ALU.mult, ALU.add, [f"ss6{b}"], [f"ss6{b}"])
        act(ss6[b][:], ss6[b][:], AF.Sqrt, [f"ss6{b}"], [f"ss6{b}"])
        P.add("dve", lambda e, b=b: e.reciprocal(out=ss6[b][:], in_=ss6[b][:]), [f"ss6{b}"], [f"ss6{b}"])
        stt(x6[b][:], x6[b][:], ss6[b][:], g2[s_][:], ALU.mult, ALU.mult, [f"x6{b}", f"ss6{b}", f"g2_{s_}"], [f"x6{b}"])
        tt(x6[b][:], x6[b][:], s2[s_][:], ALU.add, [f"x6{b}", f"s2_{s_}"], [f"x6{b}"])
        for kg in range(4):
            for kk2 in range(4):
                k = kg * 4 + kk2
                tr(ps[kg][:, kk2 * 128:(kk2 + 1) * 128], x6[b][:, k * 128:(k + 1) * 128], ident_f[:], [f"x6{b}", "identf"], [f"ps{kg}"])
            cp(hTf[b][:, kg * 4:(kg + 1) * 4, :].rearrange("p a b -> p (a b)"), ps[kg][:, :], [f"ps{kg}"], [f"hTf{b}"], q="act")
            cp(hTb6[b][:, kg * 4:(kg + 1) * 4, :].rearrange("p a b -> p (a b)"), ps[kg][:, :], [f"ps{kg}"], [f"hTb6{b}"])
        dma("dsp", h2T_d[:, :, t * 128:(t + 1) * 128].rearrange("k p t -> p k t"), hTb6[b][:], [f"hTb6{b}"], ["h2T_d"])
        for k in range(16):
            mm(ps[4][:, 0:NE], hTf[b][:, k, :], wr[:, k, :], k == 0, k == 15, [f"hTf{b}", "wr"], ["ps4"])
        act(scr[:], ps[4][:, 0:NE], AF.Sigmoid, ["ps4"], ["scr"])
        tt(bi[:], scr[:], eb[:], ALU.add, ["scr", "eb"], ["bi"])
        for g_ in range(8):
            P.add("dve", lambda e, g_=g_: e.max(out=m8[:, g_, :], in_=bi[:, g_ * 8:(g_ + 1) * 8]), ["bi"], ["m8"])
        tt(gs[:], m8[:, :, 0], m8[:, :, 1], ALU.add, ["m8"], ["gs"])
        P.add("dve", lambda e: e.max(out=g8[:], in_=gs[:]), ["gs"], ["g8"])
        ts(gmask[:], gs[:], g8[:, 3:4], None, ALU.is_ge, None, ["gs", "g8"], ["gmask"])
        cp(emask[:].rearrange("p (g j) -> p g j", j=8), gmask[:].unsqueeze(2).to_broadcast([128, 8, 8]), ["gmask"], ["emask"])
        ts(tmk[:], emask[:], 10.0, -10.0, ALU.mult, ALU.add, ["emask"], ["tmk"])
        tt(mk[:], bi[:], emask[:], ALU.mult, ["bi", "emask"], ["mk"])
        tt(mk[:], mk[:], tmk[:], ALU.add, ["mk", "tmk"], ["mk"])
        P.add("dve", lambda e: e.max(out=t8[:], in_=mk[:]), ["mk"], ["t8"])
        ts(sel[:], mk[:], t8[:, 5:6], None, ALU.is_ge, None, ["mk", "t8"], ["sel"])
        tt(wsl[:], scr[:], sel[:], ALU.mult, ["scr", "sel"], ["wsl"])
        P.add("dve", lambda e: e.tensor_reduce(out=wsum[:], in_=wsl[:], axis=AX.X, op=ALU.add), ["wsl"], ["wsum"])
        P.add("dve", lambda e: e.reciprocal(out=wsum[:], in_=wsum[:]), ["wsum"], ["wsum"])
        ts(wsl[:], wsl[:], wsum[:], 2.5, ALU.mult, ALU.mult, ["wsl", "wsum"], ["wsl"])
        tr(ps[5][0:64, 0:128], wsl[:], ident_f[:], ["wsl", "identf"], ["ps5"])
        cp(wT[:], ps[5][0:64, 0:128], ["ps5"], ["wT"], q="act")
        dma("dsp", wselT_d[:, t * 128:(t + 1) * 128], wT[:], ["wT"], ["wselT_d"])
    P.barrier()
    P.pop()

    if DBG == "p6":
        dbg = dout("dbg", [NE, T])
        dma("dsp", dbg, wselT_d, (), ["dbg"])
        P.barrier()
        P.emit()
        return nc

    NEC = DE // 128
    TBM = TB
    NTI = TBM // 128
    P.push()
    h2b = P.sb([128, 16, TBM], BF16, "h2b")
    wgt = [P.sb([128, 16, DE], BF16, f"wgt{i}") for i in range(2)]
    wut = [P.sb([128, 16, DE], BF16, f"wut{i}") for i in range(2)]
    wdt = [P.sb([128, NEC, D], BF16, f"wdt{i}") for i in range(2)]
    wbt = [P.sb([128, TBM], F32, f"wbt{i}") for i in range(2)]
    yacc = P.sb([128, NTI, D], F32, "yacc")
    actT = [P.sb([128, NEC, TBM], BF16, f"actT{i}") for i in range(2)]
    sgt = [P.sb([128, TBM], F32, f"sgt{i}") for i in range(2)]
    a1t = [P.sb([128, TBM], F32, f"a1t{i}") for i in range(2)]
    gn2 = P.sb([128, D], F32, "gn2")
    x7 = P.sb([128, D], F32, "x7")
    ss7 = P.sb([128, 1], F32, "ss7")
    junk7 = P.sb([128, D], F32, "junk7")

    def wsrc(e):
        return ("persist:wg%d" % e, "persist:wu%d" % e, "persist:wd%d" % e)

    def load_w(e, par):
        kg_, ku_, kd_ = wsrc(e)
        dma("dsp", wgt[par][:].rearrange("p k n -> p (k n)"), wg_b[e], [kg_], [f"wgt{par}"])
        dma("dsp", wut[par][:].rearrange("p k n -> p (k n)"), wu_b[e], [ku_], [f"wut{par}"])
        dma("dsp", wdt[par][:].rearrange("p k n -> p (k n)"), wd_b[e], [kd_], [f"wdt{par}"])

    order = [NE] + list(range(NE))
    gi = 0
    for tb in range(T // TBM):
        t0 = tb * TBM
        s_ = 0 if t0 < LP else 1
        dma("dsp", h2b[:], h2T_d[:, :, t0:t0 + TBM].rearrange("k p t -> p k t"), (), ["h2b"])
        dma("dsp", gn2[:], vec_d[5, s_:s_ + 1, :].partition_broadcast(128), (), ["gn2"])
        load_w(order[0], gi % 2)

        def GU(i, e, par):
            if e < NE:
                dma("dsp", wbt[par][:], wselT_d[e:e + 1, t0:t0 + TBM].partition_broadcast(128), (), [f"wbt{par}"])
            for ec in range(NEC):
                pg = ps[ec % 2]
                pu = ps[2 + ec % 2]
                kg_ = f"ps{ec % 2}"
                ku_ = f"ps{2 + ec % 2}"
                for k in range(16):
                    mm(pg[:, 0:TBM], wgt[par][:, k, ec * 128:(ec + 1) * 128], h2b[:, k, :], k == 0, k == 15, [f"wgt{par}", "h2b"], [kg_])
                for k in range(16):
                    mm(pu[:, 0:TBM], wut[par][:, k, ec * 128:(ec + 1) * 128], h2b[:, k, :], k == 0, k == 15, [f"wut{par}", "h2b"], [ku_])
                sb_ = ec % 2
                act(sgt[sb_][:], pg[:, 0:TBM], AF.Silu, [kg_], [f"sgt{sb_}"])
                if e < NE:
                    tt(a1t[sb_][:], sgt[sb_][:], pu[:, 0:TBM], ALU.mult, [f"sgt{sb_}", ku_], [f"a1t{sb_}"])
                    tt(actT[par][:, ec, :], a1t[sb_][:], wbt[par][:], ALU.mult, [f"a1t{sb_}", f"wbt{par}"], [f"actT{par}"])
                else:
                    tt(actT[par][:, ec, :], sgt[sb_][:], pu[:, 0:TBM], ALU.mult, [f"sgt{sb_}", ku_], [f"actT{par}"])

        def DN(i, e, par, first):
            j = 0
            for ti in range(NTI):
                for cg in range(4):
                    pd = ps[4 + j % 2]
                    kd_ = f"ps{4 + j % 2}"
                    j += 1
                    for ec in range(NEC):
                        mm(pd[:, :], actT[par][:, ec, ti * 128:(ti + 1) * 128], wdt[par][:, ec, cg * 512:(cg + 1) * 512],
                           ec == 0, ec == NEC - 1, [f"actT{par}", f"wdt{par}"], [kd_])
                    if first:
                        cp(yacc[:, ti, cg * 512:(cg + 1) * 512], pd[:, :], [kd_], ["yacc"])
                    else:
                        tt(yacc[:, ti, cg * 512:(cg + 1) * 512], yacc[:, ti, cg * 512:(cg + 1) * 512], pd[:, :], ALU.add, ["yacc", kd_], ["yacc"])

        n_e = len(order)
        for i, e in enumerate(order):
            par = (gi + i) % 2
            GU(i, e, par)
            if i > 0:
                DN(i - 1, order[i - 1], (gi + i - 1) % 2, i - 1 == 0)
            if i + 1 < n_e:
                load_w(order[i + 1], (gi + i + 1) % 2)
        DN(n_e - 1, order[-1], (gi + n_e - 1) % 2, False)
        gi += n_e
        for ti in range(NTI):
            r0 = t0 + ti * 128
            dma("dsp", x7[:], x1_d[r0:r0 + 128, :], (), ["x7"])
            P.add("dve", lambda e, ti=ti: e.tensor_tensor(out=junk7[:], in0=yacc[:, ti, :], in1=yacc[:, ti, :], op=ALU.mult), ["yacc"], ["junk7"])
            P.add("dve", lambda e: e.tensor_reduce(out=ss7[:], in_=junk7[:], axis=AX.X, op=ALU.add), ["junk7"], ["ss7"])
            ts(ss7[:], ss7[:], 1.0 / D, 1e-6, ALU.mult, ALU.add, ["ss7"], ["ss7"])
            act(ss7[:], ss7[:], AF.Sqrt, ["ss7"], ["ss7"])
            P.add("dve", lambda e: e.reciprocal(out=ss7[:], in_=ss7[:]), ["ss7"], ["ss7"])
            stt(junk7[:], yacc[:, ti, :], ss7[:], gn2[:], ALU.mult, ALU.mult, ["yacc", "ss7", "gn2"], ["junk7"])
            tt(junk7[:], junk7[:], x7[:], ALU.add, ["junk7", "x7"], ["junk7"])
            dma("dsp", y[r0:r0 + 128, :], junk7[:], ["junk7"], ["y"])
    P.barrier()
    P.pop()


    P.barrier()
    P.emit()
    return nc


def kernel(**inp):
    from concourse.bass_utils import run_bass_kernel_spmd
    n = 8
    LP = inp["x_prompt"].shape[1]
    LS = inp["x_sample"].shape[1]
    nc = build_program(dict(LP=LP, LS=LS, DE=int(inp["w_exp_gate"].shape[-1])))
    f = lambda a: np.ascontiguousarray(np.asarray(a, dtype=np.float32))
    shared = dict(
        w_ada=f(inp["w_ada"][0]), b_ada=f(inp["b_ada"][0:1]),
        nrm=f(np.stack([inp["norm_pre_mix"][0], inp["norm_post_mix"][0], inp["norm_pre_ffn"][0], inp["norm_post_ffn"][0]])),
        w_in=f(inp["w_in"][0]), rw_mu=f(inp["rw_mu"][0]), rw_w0=f(inp["rw_w0"][0]), rw_a0=f(inp["rw_a0"][0]),
        rw_w_up=f(inp["rw_w_up"][0]), rw_a_up=f(inp["rw_a_up"][0]), rw_g_up=f(inp["rw_g_up"][0]),
        rw_vecs=f(np.stack([inp["rw_k_k"][0], inp["rw_k_a"][0], inp["rw_r_k"][0].reshape(-1), inp["rw_ln_w"][0], inp["rw_ln_b"][0]])),
        hg_gamma=f(inp["hg_lb_gamma"]), hg_norm_w=f(inp["hg_norm_w"][0:1]), w_out=f(inp["w_out"][0]),
        w_router=f(inp["w_router"][0]), e_bias=f(inp["e_bias"][0:1]),
        w_exp_gate=f(inp["w_exp_gate"][0]), w_exp_up=f(inp["w_exp_up"][0]), w_exp_down=f(inp["w_exp_down"][0]),
        w_sh_gate=f(inp["w_sh_gate"][0]), w_sh_up=f(inp["w_sh_up"][0]), w_sh_down=f(inp["w_sh_down"][0]),
    )
    in_maps = []
    for b in range(n):
        m = dict(shared)
        m["x"] = f(np.concatenate([inp["x_prompt"][b], inp["x_sample"][b]], axis=0))
        m["c"] = f(np.stack([inp["c_prompt"][b], inp["c_sample"][b]]))
        in_maps.append(m)
    res = run_bass_kernel_spmd(nc, in_maps, core_ids=list(range(n)))
    ys = [r["y"] for r in res.results]
    y_prompt = np.stack([yy[:LP] for yy in ys]).astype(np.float32)
    y_sample = np.stack([yy[LP:] for yy in ys]).astype(np.float32)
    return (y_prompt, y_sample)
```
